# Optimizing a Trainium2 kernel written in Bass

```python
import math
import jax, jax.numpy as jnp
from jax import lax
import numpy as np

D_MODEL = 1024
BATCH = 8
SEQ = 4096
DEPTH = 1

PLE_DIM = 256
ATTN_WIDTH = D_MODEL // 2
CONV_WIDTH = D_MODEL - ATTN_WIDTH
ATTN_HEAD_DIM = 64
N_ATTN_HEADS = ATTN_WIDTH // (2 * ATTN_HEAD_DIM)
ATTN_V_DIM = 2 * ATTN_HEAD_DIM
QK_COLS = N_ATTN_HEADS * 2 * ATTN_HEAD_DIM
V_COLS = N_ATTN_HEADS * ATTN_V_DIM
IN_COLS = 2 * QK_COLS + V_COLS + 2 * CONV_WIDTH
CONV_KERNEL = 31
ROPE_THETA = 10000.0
Q_BLOCK = 128
N_PEER_HEADS = 8
N_KEYS = 128
N_EXPERTS = N_KEYS * N_KEYS
PEER_TOPK = 16
PEER_QUERY_DIM = 256
PEER_HALF = PEER_QUERY_DIM // 2
PEER_CHUNK = 128
EPS = 1e-6

kernel_name = "hymba_conv_diffattn_peer_ple"


def rms_norm(x, g):
    xf = x.astype(jnp.float32)
    y = xf * lax.rsqrt(jnp.mean(xf * xf, axis=-1, keepdims=True) + EPS)
    return (y * g.astype(jnp.float32)).astype(x.dtype)


def layer_norm(x, g, b):
    xf = x.astype(jnp.float32)
    mu = jnp.mean(xf, axis=-1, keepdims=True)
    var = jnp.mean(jnp.square(xf - mu), axis=-1, keepdims=True)
    y = (xf - mu) * lax.rsqrt(var + EPS)
    return (y * g.astype(jnp.float32) + b.astype(jnp.float32)).astype(x.dtype)


def rope(x, pos):
    d = x.shape[-1]
    half = d // 2
    inv_freq = 1.0 / (ROPE_THETA ** (jnp.arange(half, dtype=jnp.float32) * 2.0 / d))
    ang = pos.astype(jnp.float32)[:, None] * inv_freq[None, :]
    cos, sin = jnp.cos(ang), jnp.sin(ang)
    x1, x2 = x[..., :half], x[..., half:]
    return jnp.concatenate([x1 * cos - x2 * sin, x2 * cos + x1 * sin], axis=-1)


def diff_attention(q, k, v, lam, subln_g, lambda_init):
    B, H, _, S, d = q.shape
    pos = jnp.arange(S)
    qf = rope(q.astype(jnp.float32), pos) * (d ** -0.5)
    kf = rope(k.astype(jnp.float32), pos)
    vf = v.astype(jnp.float32)
    nb = S // Q_BLOCK
    qb = jnp.moveaxis(qf.reshape(B, H, 2, nb, Q_BLOCK, d), 3, 0)

    def block(args):
        qblk, i = args
        s = jnp.einsum('bhcqd,bhckd->bhcqk', qblk, kf)
        qpos = i * Q_BLOCK + jnp.arange(Q_BLOCK)
        mask = pos[None, :] <= qpos[:, None]
        s = jnp.where(mask, s, -jnp.inf)
        pr = jax.nn.softmax(s, axis=-1)
        a = pr[:, :, 0] - lam * pr[:, :, 1]
        return jnp.einsum('bhqk,bhkd->bhqd', a, vf)

    out = lax.map(block, (qb, jnp.arange(nb)))
    out = out.transpose(1, 0, 3, 2, 4).reshape(B, S, H, -1)
    out = rms_norm(out, subln_g) * (1.0 - lambda_init)
    return out.reshape(B, S, -1)


def conv_module(cv, cg, w, b, ln_g, ln_b):
    u = cv * jax.nn.sigmoid(cg)
    y = lax.conv_general_dilated(
        u, w[:, None, :].astype(u.dtype), window_strides=(1,),
        padding=[(CONV_KERNEL - 1, 0)], dimension_numbers=('NWC', 'WIO', 'NWC'),
        feature_group_count=CONV_WIDTH) + b
    return jax.nn.silu(layer_norm(y, ln_g, ln_b))


def peer(m, wq, keys, u_tab, v_tab):
    B, S, D = m.shape
    T = B * S
    mf = m.reshape(T, D)
    q = (mf @ wq).reshape(T, N_PEER_HEADS, 2, PEER_HALF)
    s = jnp.einsum('thcd,cnd->thcn', q.astype(jnp.float32), keys.astype(jnp.float32))
    sv, si = lax.top_k(s, PEER_TOPK)
    cand = sv[:, :, 0, :, None] + sv[:, :, 1, None, :]
    cs, ci = lax.top_k(cand.reshape(T, N_PEER_HEADS, PEER_TOPK * PEER_TOPK), PEER_TOPK)
    e1 = jnp.take_along_axis(si[:, :, 0], ci // PEER_TOPK, axis=-1)
    e2 = jnp.take_along_axis(si[:, :, 1], ci % PEER_TOPK, axis=-1)
    hk = N_PEER_HEADS * PEER_TOPK
    experts = (e1 * N_KEYS + e2).reshape(T, hk)
    gates = jax.nn.softmax(cs, axis=-1).reshape(T, hk)
    nc = T // PEER_CHUNK

    def chunk(args):
        xc, ec, gc = args
        u = u_tab[ec]
        a = jnp.einsum('cd,ckd->ck', xc, u).astype(jnp.float32)
        h = jax.nn.gelu(a, approximate=False) * gc
        vv = v_tab[ec]
        return jnp.einsum('ck,ckd->cd', h.astype(vv.dtype), vv)

    y = lax.map(chunk, (mf.reshape(nc, PEER_CHUNK, D), experts.reshape(nc, PEER_CHUNK, hk),
                        gates.reshape(nc, PEER_CHUNK, hk)))
    return y.reshape(B, S, D).astype(m.dtype)


def setup_inputs(seed: int = 0) -> dict:
    key = jax.random.key(seed)
    ks = jax.random.split(key, 24)
    f32 = jnp.float32
    nrm = lambda k, shape, s: jax.random.normal(k, shape, f32) * s
    gain = lambda k, shape: 1.0 + 0.02 * jax.random.normal(k, shape, f32)
    L, D = DEPTH, D_MODEL
    return {
        "x": nrm(ks[0], (BATCH, SEQ, D), 1.0),
        "p": nrm(ks[1], (DEPTH, BATCH, SEQ, PLE_DIM), 1.0),
        "attn_norm_g": gain(ks[2], (L, D)),
        "w_in": nrm(ks[3], (L, D, IN_COLS), D ** -0.5),
        "lambda_q1": nrm(ks[4], (L, ATTN_HEAD_DIM), 0.1),
        "lambda_k1": nrm(ks[5], (L, ATTN_HEAD_DIM), 0.1),
        "lambda_q2": nrm(ks[6], (L, ATTN_HEAD_DIM), 0.1),
        "lambda_k2": nrm(ks[7], (L, ATTN_HEAD_DIM), 0.1),
        "subln_g": gain(ks[8], (L, ATTN_V_DIM)),
        "conv_w": nrm(ks[9], (L, CONV_KERNEL, CONV_WIDTH), CONV_KERNEL ** -0.5),
        "conv_b": nrm(ks[10], (L, CONV_WIDTH), 0.02),
        "conv_ln_g": gain(ks[11], (L, CONV_WIDTH)),
        "conv_ln_b": nrm(ks[12], (L, CONV_WIDTH), 0.02),
        "w_out": nrm(ks[13], (L, D, D), D ** -0.5),
        "ffn_norm_g": gain(ks[14], (L, D)),
        "peer_wq": nrm(ks[15], (L, D, N_PEER_HEADS * PEER_QUERY_DIM), D ** -0.5),
        "peer_keys": nrm(ks[16], (L, 2, N_KEYS, PEER_HALF), PEER_HALF ** -0.5),
        "peer_u": nrm(ks[17], (L, N_EXPERTS, D), D ** -0.5),
        "peer_v": nrm(ks[18], (L, N_EXPERTS, D), N_PEER_HEADS ** -0.5),
        "ple_norm_g": gain(ks[19], (L, D)),
        "ple_w_gate": nrm(ks[20], (L, D, D), D ** -0.5),
        "ple_w_proj": nrm(ks[21], (L, PLE_DIM, D), PLE_DIM ** -0.5),
        "final_norm_g": gain(ks[22], (D,)),
    }


def reference(x, p, attn_norm_g, w_in, lambda_q1, lambda_k1, lambda_q2, lambda_k2, subln_g,
              conv_w, conv_b, conv_ln_g, conv_ln_b, w_out, ffn_norm_g, peer_wq, peer_keys,
              peer_u, peer_v, ple_norm_g, ple_w_gate, ple_w_proj, final_norm_g):
    B, S, D = x.shape
    H, d, dv = N_ATTN_HEADS, ATTN_HEAD_DIM, ATTN_V_DIM
    h = x
    for l in range(DEPTH):
        a = rms_norm(h, attn_norm_g[l])
        z = a @ w_in[l]
        q = z[..., :QK_COLS].reshape(B, S, H, 2, d).transpose(0, 2, 3, 1, 4)
        k = z[..., QK_COLS:2 * QK_COLS].reshape(B, S, H, 2, d).transpose(0, 2, 3, 1, 4)
        v = z[..., 2 * QK_COLS:2 * QK_COLS + V_COLS].reshape(B, S, H, dv).transpose(0, 2, 1, 3)
        c0 = 2 * QK_COLS + V_COLS
        cv = z[..., c0:c0 + CONV_WIDTH]
        cg = z[..., c0 + CONV_WIDTH:]
        lambda_init = 0.8 - 0.6 * math.exp(-0.3 * l)
        lam = (jnp.exp(jnp.sum(lambda_q1[l].astype(jnp.float32) * lambda_k1[l].astype(jnp.float32)))
               - jnp.exp(jnp.sum(lambda_q2[l].astype(jnp.float32) * lambda_k2[l].astype(jnp.float32)))
               + lambda_init)
        attn_out = diff_attention(q, k, v, lam, subln_g[l], lambda_init).astype(h.dtype)
        conv_out = conv_module(cv, cg, conv_w[l], conv_b[l], conv_ln_g[l], conv_ln_b[l]).astype(h.dtype)
        h = h + jnp.concatenate([attn_out, conv_out], axis=-1) @ w_out[l]
        h = h + peer(rms_norm(h, ffn_norm_g[l]), peer_wq[l], peer_keys[l], peer_u[l], peer_v[l])
        e = p[l] @ ple_w_proj[l]
        gate = jax.nn.sigmoid(rms_norm(h, ple_norm_g[l]) @ ple_w_gate[l])
        h = h + e * gate
    return rms_norm(h, final_norm_g)
```

```python
import numpy as np
import concourse.bass as bass
import concourse.mybir as mybir
from concourse.bass_utils import run_bass_kernel_spmd

F32 = mybir.dt.float32
BF16 = mybir.dt.bfloat16
U32 = mybir.dt.uint32
AF = mybir.ActivationFunctionType
ALU = mybir.AluOpType
AX = mybir.AxisListType

D = 1024
S = 4096
NCORES = 8
EPS = 1e-6
NT = S // 128
TC = 512
NCH = S // TC
PC = 256
NPC = S // PC
LAMBDA_INIT = 0.2


class Buf:
    __slots__ = ("name", "last_w", "rd_comp", "rd_dma", "pre", "subs")

    def __init__(self, name):
        self.name = name
        self.pre = None
        self.subs = None
        self.last_w = None
        self.rd_comp = {}
        self.rd_dma = []


class Op:
    __slots__ = ("eng", "fn", "deps", "is_dma", "slot", "sem_val", "signal", "cnt", "idx")


ENGS = ["pe", "act", "dve", "pool", "sp"]
DMA_SLOTS = {"sp": 24, "pool": 8, "act": 4}


class Prog:
    def __init__(self, nc):
        self.nc = nc
        self.by_eng = {e: [] for e in ENGS}
        self.nops = 0
        self.dma_rr = {e: 0 for e in DMA_SLOTS}
        self.dma_last = {}
        self.dma_tot = {}

    def add(self, eng, fn, reads=(), writes=(), dma=False):
        op = Op()
        op.eng = eng
        op.fn = fn
        op.is_dma = dma
        op.signal = False
        op.cnt = 0
        op.idx = self.nops
        self.nops += 1
        deps = set()
        weak = set()
        for b in reads:
            if b.last_w is not None:
                deps.add(b.last_w)
        for b in writes:
            if b.last_w is not None:
                weak.add(b.last_w)
            weak.update(b.rd_comp.values())
            weak.update(b.rd_dma)
            if b.pre:
                for (_lo, _hi, ot) in b.pre:
                    for ob in [ot.b] + (ot.b.subs or []):
                        if ob.last_w is not None:
                            weak.add(ob.last_w)
                        weak.update(ob.rd_comp.values())
                        weak.update(ob.rd_dma)
                b.pre = None
        for d in weak:
            deps.add(d)
        if dma:
            slot = self.dma_rr[eng]
            self.dma_rr[eng] = (slot + 1) % DMA_SLOTS[eng]
            prev = self.dma_last.get((eng, slot))
            if prev is not None:
                deps.add(prev)
            self.dma_last[(eng, slot)] = op
            tot = self.dma_tot.get((eng, slot), 0) + 16
            self.dma_tot[(eng, slot)] = tot
            op.slot = slot
            op.sem_val = tot
        op.deps = [d for d in deps if not (d is op) and not (eng == "pe" and d.eng == "pe" and not d.is_dma and not dma)]
        for d in op.deps:
            if not d.is_dma:
                d.signal = True
        for b in reads:
            if dma:
                b.rd_dma.append(op)
            else:
                b.rd_comp[eng] = op
        for b in writes:
            b.last_w = op
            b.rd_comp = {}
            b.rd_dma = []
        self.by_eng[eng].append(op)
        return op

    def emit(self):
        nc = self.nc
        from contextlib import ExitStack
        with ExitStack() as es:
            csem = {e: es.enter_context(nc.semaphore("c_" + e)) for e in ENGS}
            dsem = {}
            for e, n in DMA_SLOTS.items():
                for s in range(n):
                    dsem[(e, s)] = es.enter_context(nc.semaphore("d_%s_%d" % (e, s)))
            for e in ENGS:
                c = 0
                for op in self.by_eng[e]:
                    if op.signal and not op.is_dma:
                        c += 1
                        op.cnt = c
            engobj = {"pe": "tensor", "act": "scalar", "dve": "vector", "pool": "gpsimd", "sp": "sync"}
            block = es.enter_context(nc.Block())

            def run(e, eng):
                waited = {}
                for op in self.by_eng[e]:
                    need = {}
                    for d in op.deps:
                        if d.is_dma:
                            key = ("d", d.eng, d.slot)
                            val = d.sem_val
                        else:
                            key = ("c", d.eng)
                            val = d.cnt
                        if need.get(key, 0) < val:
                            need[key] = val
                    for key, val in need.items():
                        if waited.get(key, 0) >= val:
                            continue
                        waited[key] = val
                        sem = csem[key[1]] if key[0] == "c" else dsem[(key[1], key[2])]
                        eng.wait_ge(sem, val)
                    ins = op.fn(eng)
                    if op.is_dma:
                        ins.then_inc(dsem[(e, op.slot)], 16)
                    elif op.signal:
                        ins.then_inc(csem[e], 1)
                if e == "sp":
                    for (qe, s), tot in self.dma_tot.items():
                        eng.wait_ge(dsem[(qe, s)], tot)

            for e in ENGS:
                if not self.by_eng[e] and e != "sp":
                    continue
                getattr(block, engobj[e])(lambda eng, e=e: run(e, eng))


class Tile:
    __slots__ = ("t", "b", "lo", "hi", "pre")

    def __getitem__(self, k):
        return self.t[k]

    def sub(self, n):
        out = []
        for i in range(n):
            b = Buf("%s.%d" % (self.b.name, i))
            b.pre = list(self.pre)
            out.append(b)
        self.b.subs = (self.b.subs or []) + out
        return out


class Arena:
    def __init__(self, nc, limit=229344):
        self.nc = nc
        self.off = 16512
        self.limit = limit
        self.n = 0
        self.hist = []

    def alloc(self, shape, dtype, name="t"):
        esz = {F32: 4, BF16: 2, U32: 4}[dtype]
        free = 1
        for s in shape[1:]:
            free *= s
        nbytes = (free * esz + 63) // 64 * 64
        assert self.off + nbytes <= self.limit, ("SBUF overflow", name, self.off, nbytes)
        self.n += 1
        T = Tile()
        T.t = self.nc.alloc_sbuf_tensor_at("%s_%d" % (name, self.n), list(shape), dtype, offset=self.off)
        T.lo = self.off
        T.hi = self.off + nbytes
        T.b = Buf(name)
        T.pre = [o for o in self.hist if o[0] < T.hi and T.lo < o[1]]
        T.b.pre = list(T.pre)
        self.hist.append((T.lo, T.hi, T))
        self.off += nbytes
        return T

    def mark(self):
        return self.off

    def release(self, m):
        self.off = m


class KB:
    def __init__(self, debug=False):
        self.nc = nc = bass.Bass("TRN2", target_bir_lowering=False)
        self.P = Prog(nc)
        self.A = Arena(nc)
        self.debug = debug
        self.ps = [nc.alloc_psum_tensor("ps%d" % i, [128, 512], F32).ap() for i in range(8)]
        self.psb = [p.bitcast(BF16) for p in self.ps]
        self.bps = [[Buf("ps%d" % i)] for i in range(8)]

    def din(self, name, shape, dt=F32):
        return self.nc.dram_tensor(name, list(shape), dt, kind="ExternalInput").ap()

    def dout(self, name, shape, dt=F32):
        return self.nc.dram_tensor(name, list(shape), dt, kind="ExternalOutput").ap()

    def dscr(self, name, shape, dt):
        return self.nc.dram_tensor(name, list(shape), dt, kind="Internal").ap()

    def dma(self, out, in_, reads=(), writes=(), q="sp", **kw):
        return self.P.add(q, lambda e: e.dma_start(out=out, in_=in_, **kw), reads, writes, dma=True)

    def act(self, out, in_, func, reads, writes, **kw):
        return self.P.add("act", lambda e: e.activation(out=out, in_=in_, func=func, **kw), reads, writes)

    def tt(self, eng, out, in0, in1, op, reads, writes):
        return self.P.add(eng, lambda e: e.tensor_tensor(out=out, in0=in0, in1=in1, op=op), reads, writes)

    def ts(self, eng, out, in0, s1, s2, op0, op1, reads, writes):
        if op1 is None:
            return self.P.add(eng, lambda e: e.tensor_scalar(out=out, in0=in0, scalar1=s1, scalar2=None, op0=op0), reads, writes)
        return self.P.add(eng, lambda e: e.tensor_scalar(out=out, in0=in0, scalar1=s1, scalar2=s2, op0=op0, op1=op1), reads, writes)

    def stt(self, out, in0, scalar, in1, op0, op1, reads, writes):
        return self.P.add("dve", lambda e: e.scalar_tensor_tensor(out=out, in0=in0, scalar=scalar, in1=in1, op0=op0, op1=op1), reads, writes)

    def cp(self, eng, out, in_, reads, writes):
        if eng == "act":
            return self.P.add("act", lambda e: e.copy(out=out, in_=in_), reads, writes)
        return self.P.add(eng, lambda e: e.tensor_copy(out=out, in_=in_), reads, writes)

    def mm(self, out, lhsT, rhs, start, stop, reads, writes, skip=False):
        return self.P.add("pe", lambda e: e.matmul(out, lhsT=lhsT, rhs=rhs, start=start, stop=stop, skip_group_check=skip), reads, writes)

    def tr(self, out, in_, ident, reads, writes):
        return self.P.add("pe", lambda e: e.transpose(out=out, in_=in_, identity=ident), reads, writes)

    def recip(self, out, in_, reads, writes):
        return self.P.add("dve", lambda e: e.reciprocal(out=out, in_=in_), reads, writes)

    def memset(self, eng, ap, val, writes):
        return self.P.add(eng, lambda e: e.memset(ap, val), (), writes)


def load_norm_T(K, src, xt, xn, xnT, ss, junk, g_col, idb, bidb, bconst, tr_banks, ntt, rd=()):
    nt = ntt * 128
    K.dma(xt[:, 0:ntt, :], src.rearrange("(tt p) d -> p tt d", p=128), reads=list(rd), writes=[xt.b])
    for tt in range(ntt):
        if junk is None:
            K.act(xn[:, tt, :], xt[:, tt, :], AF.Square, [xt.b], [xn.b, ss.b], accum_out=ss[:, tt:tt + 1])
        else:
            K.act(junk[:], xt[:, tt, :], AF.Square, [xt.b], [junk.b, ss.b], accum_out=ss[:, tt:tt + 1])
    K.ts("pool", ss[:, 8:8 + ntt], ss[:, 0:ntt], 1.0 / D, EPS, ALU.mult, ALU.add, [ss.b], [ss.b])
    K.tt("pool", ss[:, 16:16 + ntt], ss[:, 8:8 + ntt], K.mhalf[:, 0:ntt], ALU.pow, [ss.b, K.mhalf.b], [ss.b])
    for tt in range(ntt):
        K.ts("dve", xn[:, tt, :], xt[:, tt, :], ss[:, 16 + tt:17 + tt], None, ALU.mult, None, [xt.b, ss.b], [xn.b])
    for k in range(8):
        bk = tr_banks[k % len(tr_banks)]
        for tt in range(ntt):
            K.tr(K.psb[bk][:, tt * 128:(tt + 1) * 128], xn[:, tt, k * 128:(k + 1) * 128], idb, [xn.b, bidb], K.bps[bk])
        if k % 2 == 0:
            K.act(xnT[:, k, 0:nt], K.psb[bk][:, 0:nt], AF.Copy, [K.bps[bk][0], bconst], [xnT.b], scale=g_col[:, k:k + 1])
        else:
            K.ts("dve", xnT[:, k, 0:nt], K.psb[bk][:, 0:nt], g_col[:, k:k + 1], None, ALU.mult, None, [K.bps[bk][0], bconst], [xnT.b])


def build(debug=False, phases="TCAP"):
    K = KB(debug)
    nc, P, A = K.nc, K.P, K.A
    x_d = K.din("x", [S, D])
    p_d = K.din("p", [S, 256])
    w_in_d = K.din("w_in", [D, 2560])
    w_out_d = K.din("w_out", [D, D])
    wq_d = K.din("wq", [D, 2048])
    keys_d = K.din("keys", [2, 128, 128])
    wg_d = K.din("wg", [D, D])
    wp_d = K.din("wp", [256, D])
    tab_d = K.din("tab", [128, 128, 2048])
    pk_d = K.din("pk", [128, 160])
    rows_d = K.din("rows", [128, 1408])
    cst_d = K.din("cst", [128, 3, 128])
    rope_d = K.din("rope", [128, 2, S])
    out_d = K.dout("out", [S, D])
    tabb_d = K.dscr("tabb", [128, 128, 2048], BF16)
    h1_d = K.dscr("h1s", [S, D], F32)
    cvT_d = K.dscr("cvTs", [128, 4, S], BF16)
    btab = [Buf("tabb%d" % j) for j in range(128)]
    bh1 = [Buf("h1s%d" % i) for i in range(NT)]
    bcv = [Buf("cvT%d" % i) for i in range(NCH)]
    dbg = {}

    pk = A.alloc([128, 160], F32, "pk")
    rows = A.alloc([128, 384], F32, "rows")
    cst = A.alloc([128, 3, 128], F32, "cst")
    cstb = A.alloc([128, 2, 128], BF16, "cstb")
    onesf = A.alloc([128, 128], F32, "onesf")
    K.dma(pk[:], pk_d, writes=[pk.b])
    K.dma(rows[:], rows_d[:, 1024:1408], writes=[rows.b])
    K.dma(cst[:], cst_d, writes=[cst.b])
    K.cp("dve", cstb[:], cst[:, 0:2, :], [cst.b], [cstb.b])
    K.memset("dve", onesf[:], 1.0 / 512, [onesf.b])
    mhalf = A.alloc([128, 8], F32, "mhalf")
    K.memset("pool", mhalf[:], -0.5, [mhalf.b])
    K.mhalf = mhalf
    idb = cstb[:, 0, :]
    trib = cstb[:, 1, :]
    idf = cst[:, 0, :]
    iota = cst[:, 2, :]
    g_attn = pk[:, 0:8]
    g_ffn = pk[:, 8:16]
    g_ple = pk[:, 16:24]
    conv_w = pk[:, 24:148].rearrange("p (c k) -> p c k", c=4)
    conv_b = pk[:, 148:152]
    ln_g = pk[:, 152:156]
    ln_b = pk[:, 156:160]
    g_fin = None
    subg = rows[:, 0:128]
    lamv = rows[:, 128:384]
    base_mark = A.mark()

    if "T" in phases:
        TG = 8
        for j0 in range(0, 128, TG):
            K.dma(tabb_d[j0:j0 + TG], tab_d[j0:j0 + TG], writes=btab[j0:j0 + TG], q="pool")

    if "C" in phases:
        m0 = A.mark()
        wC = A.alloc([128, 8, 1024], BF16, "wC")
        diag = A.alloc([128, 4, 31, 128], BF16, "diag")
        m1 = A.mark()
        stg = [A.alloc([128, 1024], F32, "stg") for _ in range(2)]
        for k in range(8):
            s_ = stg[k % 2]
            K.dma(s_[:], w_in_d[k * 128:(k + 1) * 128, 1536:2560], writes=[s_.b])
            K.cp("act" if k % 2 == 0 else "dve", wC[:, k, :], s_[:], [s_.b], [wC.b])
        for c in range(4):
            for k in range(31):
                K.ts("pool" if (k % 2) else "dve", diag[:, c, k, :], idf, conv_w[:, c, k:k + 1], None, ALU.mult, None, [cst.b, pk.b], [diag.b])
        A.release(m1)
        xt = A.alloc([128, 4, 1024], F32, "xt")
        xn = A.alloc([128, 4, 1024], BF16, "xn")
        xnT = A.alloc([128, 8, 512], BF16, "xnT")
        ss = A.alloc([128, 24], F32, "ss")
        junk = A.alloc([128, 1024], BF16, "junk")
        u = [A.alloc([128, 4, 542], BF16, "u") for _ in range(3)]
        sig = A.alloc([128, 512], F32, "sig")
        ysb = A.alloc([128, 4, 512], F32, "ysb")
        ysq = A.alloc([128, 4, 512], F32, "ysq")
        mean_sb = A.alloc([128, 512], F32, "mean")
        m2 = A.alloc([128, 512], F32, "m2")
        var = A.alloc([128, 512], F32, "var")
        rstdb = A.alloc([128, 512], F32, "rstdb")
        zt = A.alloc([128, 512], F32, "zt")
        z2 = A.alloc([128, 512], F32, "z2")
        cvo = A.alloc([128, 4, 512], BF16, "cvo")
        K.memset("pool", u[0][:, :, 0:30], 0.0, [u[0].b])
        import os
        NCHC = int(os.environ.get('DBG_NCHC', NCH))

        def c_front(ci):
            uc, un = u[ci % 3], u[(ci + 1) % 3]
            load_norm_T(K, x_d[ci * TC:(ci + 1) * TC, :], xt, xn, xnT, ss, junk, g_attn, idb, cstb.b, pk.b, [0, 1], 4)
            yield
            for c in range(4):
                for k in range(8):
                    K.mm(K.ps[2][:, :], wC[:, k, c * 128:(c + 1) * 128], xnT[:, k, :], k == 0, k == 7, [wC.b, xnT.b], K.bps[2])
                for k in range(8):
                    K.mm(K.ps[3][:, :], wC[:, k, 512 + c * 128:512 + (c + 1) * 128], xnT[:, k, :], k == 0, k == 7, [wC.b, xnT.b], K.bps[3])
                K.act(sig[:], K.ps[3][:, :], AF.Sigmoid, K.bps[3], [sig.b])
                K.tt("dve", uc[:, c, 30:542], K.ps[2][:, :], sig[:], ALU.mult, K.bps[2] + [sig.b], [uc.b])
                yield
            if ci + 1 < NCH:
                K.cp("pool", un[:, :, 0:30], uc[:, :, 512:542], [uc.b], [un.b])
            yield

        def c_back(ci):
            uc = u[ci % 3]
            for c in range(4):
                yb = 4 + c % 2
                for k in range(31):
                    K.mm(K.ps[yb][:, :], diag[:, c, k, :], uc[:, c, k:k + 512], k == 0, k == 30, [diag.b, uc.b], K.bps[yb])
                K.act(ysb[:, c, :], K.ps[yb][:, :], AF.Identity, K.bps[yb] + [pk.b], [ysb.b], bias=conv_b[:, c:c + 1])
                K.act(ysq[:, c, :], K.ps[yb][:, :], AF.Square, K.bps[yb] + [pk.b], [ysq.b], bias=conv_b[:, c:c + 1])
                yield
            for c in range(4):
                K.mm(K.ps[6][:, :], onesf[:], ysb[:, c, :], c == 0, c == 3, [onesf.b, ysb.b], K.bps[6])
            for c in range(4):
                K.mm(K.ps[7][:, :], onesf[:], ysq[:, c, :], c == 0, c == 3, [onesf.b, ysq.b], K.bps[7])
            K.cp("act", mean_sb[:], K.ps[6][:, :], K.bps[6], [mean_sb.b])
            K.tt("dve", m2[:], mean_sb[:], mean_sb[:], ALU.mult, [mean_sb.b], [m2.b])
            K.tt("dve", var[:], K.ps[7][:, :], m2[:], ALU.subtract, K.bps[7] + [m2.b], [var.b])
            K.act(var[:], var[:], AF.Sqrt, [var.b], [var.b], bias=EPS)
            K.recip(rstdb[:], var[:], [var.b], [rstdb.b])
            yield
            for c in range(4):
                K.tt("dve", zt[:], ysb[:, c, :], mean_sb[:], ALU.subtract, [ysb.b, mean_sb.b], [zt.b])
                K.tt("dve", z2[:], zt[:], rstdb[:], ALU.mult, [zt.b, rstdb.b], [z2.b])
                K.act(cvo[:, c, :], z2[:], AF.Silu, [z2.b, pk.b], [cvo.b], scale=ln_g[:, c:c + 1], bias=ln_b[:, c:c + 1])
                yield
            K.dma(cvT_d[:, :, ci * TC:(ci + 1) * TC], cvo[:], reads=[cvo.b], writes=[bcv[ci]])

        def rr(gens):
            live = [g for g in gens if g is not None]
            while live:
                for g_ in list(live):
                    try:
                        next(g_)
                    except StopIteration:
                        live.remove(g_)

        rr([c_front(0)])
        for ci in range(NCHC):
            rr([c_back(ci), c_front(ci + 1) if ci + 1 < NCHC else None])
        A.release(m0)

    K.extra = dict(x_d=x_d, p_d=p_d, out_d=out_d, h1_d=h1_d, cvT_d=cvT_d, tabb_d=tabb_d, btab=btab, bh1=bh1, bcv=bcv,
                   rows_d=rows_d, w_in_d=w_in_d, w_out_d=w_out_d, wq_d=wq_d, keys_d=keys_d, wg_d=wg_d, wp_d=wp_d, rope_d=rope_d,
                   pk=pk, rows=rows, cst=cst, cstb=cstb, idb=idb, trib=trib, idf=idf, iota=iota, g_attn=g_attn,
                   g_ffn=g_ffn, g_ple=g_ple, g_fin=g_fin, subg=subg, lamv=lamv, onesf=onesf)
    if "A" in phases:
        phase_A(K)
    if "P" in phases:
        phase_P(K)
    if debug:
        if "C" in phases and "A" not in phases:
            o = K.dout("dbg_cvT", [128, 4, S], BF16)
            K.dma(o, cvT_d, reads=bcv)
        if "A" in phases and "P" not in phases:
            o = K.dout("dbg_h1", [S, D], F32)
            K.dma(o, h1_d, reads=bh1)
    P.emit()
    return nc


def phase_A(K):
    nc, P, A = K.nc, K.P, K.A
    X = K.extra
    x_d, w_in_d, w_out_d, rope_d, cvT_d, h1_d = X["x_d"], X["w_in_d"], X["w_out_d"], X["rope_d"], X["cvT_d"], X["h1_d"]
    pk, rows, cstb = X["pk"], X["rows"], X["cstb"]
    idb, trib, g_attn, subg, lamv = X["idb"], X["trib"], X["g_attn"], X["subg"], X["lamv"]
    bcv, bh1 = X["bcv"], X["bh1"]
    m0 = A.mark()
    wA = A.alloc([128, 8, 2560], BF16, "wA")
    wo = A.alloc([128, 8, 1024], BF16, "wo")
    kT = A.alloc([128, 4, S], BF16, "kT")
    bkT = kT.sub(NCH)
    Va = A.alloc([128, NT, 4, 130], BF16, "Va")
    bVa = Va.sub(NCH)
    lam = A.alloc([128, 8], F32, "lam")
    sg = A.alloc([128, 128], F32, "sg")
    ltmp = A.alloc([128, 128], F32, "ltmp")
    m1 = A.mark()
    stg = [A.alloc([128, 1536], F32, "stgA") for _ in range(2)]
    for k in range(8):
        s_ = stg[k % 2]
        K.dma(s_[:], w_in_d[k * 128:(k + 1) * 128, 0:1536], writes=[s_.b])
        K.cp("act", wA[:, k, 0:1536], s_[:], [s_.b], [wA.b])
        sv = s_[:, 0:1024].rearrange("p (j d) -> p j d", d=64)
        wr = wA[:, k, 1536:2560].rearrange("p (j d) -> p j d", d=64)
        K.ts("dve", wr[:, :, 0:32], sv[:, :, 32:64], -1.0, None, ALU.mult, None, [s_.b], [wA.b])
        K.cp("pool", wr[:, :, 32:64], sv[:, :, 0:32], [s_.b], [wA.b])
    stg2 = [A.alloc([128, 1024], F32, "stgO") for _ in range(2)]
    for k in range(8):
        s_ = stg2[k % 2]
        K.dma(s_[:], w_out_d[k * 128:(k + 1) * 128, :], writes=[s_.b])
        K.cp("act" if k % 2 else "dve", wo[:, k, :], s_[:], [s_.b], [wo.b])
    A.release(m1)
    K.tt("dve", ltmp[:, 0:64], lamv[:, 0:64], lamv[:, 64:128], ALU.mult, [rows.b], [ltmp.b])
    K.tt("dve", ltmp[:, 64:128], lamv[:, 128:192], lamv[:, 192:256], ALU.mult, [rows.b], [ltmp.b])
    K.P.add("dve", lambda e: e.tensor_reduce(out=lam[:, 0:2], in_=ltmp[:].rearrange("p (a b) -> p a b", a=2), axis=AX.X, op=ALU.add), [ltmp.b], [lam.b])
    K.act(lam[:, 2:4], lam[:, 0:2], AF.Exp, [lam.b], [lam.b])
    K.tt("dve", lam[:, 4:5], lam[:, 2:3], lam[:, 3:4], ALU.subtract, [lam.b], [lam.b])
    K.ts("dve", lam[:, 5:6], lam[:, 4:5], LAMBDA_INIT, -1.0, ALU.add, ALU.mult, [lam.b], [lam.b])
    K.ts("dve", sg[:], subg, 1.0 - LAMBDA_INIT, None, ALU.mult, None, [rows.b], [sg.b])
    neglam = lam[:, 5:6]
    K.memset("pool", Va[:], 1.0, bVa)

    import os
    LVL = int(os.environ.get('DBG_STOP', 9))
    xt = A.alloc([128, 4, 1024], F32, "xt")
    xn = A.alloc([128, 4, 1024], BF16, "xn")
    xnT = A.alloc([128, 8, 512], BF16, "xnT")
    ss = A.alloc([128, 24], F32, "ss")
    rp = [A.alloc([128, 2, 512], F32, "rp") for _ in range(1)]
    t1 = [A.alloc([128, 512], F32, "t1") for _ in range(1)]
    t2 = [A.alloc([128, 512], F32, "t2") for _ in range(1)]
    qTz = [A.alloc([128, 4, 512], BF16, "qTz") for _ in range(2)]
    bqT = [qTz[0].sub(4), qTz[1].sub(4)]
    K.memset("pool", qTz[0][64:128, :, :], 0.0, bqT[0])
    K.memset("pool", qTz[1][0:64, :, :], 0.0, bqT[1])
    PT = [A.alloc([128, 512], BF16, "PT") for _ in range(4)]
    att = A.alloc([128, 128], F32, "att")
    a1 = A.alloc([128, 128], F32, "a1")
    sm = A.alloc([128, 4, 8], F32, "sm")
    Osb = A.alloc([128, 8, 129], F32, "Osb")
    attn_sb = A.alloc([128, 4, 512], BF16, "attn_sb")
    catT = A.alloc([128, 8, 512], BF16, "catT")
    h1o = [A.alloc([128, 1024], F32, "h1o") for _ in range(2)]
    cnt = 0
    tcnt = 0
    import os
    for ci in range(int(os.environ.get('DBG_NCH', NCH))):
        if LVL < 2:
            break
        load_norm_T(K, x_d[ci * TC:(ci + 1) * TC, :], xt, xn, xnT, ss, None, g_attn, idb, cstb.b, pk.b, [0, 1], 4)
        rpc = rp[0]
        K.dma(rpc[:], rope_d[:, :, ci * TC:(ci + 1) * TC], writes=[rpc.b])
        K.dma(catT[:, 4:8, :], cvT_d[:, :, ci * TC:(ci + 1) * TC], reads=[bcv[ci]], writes=[catT.b])
        cos, sin = rpc[:, 0, :], rpc[:, 1, :]
        for h in range(4):
            for which in range(2):
                c0 = which * 512 + h * 128
                for k in range(8):
                    K.mm(K.ps[0][:, :], wA[:, k, c0:c0 + 128], xnT[:, k, :], k == 0, k == 7, [wA.b, xnT.b], K.bps[0])
                for k in range(8):
                    K.mm(K.ps[1][:, :], wA[:, k, 1536 + c0:1536 + c0 + 128], xnT[:, k, :], k == 0, k == 7, [wA.b, xnT.b], K.bps[1])
                ta, tb = t1[0], t2[0]
                tcnt += 1
                K.tt("dve", ta[:], K.ps[0][:, :], cos, ALU.mult, K.bps[0] + [rpc.b], [ta.b])
                K.tt("dve", tb[:], K.ps[1][:, :], sin, ALU.mult, K.bps[1] + [rpc.b], [tb.b])
                if which == 0:
                    K.tt("pool", qTz[0][0:64, h, :], ta[0:64, :], tb[0:64, :], ALU.add, [ta.b, tb.b], [bqT[0][h]])
                    K.tt("pool", qTz[1][64:128, h, :], ta[64:128, :], tb[64:128, :], ALU.add, [ta.b, tb.b], [bqT[1][h]])
                else:
                    K.tt("pool", kT[:, h, ci * TC:(ci + 1) * TC], ta[:], tb[:], ALU.add, [ta.b, tb.b], [bkT[ci]])
        if LVL < 3:
            continue
        for tt_ in range(4):
            bk = tt_ % 2
            for k in range(8):
                K.mm(K.ps[bk][:, :], xnT[:, k, tt_ * 128:(tt_ + 1) * 128], wA[:, k, 1024:1536], k == 0, k == 7, [wA.b, xnT.b], K.bps[bk])
            K.P.add("act", lambda e, bk=bk, tt_=tt_, ci=ci: e.copy(out=Va[:, ci * 4 + tt_, :, 0:128], in_=K.ps[bk][:, :].rearrange("p (h d) -> p h d", d=128)),
                    K.bps[bk], [bVa[ci]])
        if LVL < 4:
            continue
        items = [(h, c, kt) for h in range(4) for c in range(2) for kt in range(4 * ci + 4)]
        DP = 2

        def emit_qk(h, c, kt, idx):
            r = kt - 4 * ci
            c0 = max(r, 0) * 128
            sbk = idx % 4
            pt = PT[idx % 4]
            K.mm(K.ps[sbk][:, c0:512], kT[:, h, kt * 128:(kt + 1) * 128], qTz[c][:, h, c0:512],
                 True, True, [bkT[kt // 4], bqT[c][h]], K.bps[sbk])
            K.act(pt[:, c0:512], K.ps[sbk][:, c0:512], AF.Exp, K.bps[sbk], [pt.b], scale=0.125)
            if r >= 0:
                K.tt("pool", pt[:, c0:c0 + 128], pt[:, c0:c0 + 128], trib, ALU.mult, [pt.b, cstb.b], [pt.b])

        def emit_pv(h, c, kt, idx):
            r = kt - 4 * ci
            pt = PT[idx % 4]
            for tqi in range(max(r, 0), 4):
                a = c * 4 + tqi
                bank, col = 4 + a // 2, (a % 2) * 256
                K.mm(K.ps[bank][:, col:col + 129], pt[:, tqi * 128:(tqi + 1) * 128], Va[:, kt, h, 0:129],
                     kt == 0 and a % 2 == 0, kt == 4 * ci + tqi, [pt.b, bVa[kt // 4]], K.bps[bank], skip=True)
            if c == 1 and kt == 4 * ci + 3:
                post(h)

        def post(h):
            if LVL >= 5:
                for bi in range(4):
                    src = K.ps[4 + bi][:, :].rearrange("p (a b) -> p a b", b=256)[:, :, 0:129]
                    K.cp("act" if bi % 2 == 0 else "dve", Osb[:, 2 * bi:2 * bi + 2, :], src, K.bps[4 + bi], [Osb.b])
            for tqi in range(4):
                if LVL < 5:
                    continue
                O1 = Osb[:, tqi, :]
                O2 = Osb[:, 4 + tqi, :]
                b1 = [Osb.b]
                b2 = [Osb.b]
                SUB = int(os.environ.get("DBG_SUB", 9))
                K.recip(sm[:, tqi, 0:1], O1[:, 128:129], b1, [sm.b])
                K.recip(sm[:, tqi, 1:2], O2[:, 128:129], b2, [sm.b])
                if SUB < 2:
                    continue
                K.tt("dve", sm[:, tqi, 2:3], sm[:, tqi, 1:2], neglam, ALU.mult, [sm.b, lam.b], [sm.b])
                K.act(a1[:], O1[:, 0:128], AF.Copy, b1 + [sm.b], [a1.b], scale=sm[:, tqi, 0:1])
                if SUB < 3:
                    continue
                K.stt(att[:], O2[:, 0:128], sm[:, tqi, 2:3], a1[:], ALU.mult, ALU.add, b2 + [sm.b, a1.b], [att.b])
                if SUB < 4:
                    continue
                K.act(a1[:], att[:], AF.Square, [att.b], [a1.b, sm.b], accum_out=sm[:, tqi, 3:4])
                if SUB < 5:
                    continue
                K.ts("pool", sm[:, tqi, 4:5], sm[:, tqi, 3:4], 1.0 / 128, EPS, ALU.mult, ALU.add, [sm.b], [sm.b])
                K.tt("pool", sm[:, tqi, 5:6], sm[:, tqi, 4:5], K.mhalf[:, 0:1], ALU.pow, [sm.b, K.mhalf.b], [sm.b])
                if SUB < 6:
                    continue
                K.stt(attn_sb[:, tqi, h * 128:(h + 1) * 128], att[:], sm[:, tqi, 5:6], sg[:], ALU.mult, ALU.mult, [att.b, sm.b, sg.b], [attn_sb.b])
        for idx in range(len(items) + DP):
            if idx < len(items):
                emit_qk(*items[idx], cnt + idx)
            if idx >= DP:
                emit_pv(*items[idx - DP], cnt + idx - DP)
        cnt += len(items)
        if LVL < 6:
            continue
        for tqi in range(4):
            bk = tqi % 2
            for h in range(4):
                K.tr(K.psb[bk][:, h * 128:(h + 1) * 128], attn_sb[:, tqi, h * 128:(h + 1) * 128], idb, [attn_sb.b, cstb.b], K.bps[bk])
            if os.environ.get("DBG_V", "0") == "1":
                continue
            if os.environ.get("DBG_V", "0") == "2":
                for h in range(4):
                    K.cp("dve", catT[:, h, tqi * 128:(tqi + 1) * 128], K.psb[bk][:, h * 128:(h + 1) * 128], K.bps[bk], [catT.b])
                continue
            K.cp("dve", catT[:, 0:4, tqi * 128:(tqi + 1) * 128], K.psb[bk][:, 0:512].rearrange("p (h d) -> p h d", d=128), K.bps[bk], [catT.b])
        for tqi in range(4):
            if LVL < 7:
                continue
            ho = h1o[tqi % 2]
            for half in range(2):
                for k in range(8):
                    K.mm(K.ps[half][:, :], catT[:, k, tqi * 128:(tqi + 1) * 128], wo[:, k, half * 512:(half + 1) * 512], k == 0, k == 7, [catT.b, wo.b], K.bps[half])
                K.tt("dve", ho[:, half * 512:(half + 1) * 512], K.ps[half][:, :], xt[:, tqi, half * 512:(half + 1) * 512], ALU.add, K.bps[half] + [xt.b], [ho.b])
            ti = ci * 4 + tqi
            if LVL < 8:
                continue
            K.dma(h1_d[ti * 128:(ti + 1) * 128, :], ho[:], reads=[ho.b], writes=[bh1[ti]])
    A.release(m0)


def phase_P(K):
    import os
    nc, P, A = K.nc, K.P, K.A
    X = K.extra
    h1_d, out_d, p_d, wq_d, keys_d, wg_d, wp_d, tabb_d = X["h1_d"], X["out_d"], X["p_d"], X["wq_d"], X["keys_d"], X["wg_d"], X["wp_d"], X["tabb_d"]
    pk, rows, cst, cstb = X["pk"], X["rows"], X["cst"], X["cstb"]
    idb, idf, iota, g_ffn, g_ple, g_fin = X["idb"], X["idf"], X["iota"], X["g_ffn"], X["g_ple"], X["g_fin"]
    bh1, btab = X["bh1"], X["btab"]
    mT_d = K.dscr("mTs", [128, 8, S], BF16)
    pk_s = K.dscr("picks", [S, 3, 128], F32)
    bmT = [Buf("mTs%d" % i) for i in range(NPC)]
    bpk = [Buf("pks%d" % i) for i in range(NPC)]
    NPCR = int(os.environ.get("DBG_NPC", NPC))
    SUBP = os.environ.get("DBG_SUBP", "abc")

    sc_d = K.dscr("scs", [S, 2048], F32)
    bscd = [Buf("scs%d" % i) for i in range(NT)]
    bpkt = [Buf("pkt%d" % i) for i in range(NT)]
    NTR = NPCR * 2
    K.tk_done = 0
    B4 = [128, 8, 16, 16]
    iota16 = iota[:, 0:16].unsqueeze(1).unsqueeze(1).to_broadcast(B4)

    def alloc_topk_ws():
        w = dict(sc=A.alloc([128, 16, 128], F32, "sc"), sc2=A.alloc([128, 16, 128], F32, "sc2"),
                 sv=A.alloc([128, 16, 16], F32, "sv"), si=A.alloc([128, 16, 16], U32, "si"), sif=A.alloc([128, 16, 16], F32, "sif"),
                 cs=A.alloc([128, 8, 16], F32, "cs"), ci=A.alloc([128, 8, 16], U32, "ci"),
                 abu=A.alloc([128, 2, 8, 16], U32, "abu"), abf=A.alloc([128, 2, 8, 16], F32, "abf"),
                 pkt=A.alloc([128, 3, 128], F32, "pkt"),
                 ex=A.alloc([128, 8, 16], F32, "ex"), rs=A.alloc([128, 16], F32, "rs"))
        w["bsc"] = w["sc"].sub(4)
        w["bsv"] = w["sv"].sub(16)
        w["bsi"] = w["si"].sub(16)
        w["bsc2"] = w["sc2"].sub(16)
        w["bcs"] = w["cs"].sub(8)
        w["bci"] = w["ci"].sub(8)
        return w

    def topk_steps(ti, w):
        sc, sc2, sv, si, sif, cs, ci = w["sc"], w["sc2"], w["sv"], w["si"], w["sif"], w["cs"], w["ci"]
        abu, abf, pkt, ex, rs = w["abu"], w["abf"], w["pkt"], w["ex"], w["rs"]
        bsc, bsv, bsi, bsc2, bcs, bci = w["bsc"], w["bsv"], w["bsi"], w["bsc2"], w["bcs"], w["bci"]
        cand = sc[:].rearrange("p (h c) n -> p h (c n)", c=2)
        cand2 = sc2[:].rearrange("p (h c) n -> p h (c n)", c=2)
        eq = sc[:].rearrange("p (h c) (a b) -> p h (c a) b", c=2, b=16)
        svv = sv[:].rearrange("p (h c) k -> p h c k", c=2)
        sifv = sif[:].rearrange("p (h c) k -> p h c k", c=2)
        K.dma(sc[:].rearrange("p g n -> p (g n)"), sc_d[ti * 128:(ti + 1) * 128, :], reads=[bscd[ti]], writes=bsc)
        yield
        for g in range(16):
            K.P.add("dve", lambda e, g=g: e.max(out=sv[:, g, 0:8], in_=sc[:, g, :]), [bsc[g // 4]], [bsv[g]])
            if g % 3 == 2:
                yield
        yield
        for g in range(16):
            K.P.add("dve", lambda e, g=g: e.max_index(out=si[:, g, 0:8], in_max=sv[:, g, 0:8], in_values=sc[:, g, :]), [bsc[g // 4], bsv[g]], [bsi[g]])
            if g % 3 == 2:
                yield
        yield
        for g in range(16):
            K.P.add("dve", lambda e, g=g: e.match_replace(out=sc2[:, g, :], in_to_replace=sv[:, g, 0:8], in_values=sc[:, g, :], imm_value=-1e30), [bsc[g // 4], bsv[g]], [bsc2[g]])
            if g % 3 == 2:
                yield
        yield
        for g in range(16):
            K.P.add("dve", lambda e, g=g: e.max(out=sv[:, g, 8:16], in_=sc2[:, g, :]), [bsc2[g]], [bsv[g]])
            if g % 3 == 2:
                yield
        yield
        for g in range(16):
            K.P.add("dve", lambda e, g=g: e.max_index(out=si[:, g, 8:16], in_max=sv[:, g, 8:16], in_values=sc2[:, g, :]), [bsc2[g], bsv[g]], [bsi[g]])
            if g % 3 == 2:
                yield
        yield
        K.cp("dve", sif[:], si[:], bsi, [sif.b])
        K.tt("dve", cand.rearrange("p h (a b) -> p h a b", b=16), svv[:, :, 0, :].unsqueeze(3).to_broadcast(B4),
             svv[:, :, 1, :].unsqueeze(2).to_broadcast(B4), ALU.add, bsv, bsc)
        yield
        for h in range(8):
            K.P.add("dve", lambda e, h=h: e.max(out=cs[:, h, 0:8], in_=cand[:, h, :]), [bsc[h // 2]], [bcs[h]])
            if h % 3 == 2:
                yield
        yield
        for h in range(8):
            K.P.add("dve", lambda e, h=h: e.max_index(out=ci[:, h, 0:8], in_max=cs[:, h, 0:8], in_values=cand[:, h, :]), [bsc[h // 2], bcs[h]], [bci[h]])
            if h % 3 == 2:
                yield
        for h in range(8):
            K.P.add("dve", lambda e, h=h: e.match_replace(out=cand2[:, h, :], in_to_replace=cs[:, h, 0:8], in_values=cand[:, h, :], imm_value=-1e30),
                    [bsc[h // 2], bcs[h]], [bsc2[2 * h], bsc2[2 * h + 1]])
            if h % 3 == 2:
                yield
        yield
        for h in range(8):
            K.P.add("dve", lambda e, h=h: e.max(out=cs[:, h, 8:16], in_=cand2[:, h, :]), [bsc2[2 * h], bsc2[2 * h + 1]], [bcs[h]])
            if h % 3 == 2:
                yield
        yield
        for h in range(8):
            K.P.add("dve", lambda e, h=h: e.max_index(out=ci[:, h, 8:16], in_max=cs[:, h, 8:16], in_values=cand2[:, h, :]), [bsc2[2 * h], bsc2[2 * h + 1], bcs[h]], [bci[h]])
            if h % 3 == 2:
                yield
        yield
        K.P.add("dve", lambda e: e.tensor_single_scalar(out=abu[:, 0, :, :], in_=ci[:], scalar=4, op=ALU.logical_shift_right), bci, [abu.b])
        K.P.add("dve", lambda e: e.tensor_single_scalar(out=abu[:, 1, :, :], in_=ci[:], scalar=15, op=ALU.bitwise_and), bci, [abu.b])
        K.cp("dve", abf[:], abu[:], [abu.b], [abf.b])
        K.tt("dve", ex[:], cs[:], cs[:, :, 0:1].to_broadcast([128, 8, 16]), ALU.subtract, bcs, [ex.b])
        yield
        for a in range(2):
            K.tt("dve", eq, iota16, abf[:, a, :, :].unsqueeze(3).to_broadcast(B4), ALU.is_equal, [cst.b, abf.b], bsc)
            yield
            K.tt("dve", eq, eq, sifv[:, :, a, :].unsqueeze(2).to_broadcast(B4), ALU.mult, bsc + [sif.b], bsc)
            yield
            if a == 0:
                K.act(ex[:], ex[:], AF.Exp, [ex.b], [ex.b])
            K.P.add("dve", lambda e, a=a: e.tensor_reduce(out=pkt[:, a, :].rearrange("p (h k) -> p h k", k=16), in_=eq, axis=AX.X, op=ALU.add), bsc, [pkt.b])
            yield
        K.P.add("dve", lambda e: e.tensor_reduce(out=rs[:, 0:8], in_=ex[:], axis=AX.X, op=ALU.add), [ex.b], [rs.b])
        K.recip(rs[:, 8:16], rs[:, 0:8], [rs.b], [rs.b])
        K.tt("dve", pkt[:, 2, :].rearrange("p (h k) -> p h k", k=16), ex[:], rs[:, 8:16].unsqueeze(2).to_broadcast([128, 8, 16]), ALU.mult,
             [ex.b, rs.b], [pkt.b])
        K.dma(pk_s[ti * 128:(ti + 1) * 128], pkt[:], reads=[pkt.b], writes=[bpkt[ti]])
        yield

    def drain(g_):
        if g_ is not None:
            for _ in g_:
                pass

    def chain(*gs):
        for g_ in gs:
            yield from g_

    if "a" in SUBP:
        m0 = A.mark()
        wqb = A.alloc([128, 8, 2048], BF16, "wqb")
        keysT = A.alloc([128, 2, 128], BF16, "keysT")
        m1 = A.mark()
        stg = [A.alloc([128, 2048], F32, "stgq") for _ in range(2)]
        for k in range(8):
            s_ = stg[k % 2]
            K.dma(s_[:], wq_d[k * 128:(k + 1) * 128, :], writes=[s_.b])
            K.cp("act" if k % 2 else "dve", wqb[:, k, :], s_[:], [s_.b], [wqb.b])
        kst = A.alloc([128, 2, 128], F32, "kst")
        K.dma(kst[:], keys_d.rearrange("c n d -> n c d"), writes=[kst.b])
        for c in range(2):
            K.tr(K.ps[0][:, c * 128:(c + 1) * 128], kst[:, c, :], idf, [kst.b, cst.b], K.bps[0])
        K.cp("act", keysT[:], K.ps[0][:, 0:256].rearrange("p (c n) -> p c n", c=2), K.bps[0], [keysT.b])
        A.release(m1)
        ss = A.alloc([128, 24], F32, "ss")
        junk = A.alloc([128, 1024], BF16, "junk")
        CH = []
        for _ in range(2):
            CH.append(dict(ht=A.alloc([128, 2, 1024], F32, "ht"), hn=A.alloc([128, 2, 1024], BF16, "hn"),
                           mT=A.alloc([128, 8, PC], BF16, "mT"), qpT=A.alloc([128, 16, PC], BF16, "qpT")))
        scb = [A.alloc([128, 16, 128], F32, "scb") for _ in range(2)]

        def chunk_front(pc):
            c_ = CH[pc % 2]
            ht, hn, mT, qpT = c_["ht"], c_["hn"], c_["mT"], c_["qpT"]
            load_norm_T(K, h1_d[pc * PC:(pc + 1) * PC, :], ht, hn, mT, ss, junk, g_ffn, idb, cstb.b, pk.b, [0, 1], 2,
                        rd=bh1[pc * 2:pc * 2 + 2])
            K.dma(mT_d[:, :, pc * PC:(pc + 1) * PC], mT[:], reads=[mT.b], writes=[bmT[pc]])
            yield
            for g in range(16):
                bk = 2 + g % 2
                for k in range(8):
                    K.mm(K.ps[bk][:, 0:PC], wqb[:, k, g * 128:(g + 1) * 128], mT[:, k, :], k == 0, k == 7, [wqb.b, mT.b], K.bps[bk])
                K.cp("act" if g % 2 else "dve", qpT[:, g, :], K.ps[bk][:, 0:PC], K.bps[bk], [qpT.b])
                if g % 4 == 3:
                    yield

        def chunk_scores(pc):
            qpT = CH[pc % 2]["qpT"]
            for tt_ in range(2):
                sct = scb[tt_]
                for g in range(16):
                    bk = 4 + g // 4
                    K.mm(K.ps[bk][:, (g % 4) * 128:(g % 4 + 1) * 128], qpT[:, g, tt_ * 128:(tt_ + 1) * 128], keysT[:, g % 2, :], True, True,
                         [qpT.b, keysT.b], K.bps[bk], skip=True)
                yield
                for q4 in range(4):
                    K.cp("act" if q4 % 2 else "dve", sct[:, q4 * 4:q4 * 4 + 4, :], K.ps[4 + q4][:, :].rearrange("p (g n) -> p g n", n=128), K.bps[4 + q4], [sct.b])
                ti = pc * 2 + tt_
                K.dma(sc_d[ti * 128:(ti + 1) * 128, :], sct[:].rearrange("p g n -> p (g n)"), reads=[sct.b], writes=[bscd[ti]])
                yield

        def rr3(gens):
            live = [g for g in gens if g is not None]
            while live:
                for g_ in list(live):
                    try:
                        next(g_)
                    except StopIteration:
                        live.remove(g_)

        NEARLY = min(4, NTR)
        WSa = alloc_topk_ws()

        def early_topk():
            inner = chain(*[topk_steps(ti, WSa) for ti in range(NEARLY)])
            while True:
                for _ in range(3):
                    try:
                        next(inner)
                    except StopIteration:
                        return
                yield

        etk = None
        rr3([chunk_front(0)])
        for pc in range(NPCR):
            if pc == 2:
                etk = early_topk()
            rr3([chunk_scores(pc), chunk_front(pc + 1) if pc + 1 < NPCR else None])
            if etk is not None:
                for _ in range(6):
                    next(etk, None)
        if NPCR <= 2:
            etk = early_topk()
        drain(etk)
        K.tk_done = NEARLY
        A.release(m0)

    if "b" in SUBP:
        m0 = A.mark()
        NB = 6
        G = [A.alloc([128, PC, 128], BF16, "G") for _ in range(2)]
        wt = [A.alloc([128, 2048], BF16, "wt") for _ in range(NB)]
        mTb = [A.alloc([128, 8, PC], BF16, "mTb") for _ in range(1)]
        pks = [A.alloc([128, 3, PC], F32, "pks") for _ in range(2)]
        npk = [A.alloc([128, 2, PC], F32, "npk") for _ in range(2)]
        ht = A.alloc([128, 512], F32, "htb")
        Ast = [A.alloc([128, 4, 128], BF16, "Ast") for _ in range(2)]
        Bst = [A.alloc([128, 4, 128], BF16, "Bst") for _ in range(2)]
        tmpa = [A.alloc([128, 128], F32, "tmpa") for _ in range(1)]
        ge = [A.alloc([128, PC], BF16, "ge") for _ in range(3)]
        Hs = [A.alloc([128, PC], BF16, "Hs") for _ in range(4)]
        WS = alloc_topk_ws()
        pkl = A.alloc([128, 2, 3, 128], F32, "pkl")
        iotaB = iota.unsqueeze(1).to_broadcast([128, 4, 128])

        def gbuild(pc):
            Gc, pk_, nk = G[pc % 2], pks[pc % 2], npk[pc % 2]
            K.dma(pkl[:], pk_s[pc * PC:(pc + 1) * PC].rearrange("(tt p) a k -> p tt a k", p=128), reads=bpkt[2 * pc:2 * pc + 2], writes=[pkl.b])
            yield
            for tt_ in range(2):
                for a in range(3):
                    K.tr(K.ps[7][:, a * 128:(a + 1) * 128], pkl[:, tt_, a, :], idf, [pkl.b, cst.b], K.bps[7])
                yield
                K.cp("act", pk_[:, :, tt_ * 128:(tt_ + 1) * 128], K.ps[7][:, 0:384].rearrange("p (a t) -> p a t", a=3), K.bps[7], [pk_.b])
                yield
            K.ts("dve", nk[:, 0, :], pk_[:, 0, :], -1.0, None, ALU.mult, None, [pk_.b], [nk.b])
            K.ts("dve", nk[:, 1, :], pk_[:, 2, :], -1.0, None, ALU.mult, None, [pk_.b], [nk.b])
            yield
            for grp in range(PC // 4 + 1):
                if grp < PC // 4:
                    As, Bs = Ast[grp % 2], Bst[grp % 2]
                    for tl in range(4):
                        t = grp * 4 + tl
                        if tl % 2 == 0:
                            K.ts("dve", As[:, tl, :], iota, pk_[:, 0, t:t + 1], pk_[:, 2, t:t + 1], ALU.is_equal, ALU.mult, [cst.b, pk_.b], [As.b])
                        else:
                            ta = tmpa[0]
                            K.act(ta[:], iota, AF.Abs, [cst.b, nk.b], [ta.b], bias=nk[:, 0, t:t + 1])
                            K.act(As[:, tl, :], ta[:], AF.Relu, [ta.b, nk.b, pk_.b], [As.b], scale=nk[:, 1, t:t + 1], bias=pk_[:, 2, t:t + 1])
                    K.tt("dve", Bs[:], iotaB, pk_[:, 1, grp * 4:grp * 4 + 4].unsqueeze(2).to_broadcast([128, 4, 128]), ALU.is_equal, [cst.b, pk_.b], [Bs.b])
                if grp >= 1:
                    g1 = grp - 1
                    As, Bs = Ast[g1 % 2], Bst[g1 % 2]
                    for tl in range(4):
                        K.mm(K.ps[7][:, tl * 128:(tl + 1) * 128], As[:, tl, :], Bs[:, tl, :], True, True, [As.b, Bs.b], K.bps[7], skip=True)
                    K.cp("act", Gc[:, g1 * 4:g1 * 4 + 4, :], K.ps[7][:, :].rearrange("p (t j) -> p t j", j=128), K.bps[7], [Gc.b])
                yield

        def dense(pc, nxt, nxt2):
            Gc, mT = G[pc % 2], mTb[0]
            K.dma(mT[:], mT_d[:, :, pc * PC:(pc + 1) * PC], reads=[bmT[pc]], writes=[mT.b])
            for j in range(130):
                if j < 128:
                    w_ = wt[j % NB]
                    K.dma(w_[:], tabb_d[j], reads=[btab[j]], writes=[w_.b])
                    ab = 4 + j % 3
                    for k in range(8):
                        K.mm(K.ps[ab][:, 0:PC], w_[:, k * 128:(k + 1) * 128], mT[:, k, :], k == 0, k == 7, [w_.b, mT.b], K.bps[ab])
                    g_, h_ = ge[j % 3], Hs[j % 4]
                    K.act(g_[:], K.ps[ab][:, 0:PC], AF.Gelu, K.bps[ab], [g_.b])
                    K.tt("pool", h_[:], g_[:], Gc[:, :, j], ALU.mult, [g_.b, Gc.b], [h_.b])
                if j >= 2:
                    jj = j - 2
                    w_, h_ = wt[jj % NB], Hs[jj % 4]
                    for tt_ in range(2):
                        for half in range(2):
                            yb = tt_ * 2 + half
                            K.mm(K.ps[yb][:, :], h_[:, tt_ * 128:(tt_ + 1) * 128], w_[:, 1024 + half * 512:1024 + (half + 1) * 512], jj == 0, jj == 127,
                                 [h_.b, w_.b], K.bps[yb])
                if nxt is not None and j % 2 == 1:
                    next(nxt, None)
                if nxt2 is not None:
                    next(nxt2, None)
            for tt_ in range(2):
                ti = pc * 2 + tt_
                for half in range(2):
                    yb = tt_ * 2 + half
                    hsl = h1_d[ti * 128:(ti + 1) * 128, half * 512:(half + 1) * 512]
                    K.dma(ht[:], hsl, reads=[bh1[ti]], writes=[ht.b])
                    K.tt("dve", ht[:], K.ps[yb][:, :], ht[:], ALU.add, K.bps[yb] + [ht.b], [ht.b])
                    K.dma(hsl, ht[:], reads=[ht.b], writes=[bh1[ti]])

        def topk_chunk(pc):
            if pc >= NPCR:
                return None
            tiles = [ti for ti in (2 * pc, 2 * pc + 1) if ti >= K.tk_done]
            if not tiles:
                return None
            return chain(*[topk_steps(ti, WS) for ti in tiles])

        drain(topk_chunk(0))
        drain(topk_chunk(1))
        drain(gbuild(0))
        for pc in range(NPCR):
            nxt = gbuild(pc + 1) if pc + 1 < NPCR else None
            nxt2 = topk_chunk(pc + 2)
            dense(pc, nxt, nxt2)
            drain(nxt2)
            drain(nxt)
        A.release(m0)

    if "c" in SUBP:
        m0 = A.mark()
        wgb = A.alloc([128, 8, 1024], BF16, "wgb")
        wpb = A.alloc([128, 2, 1024], BF16, "wpb")
        m1 = A.mark()
        stg = [A.alloc([128, 1024], F32, "stgg") for _ in range(2)]
        for k in range(8):
            s_ = stg[k % 2]
            K.dma(s_[:], wg_d[k * 128:(k + 1) * 128, :], writes=[s_.b])
            K.cp("act" if k % 2 else "dve", wgb[:, k, :], s_[:], [s_.b], [wgb.b])
        for k in range(2):
            s_ = stg[k % 2]
            K.dma(s_[:], wp_d[k * 128:(k + 1) * 128, :], writes=[s_.b])
            K.cp("act" if k % 2 else "dve", wpb[:, k, :], s_[:], [s_.b], [wpb.b])
        A.release(m1)
        ss = A.alloc([128, 24], F32, "ssc")
        junk = A.alloc([128, 1024], BF16, "junkc")
        CHc = []
        for _ in range(2):
            CHc.append(dict(ht=A.alloc([128, 2, 1024], F32, "htc"), hn=A.alloc([128, 2, 1024], BF16, "hnc"), mT=A.alloc([128, 8, PC], BF16, "mTc"),
                            pt=A.alloc([128, 2, 256], F32, "pt"), pb=A.alloc([128, 2, 256], BF16, "pb"), pT=A.alloc([128, 2, PC], BF16, "pT")))
        gate = [A.alloc([128, 512], F32, "gate") for _ in range(4)]
        tmp = [A.alloc([128, 512], F32, "tmp") for _ in range(4)]
        ot = [A.alloc([128, 1024], F32, "ot") for _ in range(2)]
        s3 = A.alloc([128, 8], F32, "s3")
        gfin = A.alloc([128, 1024], F32, "gfin")
        K.dma(gfin[:], X["rows_d"][:, 0:1024], writes=[gfin.b])

        def pc_front(pc):
            c_ = CHc[pc % 2]
            ht, hn, mT, pt_, pb_, pT = c_["ht"], c_["hn"], c_["mT"], c_["pt"], c_["pb"], c_["pT"]
            load_norm_T(K, h1_d[pc * PC:(pc + 1) * PC, :], ht, hn, mT, ss, junk, g_ple, idb, cstb.b, pk.b, [0, 1], 2, rd=bh1[pc * 2:pc * 2 + 2])
            yield
            K.dma(pt_[:], p_d[pc * PC:(pc + 1) * PC, :].rearrange("(tt p) d -> p tt d", p=128), writes=[pt_.b])
            K.cp("pool", pb_[:], pt_[:], [pt_.b], [pb_.b])
            for kk in range(2):
                for tt_ in range(2):
                    K.tr(K.psb[2][:, kk * PC + tt_ * 128:kk * PC + (tt_ + 1) * 128], pb_[:, tt_, kk * 128:(kk + 1) * 128], idb, [pb_.b, cstb.b], K.bps[2])
            K.cp("act", pT[:], K.psb[2][:, 0:2 * PC].rearrange("p (k t) -> p k t", k=2), K.bps[2], [pT.b])
            yield

        def pc_back(pc):
            c_ = CHc[pc % 2]
            ht, mT, pT = c_["ht"], c_["mT"], c_["pT"]
            for tt_ in range(2):
                for half in range(2):
                    gbk, ebk = 4 + half, 6 + half
                    gt_, tm_ = gate[tt_ * 2 + half], tmp[tt_ * 2 + half]
                    for k in range(8):
                        K.mm(K.ps[gbk][:, :], mT[:, k, tt_ * 128:(tt_ + 1) * 128], wgb[:, k, half * 512:(half + 1) * 512], k == 0, k == 7, [mT.b, wgb.b], K.bps[gbk])
                    K.act(gt_[:], K.ps[gbk][:, :], AF.Sigmoid, K.bps[gbk], [gt_.b])
                    for kk in range(2):
                        K.mm(K.ps[ebk][:, :], pT[:, kk, tt_ * 128:(tt_ + 1) * 128], wpb[:, kk, half * 512:(half + 1) * 512], kk == 0, kk == 1, [pT.b, wpb.b], K.bps[ebk])
                    K.tt("dve", tm_[:], K.ps[ebk][:, :], gt_[:], ALU.mult, K.bps[ebk] + [gt_.b], [tm_.b])
                    K.tt("dve", ht[:, tt_, half * 512:(half + 1) * 512], tm_[:], ht[:, tt_, half * 512:(half + 1) * 512], ALU.add, [tm_.b, ht.b], [ht.b])
                    yield
                o_ = ot[tt_]
                K.act(junk[:], ht[:, tt_, :], AF.Square, [ht.b], [junk.b, s3.b], accum_out=s3[:, 0:1])
                K.ts("pool", s3[:, 1:2], s3[:, 0:1], 1.0 / D, EPS, ALU.mult, ALU.add, [s3.b], [s3.b])
                K.tt("pool", s3[:, 2:3], s3[:, 1:2], K.mhalf[:, 0:1], ALU.pow, [s3.b, K.mhalf.b], [s3.b])
                K.stt(o_[:], ht[:, tt_, :], s3[:, 2:3], gfin[:], ALU.mult, ALU.mult, [ht.b, s3.b, gfin.b], [o_.b])
                r0 = pc * PC + tt_ * 128
                K.dma(out_d[r0:r0 + 128, :], o_[:], reads=[o_.b])
                yield

        def rr2(gens):
            live = [g for g in gens if g is not None]
            while live:
                for g_ in list(live):
                    try:
                        next(g_)
                    except StopIteration:
                        live.remove(g_)

        rr2([pc_front(0)])
        for pc in range(NPCR):
            rr2([pc_back(pc), pc_front(pc + 1) if pc + 1 < NPCR else None])
        A.release(m0)


def _consts():
    ident = np.eye(128, dtype=np.float32)
    tri = (np.arange(128)[:, None] <= np.arange(128)[None, :]).astype(np.float32)
    iota = np.broadcast_to(np.arange(128, dtype=np.float32)[None, :], (128, 128))
    cst = np.ascontiguousarray(np.stack([ident, tri, iota], axis=1))
    half = 32
    inv_freq = (1.0 / (10000.0 ** (np.arange(half, dtype=np.float32) * 2.0 / 64))).astype(np.float32)
    ang = np.arange(S, dtype=np.float32)[:, None] * inv_freq[None, :]
    cos, sin = np.cos(ang).astype(np.float32), np.sin(ang).astype(np.float32)
    f = (np.arange(128) % 64) % 32
    rope = np.ascontiguousarray(np.stack([cos[:, f].T, sin[:, f].T], axis=1))
    return cst, rope


def prep_inputs(inp):
    l = 0
    f32 = lambda a: np.ascontiguousarray(np.asarray(a, dtype=np.float32))
    pkv = np.zeros((128, 160), np.float32)
    pkv[:, 0:8] = f32(inp["attn_norm_g"])[l].reshape(8, 128).T
    pkv[:, 8:16] = f32(inp["ffn_norm_g"])[l].reshape(8, 128).T
    pkv[:, 16:24] = f32(inp["ple_norm_g"])[l].reshape(8, 128).T
    cw = f32(inp["conv_w"])[l]
    pkv[:, 24:148] = cw.reshape(31, 4, 128).transpose(2, 1, 0).reshape(128, 124)
    pkv[:, 148:152] = f32(inp["conv_b"])[l].reshape(4, 128).T
    pkv[:, 152:156] = f32(inp["conv_ln_g"])[l].reshape(4, 128).T
    pkv[:, 156:160] = f32(inp["conv_ln_b"])[l].reshape(4, 128).T
    row = np.concatenate([f32(inp["final_norm_g"]), f32(inp["subln_g"])[l], f32(inp["lambda_q1"])[l],
                          f32(inp["lambda_k1"])[l], f32(inp["lambda_q2"])[l], f32(inp["lambda_k2"])[l]])
    rows = np.ascontiguousarray(np.broadcast_to(row[None, :], (128, 1408)))
    U = f32(inp["peer_u"])[l]
    V = f32(inp["peer_v"])[l]
    tab = np.empty((128, 128, 2048), np.float32)
    tab[:, :, 0:1024] = U.reshape(128, 128, 8, 128).transpose(1, 3, 2, 0).reshape(128, 128, 1024)
    tab[:, :, 1024:2048] = V.reshape(128, 128, 1024).transpose(1, 0, 2)
    cst, rope = _consts()
    shared = dict(w_in=f32(inp["w_in"])[l], w_out=f32(inp["w_out"])[l], wq=f32(inp["peer_wq"])[l],
                  keys=f32(inp["peer_keys"])[l], wg=f32(inp["ple_w_gate"])[l], wp=f32(inp["ple_w_proj"])[l],
                  tab=tab, pk=pkv, rows=rows, cst=cst, rope=rope)
    x = f32(inp["x"])
    p = f32(inp["p"])[l]
    maps = []
    for b in range(NCORES):
        m = dict(shared)
        m["x"] = np.ascontiguousarray(x[b])
        m["p"] = np.ascontiguousarray(p[b])
        maps.append(m)
    return maps


_NC_CACHE = {}


def kernel(**inputs):
    maps = prep_inputs(inputs)
    if "nc" not in _NC_CACHE:
        _NC_CACHE["nc"] = build()
    res = run_bass_kernel_spmd(_NC_CACHE["nc"], maps, core_ids=list(range(NCORES)))
    return np.stack([np.asarray(r["out"], dtype=np.float32) for r in res.results], axis=0)
```

```python
import numpy as np
import concourse.bass as bass
import concourse.mybir as mybir
from concourse.bass_utils import run_bass_kernel_spmd

F32 = mybir.dt.float32
BF16 = mybir.dt.bfloat16
U32 = mybir.dt.uint32
AF = mybir.ActivationFunctionType
ALU = mybir.AluOpType
AX = mybir.AxisListType

D = 1024
S = 4096
NCORES = 8
EPS = 1e-6
NT = S // 128
TC = 512
NCH = S // TC
PC = 256
NPC = S // PC
LAMBDA_INIT = 0.2


class Buf:
    __slots__ = ("name", "last_w", "rd_comp", "rd_dma", "pre", "subs")

    def __init__(self, name):
        self.name = name
        self.pre = None
        self.subs = None
        self.last_w = None
        self.rd_comp = {}
        self.rd_dma = []


class Op:
    __slots__ = ("eng", "fn", "deps", "is_dma", "slot", "sem_val", "signal", "cnt", "idx")


ENGS = ["pe", "act", "dve", "pool", "sp"]
DMA_SLOTS = {"sp": 24, "pool": 8, "act": 4}


class Prog:
    def __init__(self, nc):
        self.nc = nc
        self.by_eng = {e: [] for e in ENGS}
        self.nops = 0
        self.dma_rr = {e: 0 for e in DMA_SLOTS}
        self.dma_last = {}
        self.dma_tot = {}

    def add(self, eng, fn, reads=(), writes=(), dma=False):
        op = Op()
        op.eng = eng
        op.fn = fn
        op.is_dma = dma
        op.signal = False
        op.cnt = 0
        op.idx = self.nops
        self.nops += 1
        deps = set()
        weak = set()
        for b in reads:
            if b.last_w is not None:
                deps.add(b.last_w)
        for b in writes:
            if b.last_w is not None:
                weak.add(b.last_w)
            weak.update(b.rd_comp.values())
            weak.update(b.rd_dma)
            if b.pre:
                for (_lo, _hi, ot) in b.pre:
                    for ob in [ot.b] + (ot.b.subs or []):
                        if ob.last_w is not None:
                            weak.add(ob.last_w)
                        weak.update(ob.rd_comp.values())
                        weak.update(ob.rd_dma)
                b.pre = None
        for d in weak:
            deps.add(d)
        if dma:
            slot = self.dma_rr[eng]
            self.dma_rr[eng] = (slot + 1) % DMA_SLOTS[eng]
            prev = self.dma_last.get((eng, slot))
            if prev is not None:
                deps.add(prev)
            self.dma_last[(eng, slot)] = op
            tot = self.dma_tot.get((eng, slot), 0) + 16
            self.dma_tot[(eng, slot)] = tot
            op.slot = slot
            op.sem_val = tot
        op.deps = [d for d in deps if not (d is op) and not (eng == "pe" and d.eng == "pe" and not d.is_dma and not dma)]
        for d in op.deps:
            if not d.is_dma:
                d.signal = True
        for b in reads:
            if dma:
                b.rd_dma.append(op)
            else:
                b.rd_comp[eng] = op
        for b in writes:
            b.last_w = op
            b.rd_comp = {}
            b.rd_dma = []
        self.by_eng[eng].append(op)
        return op

    def emit(self):
        nc = self.nc
        from contextlib import ExitStack
        with ExitStack() as es:
            csem = {e: es.enter_context(nc.semaphore("c_" + e)) for e in ENGS}
            dsem = {}
            for e, n in DMA_SLOTS.items():
                for s in range(n):
                    dsem[(e, s)] = es.enter_context(nc.semaphore("d_%s_%d" % (e, s)))
            for e in ENGS:
                c = 0
                for op in self.by_eng[e]:
                    if op.signal and not op.is_dma:
                        c += 1
                        op.cnt = c
            engobj = {"pe": "tensor", "act": "scalar", "dve": "vector", "pool": "gpsimd", "sp": "sync"}
            block = es.enter_context(nc.Block())

            def run(e, eng):
                waited = {}
                for op in self.by_eng[e]:
                    need = {}
                    for d in op.deps:
                        if d.is_dma:
                            key = ("d", d.eng, d.slot)
                            val = d.sem_val
                        else:
                            key = ("c", d.eng)
                            val = d.cnt
                        if need.get(key, 0) < val:
                            need[key] = val
                    for key, val in need.items():
                        if waited.get(key, 0) >= val:
                            continue
                        waited[key] = val
                        sem = csem[key[1]] if key[0] == "c" else dsem[(key[1], key[2])]
                        eng.wait_ge(sem, val)
                    ins = op.fn(eng)
                    if op.is_dma:
                        ins.then_inc(dsem[(e, op.slot)], 16)
                    elif op.signal:
                        ins.then_inc(csem[e], 1)
                if e == "sp":
                    for (qe, s), tot in self.dma_tot.items():
                        eng.wait_ge(dsem[(qe, s)], tot)

            for e in ENGS:
                if not self.by_eng[e] and e != "sp":
                    continue
                getattr(block, engobj[e])(lambda eng, e=e: run(e, eng))


class Tile:
    __slots__ = ("t", "b", "lo", "hi", "pre")

    def __getitem__(self, k):
        return self.t[k]

    def sub(self, n):
        out = []
        for i in range(n):
            b = Buf("%s.%d" % (self.b.name, i))
            b.pre = list(self.pre)
            out.append(b)
        self.b.subs = (self.b.subs or []) + out
        return out


class Arena:
    def __init__(self, nc, limit=229344):
        self.nc = nc
        self.off = 16512
        self.limit = limit
        self.n = 0
        self.hist = []

    def alloc(self, shape, dtype, name="t"):
        esz = {F32: 4, BF16: 2, U32: 4}[dtype]
        free = 1
        for s in shape[1:]:
            free *= s
        nbytes = (free * esz + 63) // 64 * 64
        assert self.off + nbytes <= self.limit, ("SBUF overflow", name, self.off, nbytes)
        self.n += 1
        T = Tile()
        T.t = self.nc.alloc_sbuf_tensor_at("%s_%d" % (name, self.n), list(shape), dtype, offset=self.off)
        T.lo = self.off
        T.hi = self.off + nbytes
        T.b = Buf(name)
        T.pre = [o for o in self.hist if o[0] < T.hi and T.lo < o[1]]
        T.b.pre = list(T.pre)
        self.hist.append((T.lo, T.hi, T))
        self.off += nbytes
        return T

    def mark(self):
        return self.off

    def release(self, m):
        self.off = m


class KB:
    def __init__(self, debug=False):
        self.nc = nc = bass.Bass("TRN2", target_bir_lowering=False)
        self.P = Prog(nc)
        self.A = Arena(nc)
        self.debug = debug
        self.ps = [nc.alloc_psum_tensor("ps%d" % i, [128, 512], F32).ap() for i in range(8)]
        self.psb = [p.bitcast(BF16) for p in self.ps]
        self.bps = [[Buf("ps%d" % i)] for i in range(8)]

    def din(self, name, shape, dt=F32):
        return self.nc.dram_tensor(name, list(shape), dt, kind="ExternalInput").ap()

    def dout(self, name, shape, dt=F32):
        return self.nc.dram_tensor(name, list(shape), dt, kind="ExternalOutput").ap()

    def dscr(self, name, shape, dt):
        return self.nc.dram_tensor(name, list(shape), dt, kind="Internal").ap()

    def dma(self, out, in_, reads=(), writes=(), q="sp", **kw):
        return self.P.add(q, lambda e: e.dma_start(out=out, in_=in_, **kw), reads, writes, dma=True)

    def act(self, out, in_, func, reads, writes, **kw):
        return self.P.add("act", lambda e: e.activation(out=out, in_=in_, func=func, **kw), reads, writes)

    def tt(self, eng, out, in0, in1, op, reads, writes):
        return self.P.add(eng, lambda e: e.tensor_tensor(out=out, in0=in0, in1=in1, op=op), reads, writes)

    def ts(self, eng, out, in0, s1, s2, op0, op1, reads, writes):
        if op1 is None:
            return self.P.add(eng, lambda e: e.tensor_scalar(out=out, in0=in0, scalar1=s1, scalar2=None, op0=op0), reads, writes)
        return self.P.add(eng, lambda e: e.tensor_scalar(out=out, in0=in0, scalar1=s1, scalar2=s2, op0=op0, op1=op1), reads, writes)

    def stt(self, out, in0, scalar, in1, op0, op1, reads, writes):
        return self.P.add("dve", lambda e: e.scalar_tensor_tensor(out=out, in0=in0, scalar=scalar, in1=in1, op0=op0, op1=op1), reads, writes)

    def cp(self, eng, out, in_, reads, writes):
        if eng == "act":
            return self.P.add("act", lambda e: e.copy(out=out, in_=in_), reads, writes)
        return self.P.add(eng, lambda e: e.tensor_copy(out=out, in_=in_), reads, writes)

    def mm(self, out, lhsT, rhs, start, stop, reads, writes, skip=False):
        return self.P.add("pe", lambda e: e.matmul(out, lhsT=lhsT, rhs=rhs, start=start, stop=stop, skip_group_check=skip), reads, writes)

    def tr(self, out, in_, ident, reads, writes):
        return self.P.add("pe", lambda e: e.transpose(out=out, in_=in_, identity=ident), reads, writes)

    def recip(self, out, in_, reads, writes):
        return self.P.add("dve", lambda e: e.reciprocal(out=out, in_=in_), reads, writes)

    def memset(self, eng, ap, val, writes):
        return self.P.add(eng, lambda e: e.memset(ap, val), (), writes)


def load_norm_T(K, src, xt, xn, xnT, ss, junk, g_col, idb, bidb, bconst, tr_banks, ntt, rd=()):
    nt = ntt * 128
    K.dma(xt[:, 0:ntt, :], src.rearrange("(tt p) d -> p tt d", p=128), reads=list(rd), writes=[xt.b])
    for tt in range(ntt):
        if junk is None:
            K.act(xn[:, tt, :], xt[:, tt, :], AF.Square, [xt.b], [xn.b, ss.b], accum_out=ss[:, tt:tt + 1])
        else:
            K.act(junk[:], xt[:, tt, :], AF.Square, [xt.b], [junk.b, ss.b], accum_out=ss[:, tt:tt + 1])
    K.ts("pool", ss[:, 8:8 + ntt], ss[:, 0:ntt], 1.0 / D, EPS, ALU.mult, ALU.add, [ss.b], [ss.b])
    K.tt("pool", ss[:, 16:16 + ntt], ss[:, 8:8 + ntt], K.mhalf[:, 0:ntt], ALU.pow, [ss.b, K.mhalf.b], [ss.b])
    for tt in range(ntt):
        K.ts("dve", xn[:, tt, :], xt[:, tt, :], ss[:, 16 + tt:17 + tt], None, ALU.mult, None, [xt.b, ss.b], [xn.b])
    for k in range(8):
        bk = tr_banks[k % len(tr_banks)]
        for tt in range(ntt):
            K.tr(K.psb[bk][:, tt * 128:(tt + 1) * 128], xn[:, tt, k * 128:(k + 1) * 128], idb, [xn.b, bidb], K.bps[bk])
        if k % 2 == 0:
            K.act(xnT[:, k, 0:nt], K.psb[bk][:, 0:nt], AF.Copy, [K.bps[bk][0], bconst], [xnT.b], scale=g_col[:, k:k + 1])
        else:
            K.ts("dve", xnT[:, k, 0:nt], K.psb[bk][:, 0:nt], g_col[:, k:k + 1], None, ALU.mult, None, [K.bps[bk][0], bconst], [xnT.b])


def build(debug=False, phases="TCAP"):
    K = KB(debug)
    nc, P, A = K.nc, K.P, K.A
    x_d = K.din("x", [S, D])
    p_d = K.din("p", [S, 256])
    w_in_d = K.din("w_in", [D, 2560])
    w_out_d = K.din("w_out", [D, D])
    wq_d = K.din("wq", [D, 2048])
    keys_d = K.din("keys", [2, 128, 128])
    wg_d = K.din("wg", [D, D])
    wp_d = K.din("wp", [256, D])
    tab_d = K.din("tab", [128, 128, 2048])
    pk_d = K.din("pk", [128, 160])
    rows_d = K.din("rows", [128, 1408])
    cst_d = K.din("cst", [128, 3, 128])
    rope_d = K.din("rope", [128, 2, S])
    out_d = K.dout("out", [S, D])
    tabb_d = K.dscr("tabb", [128, 128, 2048], BF16)
    h1_d = K.dscr("h1s", [S, D], F32)
    cvT_d = K.dscr("cvTs", [128, 4, S], BF16)
    btab = [Buf("tabb%d" % j) for j in range(128)]
    bh1 = [Buf("h1s%d" % i) for i in range(NT)]
    bcv = [Buf("cvT%d" % i) for i in range(NCH)]
    dbg = {}

    pk = A.alloc([128, 160], F32, "pk")
    rows = A.alloc([128, 384], F32, "rows")
    cst = A.alloc([128, 3, 128], F32, "cst")
    cstb = A.alloc([128, 2, 128], BF16, "cstb")
    onesf = A.alloc([128, 128], F32, "onesf")
    K.dma(pk[:], pk_d, writes=[pk.b])
    K.dma(rows[:], rows_d[:, 1024:1408], writes=[rows.b])
    K.dma(cst[:], cst_d, writes=[cst.b])
    K.cp("dve", cstb[:], cst[:, 0:2, :], [cst.b], [cstb.b])
    K.memset("dve", onesf[:], 1.0 / 512, [onesf.b])
    mhalf = A.alloc([128, 8], F32, "mhalf")
    K.memset("pool", mhalf[:], -0.5, [mhalf.b])
    K.mhalf = mhalf
    idb = cstb[:, 0, :]
    trib = cstb[:, 1, :]
    idf = cst[:, 0, :]
    iota = cst[:, 2, :]
    g_attn = pk[:, 0:8]
    g_ffn = pk[:, 8:16]
    g_ple = pk[:, 16:24]
    conv_w = pk[:, 24:148].rearrange("p (c k) -> p c k", c=4)
    conv_b = pk[:, 148:152]
    ln_g = pk[:, 152:156]
    ln_b = pk[:, 156:160]
    g_fin = None
    subg = rows[:, 0:128]
    lamv = rows[:, 128:384]
    base_mark = A.mark()

    if "T" in phases:
        TG = 8
        for j0 in range(0, 128, TG):
            K.dma(tabb_d[j0:j0 + TG], tab_d[j0:j0 + TG], writes=btab[j0:j0 + TG], q="pool")

    if "C" in phases:
        m0 = A.mark()
        wC = A.alloc([128, 8, 1024], BF16, "wC")
        diag = A.alloc([128, 4, 31, 128], BF16, "diag")
        m1 = A.mark()
        stg = [A.alloc([128, 1024], F32, "stg") for _ in range(2)]
        for k in range(8):
            s_ = stg[k % 2]
            K.dma(s_[:], w_in_d[k * 128:(k + 1) * 128, 1536:2560], writes=[s_.b])
            K.cp("act" if k % 2 == 0 else "dve", wC[:, k, :], s_[:], [s_.b], [wC.b])
        for c in range(4):
            for k in range(31):
                K.ts("pool" if (k % 2) else "dve", diag[:, c, k, :], idf, conv_w[:, c, k:k + 1], None, ALU.mult, None, [cst.b, pk.b], [diag.b])
        A.release(m1)
        xt = A.alloc([128, 4, 1024], F32, "xt")
        xn = A.alloc([128, 4, 1024], BF16, "xn")
        xnT = A.alloc([128, 8, 512], BF16, "xnT")
        ss = A.alloc([128, 24], F32, "ss")
        junk = A.alloc([128, 1024], BF16, "junk")
        u = [A.alloc([128, 4, 542], BF16, "u") for _ in range(3)]
        sig = A.alloc([128, 512], F32, "sig")
        ysb = A.alloc([128, 4, 512], F32, "ysb")
        ysq = A.alloc([128, 4, 512], F32, "ysq")
        mean_sb = A.alloc([128, 512], F32, "mean")
        m2 = A.alloc([128, 512], F32, "m2")
        var = A.alloc([128, 512], F32, "var")
        rstdb = A.alloc([128, 512], F32, "rstdb")
        zt = A.alloc([128, 512], F32, "zt")
        z2 = A.alloc([128, 512], F32, "z2")
        cvo = A.alloc([128, 4, 512], BF16, "cvo")
        K.memset("pool", u[0][:, :, 0:30], 0.0, [u[0].b])
        import os
        NCHC = int(os.environ.get('DBG_NCHC', NCH))

        def c_front(ci):
            uc, un = u[ci % 3], u[(ci + 1) % 3]
            load_norm_T(K, x_d[ci * TC:(ci + 1) * TC, :], xt, xn, xnT, ss, junk, g_attn, idb, cstb.b, pk.b, [0, 1], 4)
            yield
            for c in range(4):
                for k in range(8):
                    K.mm(K.ps[2][:, :], wC[:, k, c * 128:(c + 1) * 128], xnT[:, k, :], k == 0, k == 7, [wC.b, xnT.b], K.bps[2])
                for k in range(8):
                    K.mm(K.ps[3][:, :], wC[:, k, 512 + c * 128:512 + (c + 1) * 128], xnT[:, k, :], k == 0, k == 7, [wC.b, xnT.b], K.bps[3])
                K.act(sig[:], K.ps[3][:, :], AF.Sigmoid, K.bps[3], [sig.b])
                K.tt("dve", uc[:, c, 30:542], K.ps[2][:, :], sig[:], ALU.mult, K.bps[2] + [sig.b], [uc.b])
                yield
            if ci + 1 < NCH:
                K.cp("pool", un[:, :, 0:30], uc[:, :, 512:542], [uc.b], [un.b])
            yield

        def c_back(ci):
            uc = u[ci % 3]
            for c in range(4):
                yb = 4 + c % 2
                for k in range(31):
                    K.mm(K.ps[yb][:, :], diag[:, c, k, :], uc[:, c, k:k + 512], k == 0, k == 30, [diag.b, uc.b], K.bps[yb])
                K.act(ysb[:, c, :], K.ps[yb][:, :], AF.Identity, K.bps[yb] + [pk.b], [ysb.b], bias=conv_b[:, c:c + 1])
                K.act(ysq[:, c, :], K.ps[yb][:, :], AF.Square, K.bps[yb] + [pk.b], [ysq.b], bias=conv_b[:, c:c + 1])
                yield
            for c in range(4):
                K.mm(K.ps[6][:, :], onesf[:], ysb[:, c, :], c == 0, c == 3, [onesf.b, ysb.b], K.bps[6])
            for c in range(4):
                K.mm(K.ps[7][:, :], onesf[:], ysq[:, c, :], c == 0, c == 3, [onesf.b, ysq.b], K.bps[7])
            K.cp("act", mean_sb[:], K.ps[6][:, :], K.bps[6], [mean_sb.b])
            K.tt("dve", m2[:], mean_sb[:], mean_sb[:], ALU.mult, [mean_sb.b], [m2.b])
            K.tt("dve", var[:], K.ps[7][:, :], m2[:], ALU.subtract, K.bps[7] + [m2.b], [var.b])
            K.act(var[:], var[:], AF.Sqrt, [var.b], [var.b], bias=EPS)
            K.recip(rstdb[:], var[:], [var.b], [rstdb.b])
            yield
            for c in range(4):
                K.tt("dve", zt[:], ysb[:, c, :], mean_sb[:], ALU.subtract, [ysb.b, mean_sb.b], [zt.b])
                K.tt("dve", z2[:], zt[:], rstdb[:], ALU.mult, [zt.b, rstdb.b], [z2.b])
                K.act(cvo[:, c, :], z2[:], AF.Silu, [z2.b, pk.b], [cvo.b], scale=ln_g[:, c:c + 1], bias=ln_b[:, c:c + 1])
                yield
            K.dma(cvT_d[:, :, ci * TC:(ci + 1) * TC], cvo[:], reads=[cvo.b], writes=[bcv[ci]])

        def rr(gens):
            live = [g for g in gens if g is not None]
            while live:
                for g_ in list(live):
                    try:
                        next(g_)
                    except StopIteration:
                        live.remove(g_)

        rr([c_front(0)])
        for ci in range(NCHC):
            rr([c_back(ci), c_front(ci + 1) if ci + 1 < NCHC else None])
        A.release(m0)

    K.extra = dict(x_d=x_d, p_d=p_d, out_d=out_d, h1_d=h1_d, cvT_d=cvT_d, tabb_d=tabb_d, btab=btab, bh1=bh1, bcv=bcv,
                   rows_d=rows_d, w_in_d=w_in_d, w_out_d=w_out_d, wq_d=wq_d, keys_d=keys_d, wg_d=wg_d, wp_d=wp_d, rope_d=rope_d,
                   pk=pk, rows=rows, cst=cst, cstb=cstb, idb=idb, trib=trib, idf=idf, iota=iota, g_attn=g_attn,
                   g_ffn=g_ffn, g_ple=g_ple, g_fin=g_fin, subg=subg, lamv=lamv, onesf=onesf)
    if "A" in phases:
        phase_A(K)
    if "P" in phases:
        phase_P(K)
    if debug:
        if "C" in phases and "A" not in phases:
            o = K.dout("dbg_cvT", [128, 4, S], BF16)
            K.dma(o, cvT_d, reads=bcv)
        if "A" in phases and "P" not in phases:
            o = K.dout("dbg_h1", [S, D], F32)
            K.dma(o, h1_d, reads=bh1)
    P.emit()
    return nc


def phase_A(K):
    nc, P, A = K.nc, K.P, K.A
    X = K.extra
    x_d, w_in_d, w_out_d, rope_d, cvT_d, h1_d = X["x_d"], X["w_in_d"], X["w_out_d"], X["rope_d"], X["cvT_d"], X["h1_d"]
    pk, rows, cstb = X["pk"], X["rows"], X["cstb"]
    idb, trib, g_attn, subg, lamv = X["idb"], X["trib"], X["g_attn"], X["subg"], X["lamv"]
    bcv, bh1 = X["bcv"], X["bh1"]
    m0 = A.mark()
    wA = A.alloc([128, 8, 2560], BF16, "wA")
    wo = A.alloc([128, 8, 1024], BF16, "wo")
    kT = A.alloc([128, 4, S], BF16, "kT")
    bkT = kT.sub(NCH)
    Va = A.alloc([128, NT, 4, 130], BF16, "Va")
    bVa = Va.sub(NCH)
    lam = A.alloc([128, 8], F32, "lam")
    sg = A.alloc([128, 128], F32, "sg")
    ltmp = A.alloc([128, 128], F32, "ltmp")
    m1 = A.mark()
    stg = [A.alloc([128, 1536], F32, "stgA") for _ in range(2)]
    for k in range(8):
        s_ = stg[k % 2]
        K.dma(s_[:], w_in_d[k * 128:(k + 1) * 128, 0:1536], writes=[s_.b])
        K.cp("act", wA[:, k, 0:1536], s_[:], [s_.b], [wA.b])
        sv = s_[:, 0:1024].rearrange("p (j d) -> p j d", d=64)
        wr = wA[:, k, 1536:2560].rearrange("p (j d) -> p j d", d=64)
        K.ts("dve", wr[:, :, 0:32], sv[:, :, 32:64], -1.0, None, ALU.mult, None, [s_.b], [wA.b])
        K.cp("pool", wr[:, :, 32:64], sv[:, :, 0:32], [s_.b], [wA.b])
    stg2 = [A.alloc([128, 1024], F32, "stgO") for _ in range(2)]
    for k in range(8):
        s_ = stg2[k % 2]
        K.dma(s_[:], w_out_d[k * 128:(k + 1) * 128, :], writes=[s_.b])
        K.cp("act" if k % 2 else "dve", wo[:, k, :], s_[:], [s_.b], [wo.b])
    A.release(m1)
    K.tt("dve", ltmp[:, 0:64], lamv[:, 0:64], lamv[:, 64:128], ALU.mult, [rows.b], [ltmp.b])
    K.tt("dve", ltmp[:, 64:128], lamv[:, 128:192], lamv[:, 192:256], ALU.mult, [rows.b], [ltmp.b])
    K.P.add("dve", lambda e: e.tensor_reduce(out=lam[:, 0:2], in_=ltmp[:].rearrange("p (a b) -> p a b", a=2), axis=AX.X, op=ALU.add), [ltmp.b], [lam.b])
    K.act(lam[:, 2:4], lam[:, 0:2], AF.Exp, [lam.b], [lam.b])
    K.tt("dve", lam[:, 4:5], lam[:, 2:3], lam[:, 3:4], ALU.subtract, [lam.b], [lam.b])
    K.ts("dve", lam[:, 5:6], lam[:, 4:5], LAMBDA_INIT, -1.0, ALU.add, ALU.mult, [lam.b], [lam.b])
    K.ts("dve", sg[:], subg, 1.0 - LAMBDA_INIT, None, ALU.mult, None, [rows.b], [sg.b])
    neglam = lam[:, 5:6]
    K.memset("pool", Va[:], 1.0, bVa)

    import os
    LVL = int(os.environ.get('DBG_STOP', 9))
    xt = A.alloc([128, 4, 1024], F32, "xt")
    xn = A.alloc([128, 4, 1024], BF16, "xn")
    xnT = A.alloc([128, 8, 512], BF16, "xnT")
    ss = A.alloc([128, 24], F32, "ss")
    rp = [A.alloc([128, 2, 512], F32, "rp") for _ in range(1)]
    t1 = [A.alloc([128, 512], F32, "t1") for _ in range(1)]
    t2 = [A.alloc([128, 512], F32, "t2") for _ in range(1)]
    qTz = [A.alloc([128, 4, 512], BF16, "qTz") for _ in range(2)]
    bqT = [qTz[0].sub(4), qTz[1].sub(4)]
    K.memset("pool", qTz[0][64:128, :, :], 0.0, bqT[0])
    K.memset("pool", qTz[1][0:64, :, :], 0.0, bqT[1])
    PT = [A.alloc([128, 512], BF16, "PT") for _ in range(4)]
    att = A.alloc([128, 128], F32, "att")
    a1 = A.alloc([128, 128], F32, "a1")
    sm = A.alloc([128, 4, 8], F32, "sm")
    Osb = A.alloc([128, 8, 129], F32, "Osb")
    attn_sb = A.alloc([128, 4, 512], BF16, "attn_sb")
    catT = A.alloc([128, 8, 512], BF16, "catT")
    h1o = [A.alloc([128, 1024], F32, "h1o") for _ in range(2)]
    cnt = 0
    tcnt = 0
    import os
    for ci in range(int(os.environ.get('DBG_NCH', NCH))):
        if LVL < 2:
            break
        load_norm_T(K, x_d[ci * TC:(ci + 1) * TC, :], xt, xn, xnT, ss, None, g_attn, idb, cstb.b, pk.b, [0, 1], 4)
        rpc = rp[0]
        K.dma(rpc[:], rope_d[:, :, ci * TC:(ci + 1) * TC], writes=[rpc.b])
        K.dma(catT[:, 4:8, :], cvT_d[:, :, ci * TC:(ci + 1) * TC], reads=[bcv[ci]], writes=[catT.b])
        cos, sin = rpc[:, 0, :], rpc[:, 1, :]
        for h in range(4):
            for which in range(2):
                c0 = which * 512 + h * 128
                for k in range(8):
                    K.mm(K.ps[0][:, :], wA[:, k, c0:c0 + 128], xnT[:, k, :], k == 0, k == 7, [wA.b, xnT.b], K.bps[0])
                for k in range(8):
                    K.mm(K.ps[1][:, :], wA[:, k, 1536 + c0:1536 + c0 + 128], xnT[:, k, :], k == 0, k == 7, [wA.b, xnT.b], K.bps[1])
                ta, tb = t1[0], t2[0]
                tcnt += 1
                K.tt("dve", ta[:], K.ps[0][:, :], cos, ALU.mult, K.bps[0] + [rpc.b], [ta.b])
                K.tt("dve", tb[:], K.ps[1][:, :], sin, ALU.mult, K.bps[1] + [rpc.b], [tb.b])
                if which == 0:
                    K.tt("pool", qTz[0][0:64, h, :], ta[0:64, :], tb[0:64, :], ALU.add, [ta.b, tb.b], [bqT[0][h]])
                    K.tt("pool", qTz[1][64:128, h, :], ta[64:128, :], tb[64:128, :], ALU.add, [ta.b, tb.b], [bqT[1][h]])
                else:
                    K.tt("pool", kT[:, h, ci * TC:(ci + 1) * TC], ta[:], tb[:], ALU.add, [ta.b, tb.b], [bkT[ci]])
        if LVL < 3:
            continue
        for tt_ in range(4):
            bk = tt_ % 2
            for k in range(8):
                K.mm(K.ps[bk][:, :], xnT[:, k, tt_ * 128:(tt_ + 1) * 128], wA[:, k, 1024:1536], k == 0, k == 7, [wA.b, xnT.b], K.bps[bk])
            K.P.add("act", lambda e, bk=bk, tt_=tt_, ci=ci: e.copy(out=Va[:, ci * 4 + tt_, :, 0:128], in_=K.ps[bk][:, :].rearrange("p (h d) -> p h d", d=128)),
                    K.bps[bk], [bVa[ci]])
        if LVL < 4:
            continue
        items = [(h, c, kt) for h in range(4) for c in range(2) for kt in range(4 * ci + 4)]
        DP = 3

        def emit_qk(h, c, kt, idx):
            r = kt - 4 * ci
            c0 = max(r, 0) * 128
            sbk = idx % 4
            pt = PT[idx % 4]
            K.mm(K.ps[sbk][:, c0:512], kT[:, h, kt * 128:(kt + 1) * 128], qTz[c][:, h, c0:512],
                 True, True, [bkT[kt // 4], bqT[c][h]], K.bps[sbk])
            K.act(pt[:, c0:512], K.ps[sbk][:, c0:512], AF.Exp, K.bps[sbk], [pt.b], scale=0.125)
            if r >= 0:
                K.tt("dve", pt[:, c0:c0 + 128], pt[:, c0:c0 + 128], trib, ALU.mult, [pt.b, cstb.b], [pt.b])

        def emit_pv(h, c, kt, idx):
            r = kt - 4 * ci
            pt = PT[idx % 4]
            for tqi in range(max(r, 0), 4):
                a = c * 4 + tqi
                bank, col = 4 + a // 2, (a % 2) * 256
                K.mm(K.ps[bank][:, col:col + 129], pt[:, tqi * 128:(tqi + 1) * 128], Va[:, kt, h, 0:129],
                     kt == 0 and a % 2 == 0, kt == 4 * ci + tqi, [pt.b, bVa[kt // 4]], K.bps[bank], skip=True)
            if c == 1 and kt == 4 * ci + 3:
                post(h)

        def post(h):
            if LVL >= 5:
                for bi in range(4):
                    src = K.ps[4 + bi][:, :].rearrange("p (a b) -> p a b", b=256)[:, :, 0:129]
                    K.cp("act" if bi % 2 == 0 else "dve", Osb[:, 2 * bi:2 * bi + 2, :], src, K.bps[4 + bi], [Osb.b])
            for tqi in range(4):
                if LVL < 5:
                    continue
                O1 = Osb[:, tqi, :]
                O2 = Osb[:, 4 + tqi, :]
                b1 = [Osb.b]
                b2 = [Osb.b]
                SUB = int(os.environ.get("DBG_SUB", 9))
                K.recip(sm[:, tqi, 0:1], O1[:, 128:129], b1, [sm.b])
                K.recip(sm[:, tqi, 1:2], O2[:, 128:129], b2, [sm.b])
                if SUB < 2:
                    continue
                K.tt("dve", sm[:, tqi, 2:3], sm[:, tqi, 1:2], neglam, ALU.mult, [sm.b, lam.b], [sm.b])
                K.act(a1[:], O1[:, 0:128], AF.Copy, b1 + [sm.b], [a1.b], scale=sm[:, tqi, 0:1])
                if SUB < 3:
                    continue
                K.stt(att[:], O2[:, 0:128], sm[:, tqi, 2:3], a1[:], ALU.mult, ALU.add, b2 + [sm.b, a1.b], [att.b])
                if SUB < 4:
                    continue
                K.act(a1[:], att[:], AF.Square, [att.b], [a1.b, sm.b], accum_out=sm[:, tqi, 3:4])
                if SUB < 5:
                    continue
                K.ts("pool", sm[:, tqi, 4:5], sm[:, tqi, 3:4], 1.0 / 128, EPS, ALU.mult, ALU.add, [sm.b], [sm.b])
                K.tt("pool", sm[:, tqi, 5:6], sm[:, tqi, 4:5], K.mhalf[:, 0:1], ALU.pow, [sm.b, K.mhalf.b], [sm.b])
                if SUB < 6:
                    continue
                K.stt(attn_sb[:, tqi, h * 128:(h + 1) * 128], att[:], sm[:, tqi, 5:6], sg[:], ALU.mult, ALU.mult, [att.b, sm.b, sg.b], [attn_sb.b])
        for idx in range(len(items) + DP):
            if idx < len(items):
                emit_qk(*items[idx], cnt + idx)
            if idx >= DP:
                emit_pv(*items[idx - DP], cnt + idx - DP)
        cnt += len(items)
        if LVL < 6:
            continue
        for tqi in range(4):
            bk = tqi % 2
            for h in range(4):
                K.tr(K.psb[bk][:, h * 128:(h + 1) * 128], attn_sb[:, tqi, h * 128:(h + 1) * 128], idb, [attn_sb.b, cstb.b], K.bps[bk])
            if os.environ.get("DBG_V", "0") == "1":
                continue
            if os.environ.get("DBG_V", "0") == "2":
                for h in range(4):
                    K.cp("dve", catT[:, h, tqi * 128:(tqi + 1) * 128], K.psb[bk][:, h * 128:(h + 1) * 128], K.bps[bk], [catT.b])
                continue
            K.cp("dve", catT[:, 0:4, tqi * 128:(tqi + 1) * 128], K.psb[bk][:, 0:512].rearrange("p (h d) -> p h d", d=128), K.bps[bk], [catT.b])
        for tqi in range(4):
            if LVL < 7:
                continue
            ho = h1o[tqi % 2]
            for half in range(2):
                for k in range(8):
                    K.mm(K.ps[half][:, :], catT[:, k, tqi * 128:(tqi + 1) * 128], wo[:, k, half * 512:(half + 1) * 512], k == 0, k == 7, [catT.b, wo.b], K.bps[half])
                K.tt("dve", ho[:, half * 512:(half + 1) * 512], K.ps[half][:, :], xt[:, tqi, half * 512:(half + 1) * 512], ALU.add, K.bps[half] + [xt.b], [ho.b])
            ti = ci * 4 + tqi
            if LVL < 8:
                continue
            K.dma(h1_d[ti * 128:(ti + 1) * 128, :], ho[:], reads=[ho.b], writes=[bh1[ti]])
    A.release(m0)


def phase_P(K):
    import os
    nc, P, A = K.nc, K.P, K.A
    X = K.extra
    h1_d, out_d, p_d, wq_d, keys_d, wg_d, wp_d, tabb_d = X["h1_d"], X["out_d"], X["p_d"], X["wq_d"], X["keys_d"], X["wg_d"], X["wp_d"], X["tabb_d"]
    pk, rows, cst, cstb = X["pk"], X["rows"], X["cst"], X["cstb"]
    idb, idf, iota, g_ffn, g_ple, g_fin = X["idb"], X["idf"], X["iota"], X["g_ffn"], X["g_ple"], X["g_fin"]
    bh1, btab = X["bh1"], X["btab"]
    mT_d = K.dscr("mTs", [128, 8, S], BF16)
    pk_s = K.dscr("picks", [S, 3, 128], F32)
    bmT = [Buf("mTs%d" % i) for i in range(NPC)]
    bpk = [Buf("pks%d" % i) for i in range(NPC)]
    NPCR = int(os.environ.get("DBG_NPC", NPC))
    SUBP = os.environ.get("DBG_SUBP", "abc")

    sc_d = K.dscr("scs", [S, 2048], F32)
    bscd = [Buf("scs%d" % i) for i in range(NT)]
    bpkt = [Buf("pkt%d" % i) for i in range(NT)]
    NTR = NPCR * 2
    K.tk_done = 0
    B4 = [128, 8, 16, 16]
    iota16 = iota[:, 0:16].unsqueeze(1).unsqueeze(1).to_broadcast(B4)

    def alloc_topk_ws():
        w = dict(sc=A.alloc([128, 16, 128], F32, "sc"), sc2=A.alloc([128, 16, 128], F32, "sc2"),
                 sv=A.alloc([128, 16, 16], F32, "sv"), si=A.alloc([128, 16, 16], U32, "si"), sif=A.alloc([128, 16, 16], F32, "sif"),
                 cs=A.alloc([128, 8, 16], F32, "cs"), ci=A.alloc([128, 8, 16], U32, "ci"),
                 abu=A.alloc([128, 2, 8, 16], U32, "abu"), abf=A.alloc([128, 2, 8, 16], F32, "abf"),
                 pkt=A.alloc([128, 3, 128], F32, "pkt"),
                 ex=A.alloc([128, 8, 16], F32, "ex"), rs=A.alloc([128, 16], F32, "rs"))
        w["bsc"] = w["sc"].sub(4)
        w["bsv"] = w["sv"].sub(16)
        w["bsi"] = w["si"].sub(16)
        w["bsc2"] = w["sc2"].sub(16)
        w["bcs"] = w["cs"].sub(8)
        w["bci"] = w["ci"].sub(8)
        return w

    def topk_steps(ti, w):
        sc, sc2, sv, si, sif, cs, ci = w["sc"], w["sc2"], w["sv"], w["si"], w["sif"], w["cs"], w["ci"]
        abu, abf, pkt, ex, rs = w["abu"], w["abf"], w["pkt"], w["ex"], w["rs"]
        bsc, bsv, bsi, bsc2, bcs, bci = w["bsc"], w["bsv"], w["bsi"], w["bsc2"], w["bcs"], w["bci"]
        cand = sc[:].rearrange("p (h c) n -> p h (c n)", c=2)
        cand2 = sc2[:].rearrange("p (h c) n -> p h (c n)", c=2)
        eq = sc[:].rearrange("p (h c) (a b) -> p h (c a) b", c=2, b=16)
        svv = sv[:].rearrange("p (h c) k -> p h c k", c=2)
        sifv = sif[:].rearrange("p (h c) k -> p h c k", c=2)
        K.dma(sc[:].rearrange("p g n -> p (g n)"), sc_d[ti * 128:(ti + 1) * 128, :], reads=[bscd[ti]], writes=bsc)
        yield
        for g in range(16):
            K.P.add("dve", lambda e, g=g: e.max(out=sv[:, g, 0:8], in_=sc[:, g, :]), [bsc[g // 4]], [bsv[g]])
            if g % 3 == 2:
                yield
        yield
        for g in range(16):
            K.P.add("dve", lambda e, g=g: e.max_index(out=si[:, g, 0:8], in_max=sv[:, g, 0:8], in_values=sc[:, g, :]), [bsc[g // 4], bsv[g]], [bsi[g]])
            if g % 3 == 2:
                yield
        yield
        for g in range(16):
            K.P.add("dve", lambda e, g=g: e.match_replace(out=sc2[:, g, :], in_to_replace=sv[:, g, 0:8], in_values=sc[:, g, :], imm_value=-1e30), [bsc[g // 4], bsv[g]], [bsc2[g]])
            if g % 3 == 2:
                yield
        yield
        for g in range(16):
            K.P.add("dve", lambda e, g=g: e.max(out=sv[:, g, 8:16], in_=sc2[:, g, :]), [bsc2[g]], [bsv[g]])
            if g % 3 == 2:
                yield
        yield
        for g in range(16):
            K.P.add("dve", lambda e, g=g: e.max_index(out=si[:, g, 8:16], in_max=sv[:, g, 8:16], in_values=sc2[:, g, :]), [bsc2[g], bsv[g]], [bsi[g]])
            if g % 3 == 2:
                yield
        yield
        K.cp("dve", sif[:], si[:], bsi, [sif.b])
        K.tt("dve", cand.rearrange("p h (a b) -> p h a b", b=16), svv[:, :, 0, :].unsqueeze(3).to_broadcast(B4),
             svv[:, :, 1, :].unsqueeze(2).to_broadcast(B4), ALU.add, bsv, bsc)
        yield
        for h in range(8):
            K.P.add("dve", lambda e, h=h: e.max(out=cs[:, h, 0:8], in_=cand[:, h, :]), [bsc[h // 2]], [bcs[h]])
            if h % 3 == 2:
                yield
        yield
        for h in range(8):
            K.P.add("dve", lambda e, h=h: e.max_index(out=ci[:, h, 0:8], in_max=cs[:, h, 0:8], in_values=cand[:, h, :]), [bsc[h // 2], bcs[h]], [bci[h]])
            if h % 3 == 2:
                yield
        for h in range(8):
            K.P.add("dve", lambda e, h=h: e.match_replace(out=cand2[:, h, :], in_to_replace=cs[:, h, 0:8], in_values=cand[:, h, :], imm_value=-1e30),
                    [bsc[h // 2], bcs[h]], [bsc2[2 * h], bsc2[2 * h + 1]])
            if h % 3 == 2:
                yield
        yield
        for h in range(8):
            K.P.add("dve", lambda e, h=h: e.max(out=cs[:, h, 8:16], in_=cand2[:, h, :]), [bsc2[2 * h], bsc2[2 * h + 1]], [bcs[h]])
            if h % 3 == 2:
                yield
        yield
        for h in range(8):
            K.P.add("dve", lambda e, h=h: e.max_index(out=ci[:, h, 8:16], in_max=cs[:, h, 8:16], in_values=cand2[:, h, :]), [bsc2[2 * h], bsc2[2 * h + 1], bcs[h]], [bci[h]])
            if h % 3 == 2:
                yield
        yield
        K.P.add("dve", lambda e: e.tensor_single_scalar(out=abu[:, 0, :, :], in_=ci[:], scalar=4, op=ALU.logical_shift_right), bci, [abu.b])
        K.P.add("dve", lambda e: e.tensor_single_scalar(out=abu[:, 1, :, :], in_=ci[:], scalar=15, op=ALU.bitwise_and), bci, [abu.b])
        K.cp("dve", abf[:], abu[:], [abu.b], [abf.b])
        K.tt("dve", ex[:], cs[:], cs[:, :, 0:1].to_broadcast([128, 8, 16]), ALU.subtract, bcs, [ex.b])
        yield
        for a in range(2):
            K.tt("dve", eq, iota16, abf[:, a, :, :].unsqueeze(3).to_broadcast(B4), ALU.is_equal, [cst.b, abf.b], bsc)
            yield
            K.tt("dve", eq, eq, sifv[:, :, a, :].unsqueeze(2).to_broadcast(B4), ALU.mult, bsc + [sif.b], bsc)
            yield
            if a == 0:
                K.act(ex[:], ex[:], AF.Exp, [ex.b], [ex.b])
            K.P.add("dve", lambda e, a=a: e.tensor_reduce(out=pkt[:, a, :].rearrange("p (h k) -> p h k", k=16), in_=eq, axis=AX.X, op=ALU.add), bsc, [pkt.b])
            yield
        K.P.add("dve", lambda e: e.tensor_reduce(out=rs[:, 0:8], in_=ex[:], axis=AX.X, op=ALU.add), [ex.b], [rs.b])
        K.recip(rs[:, 8:16], rs[:, 0:8], [rs.b], [rs.b])
        K.tt("dve", pkt[:, 2, :].rearrange("p (h k) -> p h k", k=16), ex[:], rs[:, 8:16].unsqueeze(2).to_broadcast([128, 8, 16]), ALU.mult,
             [ex.b, rs.b], [pkt.b])
        K.dma(pk_s[ti * 128:(ti + 1) * 128], pkt[:], reads=[pkt.b], writes=[bpkt[ti]])
        yield

    def drain(g_):
        if g_ is not None:
            for _ in g_:
                pass

    def chain(*gs):
        for g_ in gs:
            yield from g_

    if "a" in SUBP:
        m0 = A.mark()
        wqb = A.alloc([128, 8, 2048], BF16, "wqb")
        keysT = A.alloc([128, 2, 128], BF16, "keysT")
        m1 = A.mark()
        stg = [A.alloc([128, 2048], F32, "stgq") for _ in range(2)]
        for k in range(8):
            s_ = stg[k % 2]
            K.dma(s_[:], wq_d[k * 128:(k + 1) * 128, :], writes=[s_.b])
            K.cp("act" if k % 2 else "dve", wqb[:, k, :], s_[:], [s_.b], [wqb.b])
        kst = A.alloc([128, 2, 128], F32, "kst")
        K.dma(kst[:], keys_d.rearrange("c n d -> n c d"), writes=[kst.b])
        for c in range(2):
            K.tr(K.ps[0][:, c * 128:(c + 1) * 128], kst[:, c, :], idf, [kst.b, cst.b], K.bps[0])
        K.cp("act", keysT[:], K.ps[0][:, 0:256].rearrange("p (c n) -> p c n", c=2), K.bps[0], [keysT.b])
        A.release(m1)
        ss = A.alloc([128, 24], F32, "ss")
        junk = A.alloc([128, 1024], BF16, "junk")
        CH = []
        for _ in range(2):
            CH.append(dict(ht=A.alloc([128, 2, 1024], F32, "ht"), hn=A.alloc([128, 2, 1024], BF16, "hn"),
                           mT=A.alloc([128, 8, PC], BF16, "mT"), qpT=A.alloc([128, 16, PC], BF16, "qpT")))
        scb = [A.alloc([128, 16, 128], F32, "scb") for _ in range(2)]

        def chunk_front(pc):
            c_ = CH[pc % 2]
            ht, hn, mT, qpT = c_["ht"], c_["hn"], c_["mT"], c_["qpT"]
            load_norm_T(K, h1_d[pc * PC:(pc + 1) * PC, :], ht, hn, mT, ss, junk, g_ffn, idb, cstb.b, pk.b, [0, 1], 2,
                        rd=bh1[pc * 2:pc * 2 + 2])
            K.dma(mT_d[:, :, pc * PC:(pc + 1) * PC], mT[:], reads=[mT.b], writes=[bmT[pc]])
            yield
            for g in range(16):
                bk = 2 + g % 2
                for k in range(8):
                    K.mm(K.ps[bk][:, 0:PC], wqb[:, k, g * 128:(g + 1) * 128], mT[:, k, :], k == 0, k == 7, [wqb.b, mT.b], K.bps[bk])
                K.cp("act" if g % 2 else "dve", qpT[:, g, :], K.ps[bk][:, 0:PC], K.bps[bk], [qpT.b])
                if g % 4 == 3:
                    yield

        def chunk_scores(pc):
            qpT = CH[pc % 2]["qpT"]
            for tt_ in range(2):
                sct = scb[tt_]
                for g in range(16):
                    bk = 4 + g // 4
                    K.mm(K.ps[bk][:, (g % 4) * 128:(g % 4 + 1) * 128], qpT[:, g, tt_ * 128:(tt_ + 1) * 128], keysT[:, g % 2, :], True, True,
                         [qpT.b, keysT.b], K.bps[bk], skip=True)
                yield
                for q4 in range(4):
                    K.cp("act" if q4 % 2 else "dve", sct[:, q4 * 4:q4 * 4 + 4, :], K.ps[4 + q4][:, :].rearrange("p (g n) -> p g n", n=128), K.bps[4 + q4], [sct.b])
                ti = pc * 2 + tt_
                K.dma(sc_d[ti * 128:(ti + 1) * 128, :], sct[:].rearrange("p g n -> p (g n)"), reads=[sct.b], writes=[bscd[ti]])
                yield

        def rr3(gens):
            live = [g for g in gens if g is not None]
            while live:
                for g_ in list(live):
                    try:
                        next(g_)
                    except StopIteration:
                        live.remove(g_)

        NEARLY = min(4, NTR)
        WSa = alloc_topk_ws()

        def early_topk():
            inner = chain(*[topk_steps(ti, WSa) for ti in range(NEARLY)])
            while True:
                for _ in range(3):
                    try:
                        next(inner)
                    except StopIteration:
                        return
                yield

        etk = None
        rr3([chunk_front(0)])
        for pc in range(NPCR):
            if pc == 2:
                etk = early_topk()
            rr3([chunk_scores(pc), chunk_front(pc + 1) if pc + 1 < NPCR else None])
            if etk is not None:
                for _ in range(6):
                    next(etk, None)
        if NPCR <= 2:
            etk = early_topk()
        drain(etk)
        K.tk_done = NEARLY
        A.release(m0)

    if "b" in SUBP:
        m0 = A.mark()
        NB = 6
        G = [A.alloc([128, PC, 128], BF16, "G") for _ in range(2)]
        wt = [A.alloc([128, 2048], BF16, "wt") for _ in range(NB)]
        mTb = [A.alloc([128, 8, PC], BF16, "mTb") for _ in range(1)]
        pks = [A.alloc([128, 3, PC], F32, "pks") for _ in range(2)]
        npk = [A.alloc([128, 2, PC], F32, "npk") for _ in range(2)]
        ht = A.alloc([128, 512], F32, "htb")
        Ast = [A.alloc([128, 4, 128], BF16, "Ast") for _ in range(2)]
        Bst = [A.alloc([128, 4, 128], BF16, "Bst") for _ in range(2)]
        tmpa = [A.alloc([128, 128], F32, "tmpa") for _ in range(1)]
        ge = [A.alloc([128, PC], BF16, "ge") for _ in range(3)]
        Hs = [A.alloc([128, PC], BF16, "Hs") for _ in range(4)]
        WS = alloc_topk_ws()
        pkl = A.alloc([128, 2, 3, 128], F32, "pkl")
        iotaB = iota.unsqueeze(1).to_broadcast([128, 4, 128])

        def gbuild(pc):
            Gc, pk_, nk = G[pc % 2], pks[pc % 2], npk[pc % 2]
            K.dma(pkl[:], pk_s[pc * PC:(pc + 1) * PC].rearrange("(tt p) a k -> p tt a k", p=128), reads=bpkt[2 * pc:2 * pc + 2], writes=[pkl.b])
            yield
            for tt_ in range(2):
                for a in range(3):
                    K.tr(K.ps[7][:, a * 128:(a + 1) * 128], pkl[:, tt_, a, :], idf, [pkl.b, cst.b], K.bps[7])
                yield
                K.cp("act", pk_[:, :, tt_ * 128:(tt_ + 1) * 128], K.ps[7][:, 0:384].rearrange("p (a t) -> p a t", a=3), K.bps[7], [pk_.b])
                yield
            K.ts("dve", nk[:, 0, :], pk_[:, 0, :], -1.0, None, ALU.mult, None, [pk_.b], [nk.b])
            K.ts("dve", nk[:, 1, :], pk_[:, 2, :], -1.0, None, ALU.mult, None, [pk_.b], [nk.b])
            yield
            for grp in range(PC // 4 + 1):
                if grp < PC // 4:
                    As, Bs = Ast[grp % 2], Bst[grp % 2]
                    for tl in range(4):
                        t = grp * 4 + tl
                        if tl % 2 == 0:
                            K.ts("dve", As[:, tl, :], iota, pk_[:, 0, t:t + 1], pk_[:, 2, t:t + 1], ALU.is_equal, ALU.mult, [cst.b, pk_.b], [As.b])
                        else:
                            ta = tmpa[0]
                            K.act(ta[:], iota, AF.Abs, [cst.b, nk.b], [ta.b], bias=nk[:, 0, t:t + 1])
                            K.act(As[:, tl, :], ta[:], AF.Relu, [ta.b, nk.b, pk_.b], [As.b], scale=nk[:, 1, t:t + 1], bias=pk_[:, 2, t:t + 1])
                    K.tt("dve", Bs[:], iotaB, pk_[:, 1, grp * 4:grp * 4 + 4].unsqueeze(2).to_broadcast([128, 4, 128]), ALU.is_equal, [cst.b, pk_.b], [Bs.b])
                if grp >= 1:
                    g1 = grp - 1
                    As, Bs = Ast[g1 % 2], Bst[g1 % 2]
                    for tl in range(4):
                        K.mm(K.ps[7][:, tl * 128:(tl + 1) * 128], As[:, tl, :], Bs[:, tl, :], True, True, [As.b, Bs.b], K.bps[7], skip=True)
                    K.cp("act", Gc[:, g1 * 4:g1 * 4 + 4, :], K.ps[7][:, :].rearrange("p (t j) -> p t j", j=128), K.bps[7], [Gc.b])
                yield

        def dense(pc, nxt, nxt2):
            Gc, mT = G[pc % 2], mTb[0]
            K.dma(mT[:], mT_d[:, :, pc * PC:(pc + 1) * PC], reads=[bmT[pc]], writes=[mT.b])
            for j in range(130):
                if j < 128:
                    w_ = wt[j % NB]
                    K.dma(w_[:], tabb_d[j], reads=[btab[j]], writes=[w_.b])
                    ab = 4 + j % 3
                    for k in range(8):
                        K.mm(K.ps[ab][:, 0:PC], w_[:, k * 128:(k + 1) * 128], mT[:, k, :], k == 0, k == 7, [w_.b, mT.b], K.bps[ab])
                    g_, h_ = ge[j % 3], Hs[j % 4]
                    K.act(g_[:], K.ps[ab][:, 0:PC], AF.Gelu, K.bps[ab], [g_.b])
                    K.tt("pool", h_[:], g_[:], Gc[:, :, j], ALU.mult, [g_.b, Gc.b], [h_.b])
                if j >= 2:
                    jj = j - 2
                    w_, h_ = wt[jj % NB], Hs[jj % 4]
                    for tt_ in range(2):
                        for half in range(2):
                            yb = tt_ * 2 + half
                            K.mm(K.ps[yb][:, :], h_[:, tt_ * 128:(tt_ + 1) * 128], w_[:, 1024 + half * 512:1024 + (half + 1) * 512], jj == 0, jj == 127,
                                 [h_.b, w_.b], K.bps[yb])
                if nxt is not None and j % 2 == 1:
                    next(nxt, None)
                if nxt2 is not None:
                    next(nxt2, None)
            for tt_ in range(2):
                ti = pc * 2 + tt_
                for half in range(2):
                    yb = tt_ * 2 + half
                    hsl = h1_d[ti * 128:(ti + 1) * 128, half * 512:(half + 1) * 512]
                    K.dma(ht[:], hsl, reads=[bh1[ti]], writes=[ht.b])
                    K.tt("dve", ht[:], K.ps[yb][:, :], ht[:], ALU.add, K.bps[yb] + [ht.b], [ht.b])
                    K.dma(hsl, ht[:], reads=[ht.b], writes=[bh1[ti]])

        def topk_chunk(pc):
            if pc >= NPCR:
                return None
            tiles = [ti for ti in (2 * pc, 2 * pc + 1) if ti >= K.tk_done]
            if not tiles:
                return None
            return chain(*[topk_steps(ti, WS) for ti in tiles])

        drain(topk_chunk(0))
        drain(topk_chunk(1))
        drain(gbuild(0))
        for pc in range(NPCR):
            nxt = gbuild(pc + 1) if pc + 1 < NPCR else None
            nxt2 = topk_chunk(pc + 2)
            dense(pc, nxt, nxt2)
            drain(nxt2)
            drain(nxt)
        A.release(m0)

    if "c" in SUBP:
        m0 = A.mark()
        wgb = A.alloc([128, 8, 1024], BF16, "wgb")
        wpb = A.alloc([128, 2, 1024], BF16, "wpb")
        m1 = A.mark()
        stg = [A.alloc([128, 1024], F32, "stgg") for _ in range(2)]
        for k in range(8):
            s_ = stg[k % 2]
            K.dma(s_[:], wg_d[k * 128:(k + 1) * 128, :], writes=[s_.b])
            K.cp("act" if k % 2 else "dve", wgb[:, k, :], s_[:], [s_.b], [wgb.b])
        for k in range(2):
            s_ = stg[k % 2]
            K.dma(s_[:], wp_d[k * 128:(k + 1) * 128, :], writes=[s_.b])
            K.cp("act" if k % 2 else "dve", wpb[:, k, :], s_[:], [s_.b], [wpb.b])
        A.release(m1)
        ss = A.alloc([128, 24], F32, "ssc")
        junk = A.alloc([128, 1024], BF16, "junkc")
        CHc = []
        for _ in range(2):
            CHc.append(dict(ht=A.alloc([128, 2, 1024], F32, "htc"), hn=A.alloc([128, 2, 1024], BF16, "hnc"), mT=A.alloc([128, 8, PC], BF16, "mTc"),
                            pt=A.alloc([128, 2, 256], F32, "pt"), pb=A.alloc([128, 2, 256], BF16, "pb"), pT=A.alloc([128, 2, PC], BF16, "pT")))
        gate = [A.alloc([128, 512], F32, "gate") for _ in range(4)]
        tmp = [A.alloc([128, 512], F32, "tmp") for _ in range(4)]
        ot = [A.alloc([128, 1024], F32, "ot") for _ in range(2)]
        s3 = A.alloc([128, 8], F32, "s3")
        gfin = A.alloc([128, 1024], F32, "gfin")
        K.dma(gfin[:], X["rows_d"][:, 0:1024], writes=[gfin.b])

        def pc_front(pc):
            c_ = CHc[pc % 2]
            ht, hn, mT, pt_, pb_, pT = c_["ht"], c_["hn"], c_["mT"], c_["pt"], c_["pb"], c_["pT"]
            load_norm_T(K, h1_d[pc * PC:(pc + 1) * PC, :], ht, hn, mT, ss, junk, g_ple, idb, cstb.b, pk.b, [0, 1], 2, rd=bh1[pc * 2:pc * 2 + 2])
            yield
            K.dma(pt_[:], p_d[pc * PC:(pc + 1) * PC, :].rearrange("(tt p) d -> p tt d", p=128), writes=[pt_.b])
            K.cp("pool", pb_[:], pt_[:], [pt_.b], [pb_.b])
            for kk in range(2):
                for tt_ in range(2):
                    K.tr(K.psb[2][:, kk * PC + tt_ * 128:kk * PC + (tt_ + 1) * 128], pb_[:, tt_, kk * 128:(kk + 1) * 128], idb, [pb_.b, cstb.b], K.bps[2])
            K.cp("act", pT[:], K.psb[2][:, 0:2 * PC].rearrange("p (k t) -> p k t", k=2), K.bps[2], [pT.b])
            yield

        def pc_back(pc):
            c_ = CHc[pc % 2]
            ht, mT, pT = c_["ht"], c_["mT"], c_["pT"]
            for tt_ in range(2):
                for half in range(2):
                    gbk, ebk = 4 + half, 6 + half
                    gt_, tm_ = gate[tt_ * 2 + half], tmp[tt_ * 2 + half]
                    for k in range(8):
                        K.mm(K.ps[gbk][:, :], mT[:, k, tt_ * 128:(tt_ + 1) * 128], wgb[:, k, half * 512:(half + 1) * 512], k == 0, k == 7, [mT.b, wgb.b], K.bps[gbk])
                    K.act(gt_[:], K.ps[gbk][:, :], AF.Sigmoid, K.bps[gbk], [gt_.b])
                    for kk in range(2):
                        K.mm(K.ps[ebk][:, :], pT[:, kk, tt_ * 128:(tt_ + 1) * 128], wpb[:, kk, half * 512:(half + 1) * 512], kk == 0, kk == 1, [pT.b, wpb.b], K.bps[ebk])
                    K.tt("dve", tm_[:], K.ps[ebk][:, :], gt_[:], ALU.mult, K.bps[ebk] + [gt_.b], [tm_.b])
                    K.tt("dve", ht[:, tt_, half * 512:(half + 1) * 512], tm_[:], ht[:, tt_, half * 512:(half + 1) * 512], ALU.add, [tm_.b, ht.b], [ht.b])
                    yield
                o_ = ot[tt_]
                K.act(junk[:], ht[:, tt_, :], AF.Square, [ht.b], [junk.b, s3.b], accum_out=s3[:, 0:1])
                K.ts("pool", s3[:, 1:2], s3[:, 0:1], 1.0 / D, EPS, ALU.mult, ALU.add, [s3.b], [s3.b])
                K.tt("pool", s3[:, 2:3], s3[:, 1:2], K.mhalf[:, 0:1], ALU.pow, [s3.b, K.mhalf.b], [s3.b])
                K.stt(o_[:], ht[:, tt_, :], s3[:, 2:3], gfin[:], ALU.mult, ALU.mult, [ht.b, s3.b, gfin.b], [o_.b])
                r0 = pc * PC + tt_ * 128
                K.dma(out_d[r0:r0 + 128, :], o_[:], reads=[o_.b])
                yield

        def rr2(gens):
            live = [g for g in gens if g is not None]
            while live:
                for g_ in list(live):
                    try:
                        next(g_)
                    except StopIteration:
                        live.remove(g_)

        rr2([pc_front(0)])
        for pc in range(NPCR):
            rr2([pc_back(pc), pc_front(pc + 1) if pc + 1 < NPCR else None])
        A.release(m0)


def _consts():
    ident = np.eye(128, dtype=np.float32)
    tri = (np.arange(128)[:, None] <= np.arange(128)[None, :]).astype(np.float32)
    iota = np.broadcast_to(np.arange(128, dtype=np.float32)[None, :], (128, 128))
    cst = np.ascontiguousarray(np.stack([ident, tri, iota], axis=1))
    half = 32
    inv_freq = (1.0 / (10000.0 ** (np.arange(half, dtype=np.float32) * 2.0 / 64))).astype(np.float32)
    ang = np.arange(S, dtype=np.float32)[:, None] * inv_freq[None, :]
    cos, sin = np.cos(ang).astype(np.float32), np.sin(ang).astype(np.float32)
    f = (np.arange(128) % 64) % 32
    rope = np.ascontiguousarray(np.stack([cos[:, f].T, sin[:, f].T], axis=1))
    return cst, rope


def prep_inputs(inp):
    l = 0
    f32 = lambda a: np.ascontiguousarray(np.asarray(a, dtype=np.float32))
    pkv = np.zeros((128, 160), np.float32)
    pkv[:, 0:8] = f32(inp["attn_norm_g"])[l].reshape(8, 128).T
    pkv[:, 8:16] = f32(inp["ffn_norm_g"])[l].reshape(8, 128).T
    pkv[:, 16:24] = f32(inp["ple_norm_g"])[l].reshape(8, 128).T
    cw = f32(inp["conv_w"])[l]
    pkv[:, 24:148] = cw.reshape(31, 4, 128).transpose(2, 1, 0).reshape(128, 124)
    pkv[:, 148:152] = f32(inp["conv_b"])[l].reshape(4, 128).T
    pkv[:, 152:156] = f32(inp["conv_ln_g"])[l].reshape(4, 128).T
    pkv[:, 156:160] = f32(inp["conv_ln_b"])[l].reshape(4, 128).T
    row = np.concatenate([f32(inp["final_norm_g"]), f32(inp["subln_g"])[l], f32(inp["lambda_q1"])[l],
                          f32(inp["lambda_k1"])[l], f32(inp["lambda_q2"])[l], f32(inp["lambda_k2"])[l]])
    rows = np.ascontiguousarray(np.broadcast_to(row[None, :], (128, 1408)))
    U = f32(inp["peer_u"])[l]
    V = f32(inp["peer_v"])[l]
    tab = np.empty((128, 128, 2048), np.float32)
    tab[:, :, 0:1024] = U.reshape(128, 128, 8, 128).transpose(1, 3, 2, 0).reshape(128, 128, 1024)
    tab[:, :, 1024:2048] = V.reshape(128, 128, 1024).transpose(1, 0, 2)
    cst, rope = _consts()
    shared = dict(w_in=f32(inp["w_in"])[l], w_out=f32(inp["w_out"])[l], wq=f32(inp["peer_wq"])[l],
                  keys=f32(inp["peer_keys"])[l], wg=f32(inp["ple_w_gate"])[l], wp=f32(inp["ple_w_proj"])[l],
                  tab=tab, pk=pkv, rows=rows, cst=cst, rope=rope)
    x = f32(inp["x"])
    p = f32(inp["p"])[l]
    maps = []
    for b in range(NCORES):
        m = dict(shared)
        m["x"] = np.ascontiguousarray(x[b])
        m["p"] = np.ascontiguousarray(p[b])
        maps.append(m)
    return maps


_NC_CACHE = {}


def kernel(**inputs):
    maps = prep_inputs(inputs)
    if "nc" not in _NC_CACHE:
        _NC_CACHE["nc"] = build()
    res = run_bass_kernel_spmd(_NC_CACHE["nc"], maps, core_ids=list(range(NCORES)))
    return np.stack([np.asarray(r["out"], dtype=np.float32) for r in res.results], axis=0)
```

```python
import numpy as np
import concourse.bass as bass
import concourse.mybir as mybir
from concourse.bass_utils import run_bass_kernel_spmd

F32 = mybir.dt.float32
BF16 = mybir.dt.bfloat16
U32 = mybir.dt.uint32
AF = mybir.ActivationFunctionType
ALU = mybir.AluOpType
AX = mybir.AxisListType

D = 1024
S = 4096
NCORES = 8
EPS = 1e-6
NT = S // 128
TC = 512
NCH = S // TC
PC = 256
NPC = S // PC
LAMBDA_INIT = 0.2


class Buf:
    __slots__ = ("name", "last_w", "rd_comp", "rd_dma", "pre", "subs")

    def __init__(self, name):
        self.name = name
        self.pre = None
        self.subs = None
        self.last_w = None
        self.rd_comp = {}
        self.rd_dma = []


class Op:
    __slots__ = ("eng", "fn", "deps", "is_dma", "slot", "sem_val", "signal", "cnt", "idx")


ENGS = ["pe", "act", "dve", "pool", "sp"]
DMA_SLOTS = {"sp": 24, "pool": 8, "act": 4}


class Prog:
    def __init__(self, nc):
        self.nc = nc
        self.by_eng = {e: [] for e in ENGS}
        self.nops = 0
        self.dma_rr = {e: 0 for e in DMA_SLOTS}
        self.dma_last = {}
        self.dma_tot = {}

    def add(self, eng, fn, reads=(), writes=(), dma=False):
        op = Op()
        op.eng = eng
        op.fn = fn
        op.is_dma = dma
        op.signal = False
        op.cnt = 0
        op.idx = self.nops
        self.nops += 1
        deps = set()
        weak = set()
        for b in reads:
            if b.last_w is not None:
                deps.add(b.last_w)
        for b in writes:
            if b.last_w is not None:
                weak.add(b.last_w)
            weak.update(b.rd_comp.values())
            weak.update(b.rd_dma)
            if b.pre:
                for (_lo, _hi, ot) in b.pre:
                    for ob in [ot.b] + (ot.b.subs or []):
                        if ob.last_w is not None:
                            weak.add(ob.last_w)
                        weak.update(ob.rd_comp.values())
                        weak.update(ob.rd_dma)
                b.pre = None
        for d in weak:
            deps.add(d)
        if dma:
            slot = self.dma_rr[eng]
            self.dma_rr[eng] = (slot + 1) % DMA_SLOTS[eng]
            prev = self.dma_last.get((eng, slot))
            if prev is not None:
                deps.add(prev)
            self.dma_last[(eng, slot)] = op
            tot = self.dma_tot.get((eng, slot), 0) + 16
            self.dma_tot[(eng, slot)] = tot
            op.slot = slot
            op.sem_val = tot
        op.deps = [d for d in deps if not (d is op) and not (eng == "pe" and d.eng == "pe" and not d.is_dma and not dma)]
        for d in op.deps:
            if not d.is_dma:
                d.signal = True
        for b in reads:
            if dma:
                b.rd_dma.append(op)
            else:
                b.rd_comp[eng] = op
        for b in writes:
            b.last_w = op
            b.rd_comp = {}
            b.rd_dma = []
        self.by_eng[eng].append(op)
        return op

    def emit(self):
        nc = self.nc
        from contextlib import ExitStack
        with ExitStack() as es:
            csem = {e: es.enter_context(nc.semaphore("c_" + e)) for e in ENGS}
            dsem = {}
            for e, n in DMA_SLOTS.items():
                for s in range(n):
                    dsem[(e, s)] = es.enter_context(nc.semaphore("d_%s_%d" % (e, s)))
            for e in ENGS:
                c = 0
                for op in self.by_eng[e]:
                    if op.signal and not op.is_dma:
                        c += 1
                        op.cnt = c
            engobj = {"pe": "tensor", "act": "scalar", "dve": "vector", "pool": "gpsimd", "sp": "sync"}
            block = es.enter_context(nc.Block())

            def run(e, eng):
                waited = {}
                for op in self.by_eng[e]:
                    need = {}
                    for d in op.deps:
                        if d.is_dma:
                            key = ("d", d.eng, d.slot)
                            val = d.sem_val
                        else:
                            key = ("c", d.eng)
                            val = d.cnt
                        if need.get(key, 0) < val:
                            need[key] = val
                    for key, val in need.items():
                        if waited.get(key, 0) >= val:
                            continue
                        waited[key] = val
                        sem = csem[key[1]] if key[0] == "c" else dsem[(key[1], key[2])]
                        eng.wait_ge(sem, val)
                    ins = op.fn(eng)
                    if op.is_dma:
                        ins.then_inc(dsem[(e, op.slot)], 16)
                    elif op.signal:
                        ins.then_inc(csem[e], 1)
                if e == "sp":
                    for (qe, s), tot in self.dma_tot.items():
                        eng.wait_ge(dsem[(qe, s)], tot)

            for e in ENGS:
                if not self.by_eng[e] and e != "sp":
                    continue
                getattr(block, engobj[e])(lambda eng, e=e: run(e, eng))


class Tile:
    __slots__ = ("t", "b", "lo", "hi", "pre")

    def __getitem__(self, k):
        return self.t[k]

    def sub(self, n):
        out = []
        for i in range(n):
            b = Buf("%s.%d" % (self.b.name, i))
            b.pre = list(self.pre)
            out.append(b)
        self.b.subs = (self.b.subs or []) + out
        return out


class Arena:
    def __init__(self, nc, limit=229344):
        self.nc = nc
        self.off = 16512
        self.limit = limit
        self.n = 0
        self.hist = []

    def alloc(self, shape, dtype, name="t"):
        esz = {F32: 4, BF16: 2, U32: 4}[dtype]
        free = 1
        for s in shape[1:]:
            free *= s
        nbytes = (free * esz + 63) // 64 * 64
        assert self.off + nbytes <= self.limit, ("SBUF overflow", name, self.off, nbytes)
        self.n += 1
        T = Tile()
        T.t = self.nc.alloc_sbuf_tensor_at("%s_%d" % (name, self.n), list(shape), dtype, offset=self.off)
        T.lo = self.off
        T.hi = self.off + nbytes
        T.b = Buf(name)
        T.pre = [o for o in self.hist if o[0] < T.hi and T.lo < o[1]]
        T.b.pre = list(T.pre)
        self.hist.append((T.lo, T.hi, T))
        self.off += nbytes
        return T

    def mark(self):
        return self.off

    def release(self, m):
        self.off = m


class KB:
    def __init__(self, debug=False):
        self.nc = nc = bass.Bass("TRN2", target_bir_lowering=False)
        self.P = Prog(nc)
        self.A = Arena(nc)
        self.debug = debug
        self.ps = [nc.alloc_psum_tensor("ps%d" % i, [128, 512], F32).ap() for i in range(8)]
        self.psb = [p.bitcast(BF16) for p in self.ps]
        self.bps = [[Buf("ps%d" % i)] for i in range(8)]

    def din(self, name, shape, dt=F32):
        return self.nc.dram_tensor(name, list(shape), dt, kind="ExternalInput").ap()

    def dout(self, name, shape, dt=F32):
        return self.nc.dram_tensor(name, list(shape), dt, kind="ExternalOutput").ap()

    def dscr(self, name, shape, dt):
        return self.nc.dram_tensor(name, list(shape), dt, kind="Internal").ap()

    def dma(self, out, in_, reads=(), writes=(), q="sp", **kw):
        return self.P.add(q, lambda e: e.dma_start(out=out, in_=in_, **kw), reads, writes, dma=True)

    def act(self, out, in_, func, reads, writes, **kw):
        return self.P.add("act", lambda e: e.activation(out=out, in_=in_, func=func, **kw), reads, writes)

    def tt(self, eng, out, in0, in1, op, reads, writes):
        return self.P.add(eng, lambda e: e.tensor_tensor(out=out, in0=in0, in1=in1, op=op), reads, writes)

    def ts(self, eng, out, in0, s1, s2, op0, op1, reads, writes):
        if op1 is None:
            return self.P.add(eng, lambda e: e.tensor_scalar(out=out, in0=in0, scalar1=s1, scalar2=None, op0=op0), reads, writes)
        return self.P.add(eng, lambda e: e.tensor_scalar(out=out, in0=in0, scalar1=s1, scalar2=s2, op0=op0, op1=op1), reads, writes)

    def stt(self, out, in0, scalar, in1, op0, op1, reads, writes):
        return self.P.add("dve", lambda e: e.scalar_tensor_tensor(out=out, in0=in0, scalar=scalar, in1=in1, op0=op0, op1=op1), reads, writes)

    def cp(self, eng, out, in_, reads, writes):
        if eng == "act":
            return self.P.add("act", lambda e: e.copy(out=out, in_=in_), reads, writes)
        return self.P.add(eng, lambda e: e.tensor_copy(out=out, in_=in_), reads, writes)

    def mm(self, out, lhsT, rhs, start, stop, reads, writes, skip=False):
        return self.P.add("pe", lambda e: e.matmul(out, lhsT=lhsT, rhs=rhs, start=start, stop=stop, skip_group_check=skip), reads, writes)

    def tr(self, out, in_, ident, reads, writes):
        return self.P.add("pe", lambda e: e.transpose(out=out, in_=in_, identity=ident), reads, writes)

    def recip(self, out, in_, reads, writes):
        return self.P.add("dve", lambda e: e.reciprocal(out=out, in_=in_), reads, writes)

    def memset(self, eng, ap, val, writes):
        return self.P.add(eng, lambda e: e.memset(ap, val), (), writes)


def load_norm_T(K, src, xt, xn, xnT, ss, junk, g_col, idb, bidb, bconst, tr_banks, ntt, rd=()):
    nt = ntt * 128
    K.dma(xt[:, 0:ntt, :], src.rearrange("(tt p) d -> p tt d", p=128), reads=list(rd), writes=[xt.b])
    for tt in range(ntt):
        if junk is None:
            K.act(xn[:, tt, :], xt[:, tt, :], AF.Square, [xt.b], [xn.b, ss.b], accum_out=ss[:, tt:tt + 1])
        else:
            K.act(junk[:], xt[:, tt, :], AF.Square, [xt.b], [junk.b, ss.b], accum_out=ss[:, tt:tt + 1])
    K.ts("pool", ss[:, 8:8 + ntt], ss[:, 0:ntt], 1.0 / D, EPS, ALU.mult, ALU.add, [ss.b], [ss.b])
    K.tt("pool", ss[:, 16:16 + ntt], ss[:, 8:8 + ntt], K.mhalf[:, 0:ntt], ALU.pow, [ss.b, K.mhalf.b], [ss.b])
    for tt in range(ntt):
        K.ts("dve", xn[:, tt, :], xt[:, tt, :], ss[:, 16 + tt:17 + tt], None, ALU.mult, None, [xt.b, ss.b], [xn.b])
    for k in range(8):
        bk = tr_banks[k % len(tr_banks)]
        for tt in range(ntt):
            K.tr(K.psb[bk][:, tt * 128:(tt + 1) * 128], xn[:, tt, k * 128:(k + 1) * 128], idb, [xn.b, bidb], K.bps[bk])
        if k % 2 == 0:
            K.act(xnT[:, k, 0:nt], K.psb[bk][:, 0:nt], AF.Copy, [K.bps[bk][0], bconst], [xnT.b], scale=g_col[:, k:k + 1])
        else:
            K.ts("dve", xnT[:, k, 0:nt], K.psb[bk][:, 0:nt], g_col[:, k:k + 1], None, ALU.mult, None, [K.bps[bk][0], bconst], [xnT.b])


def build(debug=False, phases="TCAP"):
    K = KB(debug)
    nc, P, A = K.nc, K.P, K.A
    x_d = K.din("x", [S, D])
    p_d = K.din("p", [S, 256])
    w_in_d = K.din("w_in", [D, 2560])
    w_out_d = K.din("w_out", [D, D])
    wq_d = K.din("wq", [D, 2048])
    keys_d = K.din("keys", [2, 128, 128])
    wg_d = K.din("wg", [D, D])
    wp_d = K.din("wp", [256, D])
    tab_d = K.din("tab", [128, 128, 2048])
    pk_d = K.din("pk", [128, 160])
    rows_d = K.din("rows", [128, 1408])
    cst_d = K.din("cst", [128, 3, 128])
    rope_d = K.din("rope", [128, 2, S])
    out_d = K.dout("out", [S, D])
    tabb_d = K.dscr("tabb", [128, 128, 2048], BF16)
    h1_d = K.dscr("h1s", [S, D], F32)
    cvT_d = K.dscr("cvTs", [128, 4, S], BF16)
    btab = [Buf("tabb%d" % j) for j in range(128)]
    bh1 = [Buf("h1s%d" % i) for i in range(NT)]
    bcv = [Buf("cvT%d" % i) for i in range(NCH)]
    dbg = {}

    pk = A.alloc([128, 160], F32, "pk")
    rows = A.alloc([128, 384], F32, "rows")
    cst = A.alloc([128, 3, 128], F32, "cst")
    cstb = A.alloc([128, 2, 128], BF16, "cstb")
    onesf = A.alloc([128, 128], F32, "onesf")
    K.dma(pk[:], pk_d, writes=[pk.b])
    K.dma(rows[:], rows_d[:, 1024:1408], writes=[rows.b])
    K.dma(cst[:], cst_d, writes=[cst.b])
    K.cp("dve", cstb[:], cst[:, 0:2, :], [cst.b], [cstb.b])
    K.memset("dve", onesf[:], 1.0 / 512, [onesf.b])
    mhalf = A.alloc([128, 8], F32, "mhalf")
    K.memset("pool", mhalf[:], -0.5, [mhalf.b])
    K.mhalf = mhalf
    idb = cstb[:, 0, :]
    trib = cstb[:, 1, :]
    idf = cst[:, 0, :]
    iota = cst[:, 2, :]
    g_attn = pk[:, 0:8]
    g_ffn = pk[:, 8:16]
    g_ple = pk[:, 16:24]
    conv_w = pk[:, 24:148].rearrange("p (c k) -> p c k", c=4)
    conv_b = pk[:, 148:152]
    ln_g = pk[:, 152:156]
    ln_b = pk[:, 156:160]
    g_fin = None
    subg = rows[:, 0:128]
    lamv = rows[:, 128:384]
    base_mark = A.mark()

    if "T" in phases:
        TG = 8
        for j0 in range(0, 128, TG):
            K.dma(tabb_d[j0:j0 + TG], tab_d[j0:j0 + TG], writes=btab[j0:j0 + TG], q="pool")

    if "C" in phases:
        m0 = A.mark()
        wC = A.alloc([128, 8, 1024], BF16, "wC")
        diag = A.alloc([128, 4, 31, 128], BF16, "diag")
        m1 = A.mark()
        stg = [A.alloc([128, 1024], F32, "stg") for _ in range(2)]
        for k in range(8):
            s_ = stg[k % 2]
            K.dma(s_[:], w_in_d[k * 128:(k + 1) * 128, 1536:2560], writes=[s_.b])
            K.cp("act" if k % 2 == 0 else "dve", wC[:, k, :], s_[:], [s_.b], [wC.b])
        for c in range(4):
            for k in range(31):
                K.ts("pool" if (k % 2) else "dve", diag[:, c, k, :], idf, conv_w[:, c, k:k + 1], None, ALU.mult, None, [cst.b, pk.b], [diag.b])
        A.release(m1)
        xt = A.alloc([128, 4, 1024], F32, "xt")
        xn = A.alloc([128, 4, 1024], BF16, "xn")
        xnT = A.alloc([128, 8, 512], BF16, "xnT")
        ss = A.alloc([128, 24], F32, "ss")
        junk = A.alloc([128, 1024], BF16, "junk")
        u = [A.alloc([128, 4, 542], BF16, "u") for _ in range(3)]
        sig = A.alloc([128, 512], F32, "sig")
        ysb = A.alloc([128, 4, 512], F32, "ysb")
        ysq = A.alloc([128, 4, 512], F32, "ysq")
        mean_sb = A.alloc([128, 512], F32, "mean")
        m2 = A.alloc([128, 512], F32, "m2")
        var = A.alloc([128, 512], F32, "var")
        rstdb = A.alloc([128, 512], F32, "rstdb")
        zt = A.alloc([128, 512], F32, "zt")
        z2 = A.alloc([128, 512], F32, "z2")
        cvo = A.alloc([128, 4, 512], BF16, "cvo")
        K.memset("pool", u[0][:, :, 0:30], 0.0, [u[0].b])
        import os
        NCHC = int(os.environ.get('DBG_NCHC', NCH))

        def c_front(ci):
            uc, un = u[ci % 3], u[(ci + 1) % 3]
            load_norm_T(K, x_d[ci * TC:(ci + 1) * TC, :], xt, xn, xnT, ss, junk, g_attn, idb, cstb.b, pk.b, [0, 1], 4)
            yield
            for c in range(4):
                for k in range(8):
                    K.mm(K.ps[2][:, :], wC[:, k, c * 128:(c + 1) * 128], xnT[:, k, :], k == 0, k == 7, [wC.b, xnT.b], K.bps[2])
                for k in range(8):
                    K.mm(K.ps[3][:, :], wC[:, k, 512 + c * 128:512 + (c + 1) * 128], xnT[:, k, :], k == 0, k == 7, [wC.b, xnT.b], K.bps[3])
                K.act(sig[:], K.ps[3][:, :], AF.Sigmoid, K.bps[3], [sig.b])
                K.tt("dve", uc[:, c, 30:542], K.ps[2][:, :], sig[:], ALU.mult, K.bps[2] + [sig.b], [uc.b])
                yield
            if ci + 1 < NCH:
                K.cp("pool", un[:, :, 0:30], uc[:, :, 512:542], [uc.b], [un.b])
            yield

        def c_back(ci):
            uc = u[ci % 3]
            for c in range(4):
                yb = 4 + c % 2
                for k in range(31):
                    K.mm(K.ps[yb][:, :], diag[:, c, k, :], uc[:, c, k:k + 512], k == 0, k == 30, [diag.b, uc.b], K.bps[yb])
                K.act(ysb[:, c, :], K.ps[yb][:, :], AF.Identity, K.bps[yb] + [pk.b], [ysb.b], bias=conv_b[:, c:c + 1])
                K.act(ysq[:, c, :], K.ps[yb][:, :], AF.Square, K.bps[yb] + [pk.b], [ysq.b], bias=conv_b[:, c:c + 1])
                yield
            for c in range(4):
                K.mm(K.ps[6][:, :], onesf[:], ysb[:, c, :], c == 0, c == 3, [onesf.b, ysb.b], K.bps[6])
            for c in range(4):
                K.mm(K.ps[7][:, :], onesf[:], ysq[:, c, :], c == 0, c == 3, [onesf.b, ysq.b], K.bps[7])
            K.cp("act", mean_sb[:], K.ps[6][:, :], K.bps[6], [mean_sb.b])
            K.tt("dve", m2[:], mean_sb[:], mean_sb[:], ALU.mult, [mean_sb.b], [m2.b])
            K.tt("dve", var[:], K.ps[7][:, :], m2[:], ALU.subtract, K.bps[7] + [m2.b], [var.b])
            K.act(var[:], var[:], AF.Sqrt, [var.b], [var.b], bias=EPS)
            K.recip(rstdb[:], var[:], [var.b], [rstdb.b])
            yield
            for c in range(4):
                K.tt("dve", zt[:], ysb[:, c, :], mean_sb[:], ALU.subtract, [ysb.b, mean_sb.b], [zt.b])
                K.tt("dve", z2[:], zt[:], rstdb[:], ALU.mult, [zt.b, rstdb.b], [z2.b])
                K.act(cvo[:, c, :], z2[:], AF.Silu, [z2.b, pk.b], [cvo.b], scale=ln_g[:, c:c + 1], bias=ln_b[:, c:c + 1])
                yield
            K.dma(cvT_d[:, :, ci * TC:(ci + 1) * TC], cvo[:], reads=[cvo.b], writes=[bcv[ci]])

        def rr(gens):
            live = [g for g in gens if g is not None]
            while live:
                for g_ in list(live):
                    try:
                        next(g_)
                    except StopIteration:
                        live.remove(g_)

        rr([c_front(0)])
        for ci in range(NCHC):
            rr([c_back(ci), c_front(ci + 1) if ci + 1 < NCHC else None])
        A.release(m0)

    K.extra = dict(x_d=x_d, p_d=p_d, out_d=out_d, h1_d=h1_d, cvT_d=cvT_d, tabb_d=tabb_d, btab=btab, bh1=bh1, bcv=bcv,
                   rows_d=rows_d, w_in_d=w_in_d, w_out_d=w_out_d, wq_d=wq_d, keys_d=keys_d, wg_d=wg_d, wp_d=wp_d, rope_d=rope_d,
                   pk=pk, rows=rows, cst=cst, cstb=cstb, idb=idb, trib=trib, idf=idf, iota=iota, g_attn=g_attn,
                   g_ffn=g_ffn, g_ple=g_ple, g_fin=g_fin, subg=subg, lamv=lamv, onesf=onesf)
    if "A" in phases:
        phase_A(K)
    if "P" in phases:
        phase_P(K)
    if debug:
        if "C" in phases and "A" not in phases:
            o = K.dout("dbg_cvT", [128, 4, S], BF16)
            K.dma(o, cvT_d, reads=bcv)
        if "A" in phases and "P" not in phases:
            o = K.dout("dbg_h1", [S, D], F32)
            K.dma(o, h1_d, reads=bh1)
    P.emit()
    return nc


def phase_A(K):
    nc, P, A = K.nc, K.P, K.A
    X = K.extra
    x_d, w_in_d, w_out_d, rope_d, cvT_d, h1_d = X["x_d"], X["w_in_d"], X["w_out_d"], X["rope_d"], X["cvT_d"], X["h1_d"]
    pk, rows, cstb = X["pk"], X["rows"], X["cstb"]
    idb, trib, g_attn, subg, lamv = X["idb"], X["trib"], X["g_attn"], X["subg"], X["lamv"]
    bcv, bh1 = X["bcv"], X["bh1"]
    m0 = A.mark()
    wA = A.alloc([128, 8, 2560], BF16, "wA")
    wo = A.alloc([128, 8, 1024], BF16, "wo")
    kT = A.alloc([128, 4, S], BF16, "kT")
    bkT = kT.sub(NCH)
    Va = A.alloc([128, NT, 4, 130], BF16, "Va")
    bVa = Va.sub(NCH)
    lam = A.alloc([128, 8], F32, "lam")
    sg = A.alloc([128, 128], F32, "sg")
    ltmp = A.alloc([128, 128], F32, "ltmp")
    m1 = A.mark()
    stg = [A.alloc([128, 1536], F32, "stgA") for _ in range(2)]
    for k in range(8):
        s_ = stg[k % 2]
        K.dma(s_[:], w_in_d[k * 128:(k + 1) * 128, 0:1536], writes=[s_.b])
        K.cp("act", wA[:, k, 0:1536], s_[:], [s_.b], [wA.b])
        sv = s_[:, 0:1024].rearrange("p (j d) -> p j d", d=64)
        wr = wA[:, k, 1536:2560].rearrange("p (j d) -> p j d", d=64)
        K.ts("dve", wr[:, :, 0:32], sv[:, :, 32:64], -1.0, None, ALU.mult, None, [s_.b], [wA.b])
        K.cp("pool", wr[:, :, 32:64], sv[:, :, 0:32], [s_.b], [wA.b])
    stg2 = [A.alloc([128, 1024], F32, "stgO") for _ in range(2)]
    for k in range(8):
        s_ = stg2[k % 2]
        K.dma(s_[:], w_out_d[k * 128:(k + 1) * 128, :], writes=[s_.b])
        K.cp("act" if k % 2 else "dve", wo[:, k, :], s_[:], [s_.b], [wo.b])
    A.release(m1)
    K.tt("dve", ltmp[:, 0:64], lamv[:, 0:64], lamv[:, 64:128], ALU.mult, [rows.b], [ltmp.b])
    K.tt("dve", ltmp[:, 64:128], lamv[:, 128:192], lamv[:, 192:256], ALU.mult, [rows.b], [ltmp.b])
    K.P.add("dve", lambda e: e.tensor_reduce(out=lam[:, 0:2], in_=ltmp[:].rearrange("p (a b) -> p a b", a=2), axis=AX.X, op=ALU.add), [ltmp.b], [lam.b])
    K.act(lam[:, 2:4], lam[:, 0:2], AF.Exp, [lam.b], [lam.b])
    K.tt("dve", lam[:, 4:5], lam[:, 2:3], lam[:, 3:4], ALU.subtract, [lam.b], [lam.b])
    K.ts("dve", lam[:, 5:6], lam[:, 4:5], LAMBDA_INIT, -1.0, ALU.add, ALU.mult, [lam.b], [lam.b])
    K.ts("dve", sg[:], subg, 1.0 - LAMBDA_INIT, None, ALU.mult, None, [rows.b], [sg.b])
    neglam = lam[:, 5:6]
    K.memset("pool", Va[:], 1.0, bVa)

    import os
    LVL = int(os.environ.get('DBG_STOP', 9))
    xt = A.alloc([128, 4, 1024], F32, "xt")
    xn = A.alloc([128, 4, 1024], BF16, "xn")
    xnT = A.alloc([128, 8, 512], BF16, "xnT")
    ss = A.alloc([128, 24], F32, "ss")
    rp = [A.alloc([128, 2, 512], F32, "rp") for _ in range(1)]
    t1 = [A.alloc([128, 512], F32, "t1") for _ in range(1)]
    t2 = [A.alloc([128, 512], F32, "t2") for _ in range(1)]
    qTz = [A.alloc([128, 4, 512], BF16, "qTz") for _ in range(2)]
    bqT = [qTz[0].sub(4), qTz[1].sub(4)]
    K.memset("pool", qTz[0][64:128, :, :], 0.0, bqT[0])
    K.memset("pool", qTz[1][0:64, :, :], 0.0, bqT[1])
    PT = [A.alloc([128, 512], BF16, "PT") for _ in range(4)]
    att = A.alloc([128, 128], F32, "att")
    a1 = A.alloc([128, 128], F32, "a1")
    sm = A.alloc([128, 4, 8], F32, "sm")
    Osb = A.alloc([128, 8, 129], F32, "Osb")
    attn_sb = A.alloc([128, 4, 512], BF16, "attn_sb")
    catT = A.alloc([128, 8, 512], BF16, "catT")
    h1o = [A.alloc([128, 1024], F32, "h1o") for _ in range(2)]
    cnt = 0
    tcnt = 0
    import os
    for ci in range(int(os.environ.get('DBG_NCH', NCH))):
        if LVL < 2:
            break
        load_norm_T(K, x_d[ci * TC:(ci + 1) * TC, :], xt, xn, xnT, ss, None, g_attn, idb, cstb.b, pk.b, [0, 1], 4)
        rpc = rp[0]
        K.dma(rpc[:], rope_d[:, :, ci * TC:(ci + 1) * TC], writes=[rpc.b])
        K.dma(catT[:, 4:8, :], cvT_d[:, :, ci * TC:(ci + 1) * TC], reads=[bcv[ci]], writes=[catT.b])
        cos, sin = rpc[:, 0, :], rpc[:, 1, :]
        for h in range(4):
            for which in range(2):
                c0 = which * 512 + h * 128
                for k in range(8):
                    K.mm(K.ps[0][:, :], wA[:, k, c0:c0 + 128], xnT[:, k, :], k == 0, k == 7, [wA.b, xnT.b], K.bps[0])
                for k in range(8):
                    K.mm(K.ps[1][:, :], wA[:, k, 1536 + c0:1536 + c0 + 128], xnT[:, k, :], k == 0, k == 7, [wA.b, xnT.b], K.bps[1])
                ta, tb = t1[0], t2[0]
                tcnt += 1
                K.tt("dve", ta[:], K.ps[0][:, :], cos, ALU.mult, K.bps[0] + [rpc.b], [ta.b])
                K.tt("dve", tb[:], K.ps[1][:, :], sin, ALU.mult, K.bps[1] + [rpc.b], [tb.b])
                if which == 0:
                    K.tt("pool", qTz[0][0:64, h, :], ta[0:64, :], tb[0:64, :], ALU.add, [ta.b, tb.b], [bqT[0][h]])
                    K.tt("pool", qTz[1][64:128, h, :], ta[64:128, :], tb[64:128, :], ALU.add, [ta.b, tb.b], [bqT[1][h]])
                else:
                    K.tt("pool", kT[:, h, ci * TC:(ci + 1) * TC], ta[:], tb[:], ALU.add, [ta.b, tb.b], [bkT[ci]])
        if LVL < 3:
            continue
        for tt_ in range(4):
            bk = tt_ % 2
            for k in range(8):
                K.mm(K.ps[bk][:, :], xnT[:, k, tt_ * 128:(tt_ + 1) * 128], wA[:, k, 1024:1536], k == 0, k == 7, [wA.b, xnT.b], K.bps[bk])
            K.P.add("act", lambda e, bk=bk, tt_=tt_, ci=ci: e.copy(out=Va[:, ci * 4 + tt_, :, 0:128], in_=K.ps[bk][:, :].rearrange("p (h d) -> p h d", d=128)),
                    K.bps[bk], [bVa[ci]])
        if LVL < 4:
            continue
        items = [(h, c, kt) for h in range(4) for c in range(2) for kt in range(4 * ci + 4)]
        DP = 3

        def emit_qk(h, c, kt, idx):
            r = kt - 4 * ci
            c0 = max(r, 0) * 128
            sbk = idx % 4
            pt = PT[idx % 4]
            K.mm(K.ps[sbk][:, c0:512], kT[:, h, kt * 128:(kt + 1) * 128], qTz[c][:, h, c0:512],
                 True, True, [bkT[kt // 4], bqT[c][h]], K.bps[sbk])
            K.act(pt[:, c0:512], K.ps[sbk][:, c0:512], AF.Exp, K.bps[sbk], [pt.b], scale=0.125)
            if r >= 0:
                K.tt("dve", pt[:, c0:c0 + 128], pt[:, c0:c0 + 128], trib, ALU.mult, [pt.b, cstb.b], [pt.b])

        def emit_pv(h, c, kt, idx):
            r = kt - 4 * ci
            pt = PT[idx % 4]
            for tqi in range(max(r, 0), 4):
                a = c * 4 + tqi
                bank, col = 4 + a // 2, (a % 2) * 256
                K.mm(K.ps[bank][:, col:col + 129], pt[:, tqi * 128:(tqi + 1) * 128], Va[:, kt, h, 0:129],
                     kt == 0 and a % 2 == 0, kt == 4 * ci + tqi, [pt.b, bVa[kt // 4]], K.bps[bank], skip=True)
            if c == 1 and kt == 4 * ci + 3:
                post(h)

        def post(h):
            if LVL >= 5:
                for bi in range(4):
                    src = K.ps[4 + bi][:, :].rearrange("p (a b) -> p a b", b=256)[:, :, 0:129]
                    K.cp("act" if bi % 2 == 0 else "dve", Osb[:, 2 * bi:2 * bi + 2, :], src, K.bps[4 + bi], [Osb.b])
            for tqi in range(4):
                if LVL < 5:
                    continue
                O1 = Osb[:, tqi, :]
                O2 = Osb[:, 4 + tqi, :]
                b1 = [Osb.b]
                b2 = [Osb.b]
                SUB = int(os.environ.get("DBG_SUB", 9))
                K.recip(sm[:, tqi, 0:1], O1[:, 128:129], b1, [sm.b])
                K.recip(sm[:, tqi, 1:2], O2[:, 128:129], b2, [sm.b])
                if SUB < 2:
                    continue
                K.tt("dve", sm[:, tqi, 2:3], sm[:, tqi, 1:2], neglam, ALU.mult, [sm.b, lam.b], [sm.b])
                K.act(a1[:], O1[:, 0:128], AF.Copy, b1 + [sm.b], [a1.b], scale=sm[:, tqi, 0:1])
                if SUB < 3:
                    continue
                K.stt(att[:], O2[:, 0:128], sm[:, tqi, 2:3], a1[:], ALU.mult, ALU.add, b2 + [sm.b, a1.b], [att.b])
                if SUB < 4:
                    continue
                K.act(a1[:], att[:], AF.Square, [att.b], [a1.b, sm.b], accum_out=sm[:, tqi, 3:4])
                if SUB < 5:
                    continue
                K.ts("pool", sm[:, tqi, 4:5], sm[:, tqi, 3:4], 1.0 / 128, EPS, ALU.mult, ALU.add, [sm.b], [sm.b])
                K.tt("pool", sm[:, tqi, 5:6], sm[:, tqi, 4:5], K.mhalf[:, 0:1], ALU.pow, [sm.b, K.mhalf.b], [sm.b])
                if SUB < 6:
                    continue
                K.stt(attn_sb[:, tqi, h * 128:(h + 1) * 128], att[:], sm[:, tqi, 5:6], sg[:], ALU.mult, ALU.mult, [att.b, sm.b, sg.b], [attn_sb.b])
        for idx in range(len(items) + DP):
            if idx < len(items):
                emit_qk(*items[idx], cnt + idx)
            if idx >= DP:
                emit_pv(*items[idx - DP], cnt + idx - DP)
        cnt += len(items)
        if LVL < 6:
            continue
        for tqi in range(4):
            bk = tqi % 2
            for h in range(4):
                K.tr(K.psb[bk][:, h * 128:(h + 1) * 128], attn_sb[:, tqi, h * 128:(h + 1) * 128], idb, [attn_sb.b, cstb.b], K.bps[bk])
            if os.environ.get("DBG_V", "0") == "1":
                continue
            if os.environ.get("DBG_V", "0") == "2":
                for h in range(4):
                    K.cp("dve", catT[:, h, tqi * 128:(tqi + 1) * 128], K.psb[bk][:, h * 128:(h + 1) * 128], K.bps[bk], [catT.b])
                continue
            K.cp("dve", catT[:, 0:4, tqi * 128:(tqi + 1) * 128], K.psb[bk][:, 0:512].rearrange("p (h d) -> p h d", d=128), K.bps[bk], [catT.b])
        for tqi in range(4):
            if LVL < 7:
                continue
            ho = h1o[tqi % 2]
            for half in range(2):
                for k in range(8):
                    K.mm(K.ps[half][:, :], catT[:, k, tqi * 128:(tqi + 1) * 128], wo[:, k, half * 512:(half + 1) * 512], k == 0, k == 7, [catT.b, wo.b], K.bps[half])
                K.tt("dve", ho[:, half * 512:(half + 1) * 512], K.ps[half][:, :], xt[:, tqi, half * 512:(half + 1) * 512], ALU.add, K.bps[half] + [xt.b], [ho.b])
            ti = ci * 4 + tqi
            if LVL < 8:
                continue
            K.dma(h1_d[ti * 128:(ti + 1) * 128, :], ho[:], reads=[ho.b], writes=[bh1[ti]])
    A.release(m0)


def phase_P(K):
    import os
    nc, P, A = K.nc, K.P, K.A
    X = K.extra
    h1_d, out_d, p_d, wq_d, keys_d, wg_d, wp_d, tabb_d = X["h1_d"], X["out_d"], X["p_d"], X["wq_d"], X["keys_d"], X["wg_d"], X["wp_d"], X["tabb_d"]
    pk, rows, cst, cstb = X["pk"], X["rows"], X["cst"], X["cstb"]
    idb, idf, iota, g_ffn, g_ple, g_fin = X["idb"], X["idf"], X["iota"], X["g_ffn"], X["g_ple"], X["g_fin"]
    bh1, btab = X["bh1"], X["btab"]
    mT_d = K.dscr("mTs", [128, 8, S], BF16)
    pk_s = K.dscr("picks", [S, 3, 128], F32)
    bmT = [Buf("mTs%d" % i) for i in range(NPC)]
    bpk = [Buf("pks%d" % i) for i in range(NPC)]
    NPCR = int(os.environ.get("DBG_NPC", NPC))
    SUBP = os.environ.get("DBG_SUBP", "abc")

    sc_d = K.dscr("scs", [S, 2048], F32)
    bscd = [Buf("scs%d" % i) for i in range(NT)]
    bpkt = [Buf("pkt%d" % i) for i in range(NT)]
    NTR = NPCR * 2
    K.tk_done = 0
    B4 = [128, 8, 16, 16]
    iota16 = iota[:, 0:16].unsqueeze(1).unsqueeze(1).to_broadcast(B4)

    def alloc_topk_ws():
        w = dict(sc=A.alloc([128, 16, 128], F32, "sc"), sc2=A.alloc([128, 16, 128], F32, "sc2"),
                 sv=A.alloc([128, 16, 16], F32, "sv"), si=A.alloc([128, 16, 16], U32, "si"), sif=A.alloc([128, 16, 16], F32, "sif"),
                 cs=A.alloc([128, 8, 16], F32, "cs"), ci=A.alloc([128, 8, 16], U32, "ci"),
                 abu=A.alloc([128, 2, 8, 16], U32, "abu"), abf=A.alloc([128, 2, 8, 16], F32, "abf"),
                 pkt=A.alloc([128, 3, 128], F32, "pkt"),
                 ex=A.alloc([128, 8, 16], F32, "ex"), rs=A.alloc([128, 16], F32, "rs"))
        w["bsc"] = w["sc"].sub(4)
        w["bsv"] = w["sv"].sub(16)
        w["bsi"] = w["si"].sub(16)
        w["bsc2"] = w["sc2"].sub(16)
        w["bcs"] = w["cs"].sub(8)
        w["bci"] = w["ci"].sub(8)
        return w

    def topk_steps(ti, w):
        sc, sc2, sv, si, sif, cs, ci = w["sc"], w["sc2"], w["sv"], w["si"], w["sif"], w["cs"], w["ci"]
        abu, abf, pkt, ex, rs = w["abu"], w["abf"], w["pkt"], w["ex"], w["rs"]
        bsc, bsv, bsi, bsc2, bcs, bci = w["bsc"], w["bsv"], w["bsi"], w["bsc2"], w["bcs"], w["bci"]
        cand = sc[:].rearrange("p (h c) n -> p h (c n)", c=2)
        cand2 = sc2[:].rearrange("p (h c) n -> p h (c n)", c=2)
        eq = sc[:].rearrange("p (h c) (a b) -> p h (c a) b", c=2, b=16)
        svv = sv[:].rearrange("p (h c) k -> p h c k", c=2)
        sifv = sif[:].rearrange("p (h c) k -> p h c k", c=2)
        K.dma(sc[:].rearrange("p g n -> p (g n)"), sc_d[ti * 128:(ti + 1) * 128, :], reads=[bscd[ti]], writes=bsc)
        yield
        for g in range(16):
            K.P.add("dve", lambda e, g=g: e.max(out=sv[:, g, 0:8], in_=sc[:, g, :]), [bsc[g // 4]], [bsv[g]])
            if g % 3 == 2:
                yield
        yield
        for g in range(16):
            K.P.add("dve", lambda e, g=g: e.max_index(out=si[:, g, 0:8], in_max=sv[:, g, 0:8], in_values=sc[:, g, :]), [bsc[g // 4], bsv[g]], [bsi[g]])
            if g % 3 == 2:
                yield
        yield
        for g in range(16):
            K.P.add("dve", lambda e, g=g: e.match_replace(out=sc2[:, g, :], in_to_replace=sv[:, g, 0:8], in_values=sc[:, g, :], imm_value=-1e30), [bsc[g // 4], bsv[g]], [bsc2[g]])
            if g % 3 == 2:
                yield
        yield
        for g in range(16):
            K.P.add("dve", lambda e, g=g: e.max(out=sv[:, g, 8:16], in_=sc2[:, g, :]), [bsc2[g]], [bsv[g]])
            if g % 3 == 2:
                yield
        yield
        for g in range(16):
            K.P.add("dve", lambda e, g=g: e.max_index(out=si[:, g, 8:16], in_max=sv[:, g, 8:16], in_values=sc2[:, g, :]), [bsc2[g], bsv[g]], [bsi[g]])
            if g % 3 == 2:
                yield
        yield
        K.cp("dve", sif[:], si[:], bsi, [sif.b])
        K.tt("dve", cand.rearrange("p h (a b) -> p h a b", b=16), svv[:, :, 0, :].unsqueeze(3).to_broadcast(B4),
             svv[:, :, 1, :].unsqueeze(2).to_broadcast(B4), ALU.add, bsv, bsc)
        yield
        for h in range(8):
            K.P.add("dve", lambda e, h=h: e.max(out=cs[:, h, 0:8], in_=cand[:, h, :]), [bsc[h // 2]], [bcs[h]])
            if h % 3 == 2:
                yield
        yield
        for h in range(8):
            K.P.add("dve", lambda e, h=h: e.max_index(out=ci[:, h, 0:8], in_max=cs[:, h, 0:8], in_values=cand[:, h, :]), [bsc[h // 2], bcs[h]], [bci[h]])
            if h % 3 == 2:
                yield
        for h in range(8):
            K.P.add("dve", lambda e, h=h: e.match_replace(out=cand2[:, h, :], in_to_replace=cs[:, h, 0:8], in_values=cand[:, h, :], imm_value=-1e30),
                    [bsc[h // 2], bcs[h]], [bsc2[2 * h], bsc2[2 * h + 1]])
            if h % 3 == 2:
                yield
        yield
        for h in range(8):
            K.P.add("dve", lambda e, h=h: e.max(out=cs[:, h, 8:16], in_=cand2[:, h, :]), [bsc2[2 * h], bsc2[2 * h + 1]], [bcs[h]])
            if h % 3 == 2:
                yield
        yield
        for h in range(8):
            K.P.add("dve", lambda e, h=h: e.max_index(out=ci[:, h, 8:16], in_max=cs[:, h, 8:16], in_values=cand2[:, h, :]), [bsc2[2 * h], bsc2[2 * h + 1], bcs[h]], [bci[h]])
            if h % 3 == 2:
                yield
        yield
        K.P.add("dve", lambda e: e.tensor_single_scalar(out=abu[:, 0, :, :], in_=ci[:], scalar=4, op=ALU.logical_shift_right), bci, [abu.b])
        K.P.add("dve", lambda e: e.tensor_single_scalar(out=abu[:, 1, :, :], in_=ci[:], scalar=15, op=ALU.bitwise_and), bci, [abu.b])
        K.cp("dve", abf[:], abu[:], [abu.b], [abf.b])
        K.tt("dve", ex[:], cs[:], cs[:, :, 0:1].to_broadcast([128, 8, 16]), ALU.subtract, bcs, [ex.b])
        yield
        for a in range(2):
            K.tt("dve", eq, iota16, abf[:, a, :, :].unsqueeze(3).to_broadcast(B4), ALU.is_equal, [cst.b, abf.b], bsc)
            yield
            K.tt("dve", eq, eq, sifv[:, :, a, :].unsqueeze(2).to_broadcast(B4), ALU.mult, bsc + [sif.b], bsc)
            yield
            if a == 0:
                K.act(ex[:], ex[:], AF.Exp, [ex.b], [ex.b])
            K.P.add("dve", lambda e, a=a: e.tensor_reduce(out=pkt[:, a, :].rearrange("p (h k) -> p h k", k=16), in_=eq, axis=AX.X, op=ALU.add), bsc, [pkt.b])
            yield
        K.P.add("dve", lambda e: e.tensor_reduce(out=rs[:, 0:8], in_=ex[:], axis=AX.X, op=ALU.add), [ex.b], [rs.b])
        K.recip(rs[:, 8:16], rs[:, 0:8], [rs.b], [rs.b])
        K.tt("dve", pkt[:, 2, :].rearrange("p (h k) -> p h k", k=16), ex[:], rs[:, 8:16].unsqueeze(2).to_broadcast([128, 8, 16]), ALU.mult,
             [ex.b, rs.b], [pkt.b])
        K.dma(pk_s[ti * 128:(ti + 1) * 128], pkt[:], reads=[pkt.b], writes=[bpkt[ti]])
        yield

    def drain(g_):
        if g_ is not None:
            for _ in g_:
                pass

    def chain(*gs):
        for g_ in gs:
            yield from g_

    if "a" in SUBP:
        m0 = A.mark()
        wqb = A.alloc([128, 8, 2048], BF16, "wqb")
        keysT = A.alloc([128, 2, 128], BF16, "keysT")
        m1 = A.mark()
        stg = [A.alloc([128, 2048], F32, "stgq") for _ in range(2)]
        for k in range(8):
            s_ = stg[k % 2]
            K.dma(s_[:], wq_d[k * 128:(k + 1) * 128, :], writes=[s_.b])
            K.cp("act" if k % 2 else "dve", wqb[:, k, :], s_[:], [s_.b], [wqb.b])
        kst = A.alloc([128, 2, 128], F32, "kst")
        K.dma(kst[:], keys_d.rearrange("c n d -> n c d"), writes=[kst.b])
        for c in range(2):
            K.tr(K.ps[0][:, c * 128:(c + 1) * 128], kst[:, c, :], idf, [kst.b, cst.b], K.bps[0])
        K.cp("act", keysT[:], K.ps[0][:, 0:256].rearrange("p (c n) -> p c n", c=2), K.bps[0], [keysT.b])
        A.release(m1)
        ss = A.alloc([128, 24], F32, "ss")
        junk = A.alloc([128, 1024], BF16, "junk")
        CH = []
        for _ in range(2):
            CH.append(dict(ht=A.alloc([128, 2, 1024], F32, "ht"), hn=A.alloc([128, 2, 1024], BF16, "hn"),
                           mT=A.alloc([128, 8, PC], BF16, "mT"), qpT=A.alloc([128, 16, PC], BF16, "qpT")))
        scb = [A.alloc([128, 16, 128], F32, "scb") for _ in range(2)]

        def chunk_front(pc):
            c_ = CH[pc % 2]
            ht, hn, mT, qpT = c_["ht"], c_["hn"], c_["mT"], c_["qpT"]
            load_norm_T(K, h1_d[pc * PC:(pc + 1) * PC, :], ht, hn, mT, ss, junk, g_ffn, idb, cstb.b, pk.b, [0, 1], 2,
                        rd=bh1[pc * 2:pc * 2 + 2])
            K.dma(mT_d[:, :, pc * PC:(pc + 1) * PC], mT[:], reads=[mT.b], writes=[bmT[pc]])
            yield
            for g in range(16):
                bk = 2 + g % 2
                for k in range(8):
                    K.mm(K.ps[bk][:, 0:PC], wqb[:, k, g * 128:(g + 1) * 128], mT[:, k, :], k == 0, k == 7, [wqb.b, mT.b], K.bps[bk])
                K.cp("act" if g % 2 else "dve", qpT[:, g, :], K.ps[bk][:, 0:PC], K.bps[bk], [qpT.b])
                if g % 4 == 3:
                    yield

        def chunk_scores(pc):
            qpT = CH[pc % 2]["qpT"]
            for tt_ in range(2):
                sct = scb[tt_]
                for g in range(16):
                    bk = 4 + g // 4
                    K.mm(K.ps[bk][:, (g % 4) * 128:(g % 4 + 1) * 128], qpT[:, g, tt_ * 128:(tt_ + 1) * 128], keysT[:, g % 2, :], True, True,
                         [qpT.b, keysT.b], K.bps[bk], skip=True)
                yield
                for q4 in range(4):
                    K.cp("act" if q4 % 2 else "dve", sct[:, q4 * 4:q4 * 4 + 4, :], K.ps[4 + q4][:, :].rearrange("p (g n) -> p g n", n=128), K.bps[4 + q4], [sct.b])
                ti = pc * 2 + tt_
                K.dma(sc_d[ti * 128:(ti + 1) * 128, :], sct[:].rearrange("p g n -> p (g n)"), reads=[sct.b], writes=[bscd[ti]])
                yield

        def rr3(gens):
            live = [g for g in gens if g is not None]
            while live:
                for g_ in list(live):
                    try:
                        next(g_)
                    except StopIteration:
                        live.remove(g_)

        NEARLY = min(4, NTR)
        WSa = alloc_topk_ws()

        def early_topk():
            inner = chain(*[topk_steps(ti, WSa) for ti in range(NEARLY)])
            while True:
                for _ in range(3):
                    try:
                        next(inner)
                    except StopIteration:
                        return
                yield

        etk = None
        rr3([chunk_front(0)])
        for pc in range(NPCR):
            if pc == 2:
                etk = early_topk()
            rr3([chunk_scores(pc), chunk_front(pc + 1) if pc + 1 < NPCR else None])
            if etk is not None:
                for _ in range(6):
                    next(etk, None)
        if NPCR <= 2:
            etk = early_topk()
        drain(etk)
        K.tk_done = NEARLY
        A.release(m0)

    if "b" in SUBP:
        m0 = A.mark()
        NB = 6
        G = [A.alloc([128, PC, 128], BF16, "G") for _ in range(2)]
        wt = [A.alloc([128, 2048], BF16, "wt") for _ in range(NB)]
        mTb = [A.alloc([128, 8, PC], BF16, "mTb") for _ in range(1)]
        pks = [A.alloc([128, 3, PC], F32, "pks") for _ in range(2)]
        npk = [A.alloc([128, 2, PC], F32, "npk") for _ in range(2)]
        ht = A.alloc([128, 512], F32, "htb")
        Ast = [A.alloc([128, 4, 128], BF16, "Ast") for _ in range(2)]
        Bst = [A.alloc([128, 4, 128], BF16, "Bst") for _ in range(2)]
        tmpa = [A.alloc([128, 128], F32, "tmpa") for _ in range(1)]
        ge = [A.alloc([128, PC], BF16, "ge") for _ in range(3)]
        Hs = [A.alloc([128, PC], BF16, "Hs") for _ in range(4)]
        WS = alloc_topk_ws()
        pkl = A.alloc([128, 2, 3, 128], F32, "pkl")
        iotaB = iota.unsqueeze(1).to_broadcast([128, 4, 128])

        def gbuild(pc):
            Gc, pk_, nk = G[pc % 2], pks[pc % 2], npk[pc % 2]
            K.dma(pkl[:], pk_s[pc * PC:(pc + 1) * PC].rearrange("(tt p) a k -> p tt a k", p=128), reads=bpkt[2 * pc:2 * pc + 2], writes=[pkl.b])
            yield
            for tt_ in range(2):
                for a in range(3):
                    K.tr(K.ps[7][:, a * 128:(a + 1) * 128], pkl[:, tt_, a, :], idf, [pkl.b, cst.b], K.bps[7])
                yield
                K.cp("act", pk_[:, :, tt_ * 128:(tt_ + 1) * 128], K.ps[7][:, 0:384].rearrange("p (a t) -> p a t", a=3), K.bps[7], [pk_.b])
                yield
            K.ts("dve", nk[:, 0, :], pk_[:, 0, :], -1.0, None, ALU.mult, None, [pk_.b], [nk.b])
            K.ts("dve", nk[:, 1, :], pk_[:, 2, :], -1.0, None, ALU.mult, None, [pk_.b], [nk.b])
            yield
            for grp in range(PC // 4 + 1):
                if grp < PC // 4:
                    As, Bs = Ast[grp % 2], Bst[grp % 2]
                    for tl in range(4):
                        t = grp * 4 + tl
                        if tl % 2 == 0:
                            K.ts("dve", As[:, tl, :], iota, pk_[:, 0, t:t + 1], pk_[:, 2, t:t + 1], ALU.is_equal, ALU.mult, [cst.b, pk_.b], [As.b])
                        else:
                            ta = tmpa[0]
                            K.act(ta[:], iota, AF.Abs, [cst.b, nk.b], [ta.b], bias=nk[:, 0, t:t + 1])
                            K.act(As[:, tl, :], ta[:], AF.Relu, [ta.b, nk.b, pk_.b], [As.b], scale=nk[:, 1, t:t + 1], bias=pk_[:, 2, t:t + 1])
                    K.tt("dve", Bs[:], iotaB, pk_[:, 1, grp * 4:grp * 4 + 4].unsqueeze(2).to_broadcast([128, 4, 128]), ALU.is_equal, [cst.b, pk_.b], [Bs.b])
                if grp >= 1:
                    g1 = grp - 1
                    As, Bs = Ast[g1 % 2], Bst[g1 % 2]
                    for tl in range(4):
                        K.mm(K.ps[7][:, tl * 128:(tl + 1) * 128], As[:, tl, :], Bs[:, tl, :], True, True, [As.b, Bs.b], K.bps[7], skip=True)
                    K.cp("act", Gc[:, g1 * 4:g1 * 4 + 4, :], K.ps[7][:, :].rearrange("p (t j) -> p t j", j=128), K.bps[7], [Gc.b])
                yield

        LEAD = 3

        def wdma(g):
            jw = g % 128
            K.dma(wt[g % NB][:], tabb_d[jw], reads=[btab[jw]], writes=[wt[g % NB].b])

        def dense(pc, nxt, nxt2):
            Gc, mT = G[pc % 2], mTb[0]
            K.dma(mT[:], mT_d[:, :, pc * PC:(pc + 1) * PC], reads=[bmT[pc]], writes=[mT.b])
            for j in range(130):
                if j < 128:
                    g0 = pc * 128 + j
                    if g0 + LEAD < NPCR * 128:
                        wdma(g0 + LEAD)
                    w_ = wt[g0 % NB]
                    ab = 4 + j % 3
                    for k in range(8):
                        K.mm(K.ps[ab][:, 0:PC], w_[:, k * 128:(k + 1) * 128], mT[:, k, :], k == 0, k == 7, [w_.b, mT.b], K.bps[ab])
                    g_, h_ = ge[j % 3], Hs[j % 4]
                    K.act(g_[:], K.ps[ab][:, 0:PC], AF.Gelu, K.bps[ab], [g_.b])
                    K.tt("pool", h_[:], g_[:], Gc[:, :, j], ALU.mult, [g_.b, Gc.b], [h_.b])
                if j >= 2:
                    jj = j - 2
                    w_, h_ = wt[(pc * 128 + jj) % NB], Hs[jj % 4]
                    for tt_ in range(2):
                        for half in range(2):
                            yb = tt_ * 2 + half
                            K.mm(K.ps[yb][:, :], h_[:, tt_ * 128:(tt_ + 1) * 128], w_[:, 1024 + half * 512:1024 + (half + 1) * 512], jj == 0, jj == 127,
                                 [h_.b, w_.b], K.bps[yb])
                if nxt is not None and j % 2 == 1:
                    next(nxt, None)
                if nxt2 is not None:
                    next(nxt2, None)
            for tt_ in range(2):
                ti = pc * 2 + tt_
                for half in range(2):
                    yb = tt_ * 2 + half
                    hsl = h1_d[ti * 128:(ti + 1) * 128, half * 512:(half + 1) * 512]
                    K.dma(ht[:], hsl, reads=[bh1[ti]], writes=[ht.b])
                    K.tt("dve", ht[:], K.ps[yb][:, :], ht[:], ALU.add, K.bps[yb] + [ht.b], [ht.b])
                    K.dma(hsl, ht[:], reads=[ht.b], writes=[bh1[ti]])

        def topk_chunk(pc):
            if pc >= NPCR:
                return None
            tiles = [ti for ti in (2 * pc, 2 * pc + 1) if ti >= K.tk_done]
            if not tiles:
                return None
            return chain(*[topk_steps(ti, WS) for ti in tiles])

        drain(topk_chunk(0))
        drain(topk_chunk(1))
        drain(gbuild(0))
        for g_ in range(LEAD):
            wdma(g_)
        for pc in range(NPCR):
            nxt = gbuild(pc + 1) if pc + 1 < NPCR else None
            nxt2 = topk_chunk(pc + 2)
            dense(pc, nxt, nxt2)
            drain(nxt2)
            drain(nxt)
        A.release(m0)

    if "c" in SUBP:
        m0 = A.mark()
        wgb = A.alloc([128, 8, 1024], BF16, "wgb")
        wpb = A.alloc([128, 2, 1024], BF16, "wpb")
        m1 = A.mark()
        stg = [A.alloc([128, 1024], F32, "stgg") for _ in range(2)]
        for k in range(8):
            s_ = stg[k % 2]
            K.dma(s_[:], wg_d[k * 128:(k + 1) * 128, :], writes=[s_.b])
            K.cp("act" if k % 2 else "dve", wgb[:, k, :], s_[:], [s_.b], [wgb.b])
        for k in range(2):
            s_ = stg[k % 2]
            K.dma(s_[:], wp_d[k * 128:(k + 1) * 128, :], writes=[s_.b])
            K.cp("act" if k % 2 else "dve", wpb[:, k, :], s_[:], [s_.b], [wpb.b])
        A.release(m1)
        ss = A.alloc([128, 24], F32, "ssc")
        junk = A.alloc([128, 1024], BF16, "junkc")
        CHc = []
        for _ in range(2):
            CHc.append(dict(ht=A.alloc([128, 2, 1024], F32, "htc"), hn=A.alloc([128, 2, 1024], BF16, "hnc"), mT=A.alloc([128, 8, PC], BF16, "mTc"),
                            pt=A.alloc([128, 2, 256], F32, "pt"), pb=A.alloc([128, 2, 256], BF16, "pb"), pT=A.alloc([128, 2, PC], BF16, "pT")))
        gate = [A.alloc([128, 512], F32, "gate") for _ in range(4)]
        tmp = [A.alloc([128, 512], F32, "tmp") for _ in range(4)]
        ot = [A.alloc([128, 1024], F32, "ot") for _ in range(2)]
        s3 = A.alloc([128, 8], F32, "s3")
        gfin = A.alloc([128, 1024], F32, "gfin")
        K.dma(gfin[:], X["rows_d"][:, 0:1024], writes=[gfin.b])

        def pc_front(pc):
            c_ = CHc[pc % 2]
            ht, hn, mT, pt_, pb_, pT = c_["ht"], c_["hn"], c_["mT"], c_["pt"], c_["pb"], c_["pT"]
            load_norm_T(K, h1_d[pc * PC:(pc + 1) * PC, :], ht, hn, mT, ss, junk, g_ple, idb, cstb.b, pk.b, [0, 1], 2, rd=bh1[pc * 2:pc * 2 + 2])
            yield
            K.dma(pt_[:], p_d[pc * PC:(pc + 1) * PC, :].rearrange("(tt p) d -> p tt d", p=128), writes=[pt_.b])
            K.cp("pool", pb_[:], pt_[:], [pt_.b], [pb_.b])
            for kk in range(2):
                for tt_ in range(2):
                    K.tr(K.psb[2][:, kk * PC + tt_ * 128:kk * PC + (tt_ + 1) * 128], pb_[:, tt_, kk * 128:(kk + 1) * 128], idb, [pb_.b, cstb.b], K.bps[2])
            K.cp("act", pT[:], K.psb[2][:, 0:2 * PC].rearrange("p (k t) -> p k t", k=2), K.bps[2], [pT.b])
            yield

        def pc_back(pc):
            c_ = CHc[pc % 2]
            ht, mT, pT = c_["ht"], c_["mT"], c_["pT"]
            for tt_ in range(2):
                for half in range(2):
                    gbk, ebk = 4 + half, 6 + half
                    gt_, tm_ = gate[tt_ * 2 + half], tmp[tt_ * 2 + half]
                    for k in range(8):
                        K.mm(K.ps[gbk][:, :], mT[:, k, tt_ * 128:(tt_ + 1) * 128], wgb[:, k, half * 512:(half + 1) * 512], k == 0, k == 7, [mT.b, wgb.b], K.bps[gbk])
                    K.act(gt_[:], K.ps[gbk][:, :], AF.Sigmoid, K.bps[gbk], [gt_.b])
                    for kk in range(2):
                        K.mm(K.ps[ebk][:, :], pT[:, kk, tt_ * 128:(tt_ + 1) * 128], wpb[:, kk, half * 512:(half + 1) * 512], kk == 0, kk == 1, [pT.b, wpb.b], K.bps[ebk])
                    K.tt("dve", tm_[:], K.ps[ebk][:, :], gt_[:], ALU.mult, K.bps[ebk] + [gt_.b], [tm_.b])
                    K.tt("dve", ht[:, tt_, half * 512:(half + 1) * 512], tm_[:], ht[:, tt_, half * 512:(half + 1) * 512], ALU.add, [tm_.b, ht.b], [ht.b])
                    yield
                o_ = ot[tt_]
                K.act(junk[:], ht[:, tt_, :], AF.Square, [ht.b], [junk.b, s3.b], accum_out=s3[:, 0:1])
                K.ts("pool", s3[:, 1:2], s3[:, 0:1], 1.0 / D, EPS, ALU.mult, ALU.add, [s3.b], [s3.b])
                K.tt("pool", s3[:, 2:3], s3[:, 1:2], K.mhalf[:, 0:1], ALU.pow, [s3.b, K.mhalf.b], [s3.b])
                K.stt(o_[:], ht[:, tt_, :], s3[:, 2:3], gfin[:], ALU.mult, ALU.mult, [ht.b, s3.b, gfin.b], [o_.b])
                r0 = pc * PC + tt_ * 128
                K.dma(out_d[r0:r0 + 128, :], o_[:], reads=[o_.b])
                yield

        def rr2(gens):
            live = [g for g in gens if g is not None]
            while live:
                for g_ in list(live):
                    try:
                        next(g_)
                    except StopIteration:
                        live.remove(g_)

        rr2([pc_front(0)])
        for pc in range(NPCR):
            rr2([pc_back(pc), pc_front(pc + 1) if pc + 1 < NPCR else None])
        A.release(m0)


def _consts():
    ident = np.eye(128, dtype=np.float32)
    tri = (np.arange(128)[:, None] <= np.arange(128)[None, :]).astype(np.float32)
    iota = np.broadcast_to(np.arange(128, dtype=np.float32)[None, :], (128, 128))
    cst = np.ascontiguousarray(np.stack([ident, tri, iota], axis=1))
    half = 32
    inv_freq = (1.0 / (10000.0 ** (np.arange(half, dtype=np.float32) * 2.0 / 64))).astype(np.float32)
    ang = np.arange(S, dtype=np.float32)[:, None] * inv_freq[None, :]
    cos, sin = np.cos(ang).astype(np.float32), np.sin(ang).astype(np.float32)
    f = (np.arange(128) % 64) % 32
    rope = np.ascontiguousarray(np.stack([cos[:, f].T, sin[:, f].T], axis=1))
    return cst, rope


def prep_inputs(inp):
    l = 0
    f32 = lambda a: np.ascontiguousarray(np.asarray(a, dtype=np.float32))
    pkv = np.zeros((128, 160), np.float32)
    pkv[:, 0:8] = f32(inp["attn_norm_g"])[l].reshape(8, 128).T
    pkv[:, 8:16] = f32(inp["ffn_norm_g"])[l].reshape(8, 128).T
    pkv[:, 16:24] = f32(inp["ple_norm_g"])[l].reshape(8, 128).T
    cw = f32(inp["conv_w"])[l]
    pkv[:, 24:148] = cw.reshape(31, 4, 128).transpose(2, 1, 0).reshape(128, 124)
    pkv[:, 148:152] = f32(inp["conv_b"])[l].reshape(4, 128).T
    pkv[:, 152:156] = f32(inp["conv_ln_g"])[l].reshape(4, 128).T
    pkv[:, 156:160] = f32(inp["conv_ln_b"])[l].reshape(4, 128).T
    row = np.concatenate([f32(inp["final_norm_g"]), f32(inp["subln_g"])[l], f32(inp["lambda_q1"])[l],
                          f32(inp["lambda_k1"])[l], f32(inp["lambda_q2"])[l], f32(inp["lambda_k2"])[l]])
    rows = np.ascontiguousarray(np.broadcast_to(row[None, :], (128, 1408)))
    U = f32(inp["peer_u"])[l]
    V = f32(inp["peer_v"])[l]
    tab = np.empty((128, 128, 2048), np.float32)
    tab[:, :, 0:1024] = U.reshape(128, 128, 8, 128).transpose(1, 3, 2, 0).reshape(128, 128, 1024)
    tab[:, :, 1024:2048] = V.reshape(128, 128, 1024).transpose(1, 0, 2)
    cst, rope = _consts()
    shared = dict(w_in=f32(inp["w_in"])[l], w_out=f32(inp["w_out"])[l], wq=f32(inp["peer_wq"])[l],
                  keys=f32(inp["peer_keys"])[l], wg=f32(inp["ple_w_gate"])[l], wp=f32(inp["ple_w_proj"])[l],
                  tab=tab, pk=pkv, rows=rows, cst=cst, rope=rope)
    x = f32(inp["x"])
    p = f32(inp["p"])[l]
    maps = []
    for b in range(NCORES):
        m = dict(shared)
        m["x"] = np.ascontiguousarray(x[b])
        m["p"] = np.ascontiguousarray(p[b])
        maps.append(m)
    return maps


_NC_CACHE = {}


def kernel(**inputs):
    maps = prep_inputs(inputs)
    if "nc" not in _NC_CACHE:
        _NC_CACHE["nc"] = build()
    res = run_bass_kernel_spmd(_NC_CACHE["nc"], maps, core_ids=list(range(NCORES)))
    return np.stack([np.asarray(r["out"], dtype=np.float32) for r in res.results], axis=0)
```

```python
import numpy as np
import concourse.bass as bass
import concourse.mybir as mybir
from concourse.bass_utils import run_bass_kernel_spmd

F32 = mybir.dt.float32
BF16 = mybir.dt.bfloat16
U32 = mybir.dt.uint32
AF = mybir.ActivationFunctionType
ALU = mybir.AluOpType
AX = mybir.AxisListType

D = 1024
S = 4096
NCORES = 8
EPS = 1e-6
NT = S // 128
TC = 512
NCH = S // TC
PC = 256
NPC = S // PC
LAMBDA_INIT = 0.2


class Buf:
    __slots__ = ("name", "last_w", "rd_comp", "rd_dma", "pre", "subs")

    def __init__(self, name):
        self.name = name
        self.pre = None
        self.subs = None
        self.last_w = None
        self.rd_comp = {}
        self.rd_dma = []


class Op:
    __slots__ = ("eng", "fn", "deps", "is_dma", "slot", "sem_val", "signal", "cnt", "idx")


ENGS = ["pe", "act", "dve", "pool", "sp"]
DMA_SLOTS = {"sp": 24, "pool": 8, "act": 4}


class Prog:
    def __init__(self, nc):
        self.nc = nc
        self.by_eng = {e: [] for e in ENGS}
        self.nops = 0
        self.dma_rr = {e: 0 for e in DMA_SLOTS}
        self.dma_last = {}
        self.dma_tot = {}

    def add(self, eng, fn, reads=(), writes=(), dma=False):
        op = Op()
        op.eng = eng
        op.fn = fn
        op.is_dma = dma
        op.signal = False
        op.cnt = 0
        op.idx = self.nops
        self.nops += 1
        deps = set()
        weak = set()
        for b in reads:
            if b.last_w is not None:
                deps.add(b.last_w)
        for b in writes:
            if b.last_w is not None:
                weak.add(b.last_w)
            weak.update(b.rd_comp.values())
            weak.update(b.rd_dma)
            if b.pre:
                for (_lo, _hi, ot) in b.pre:
                    for ob in [ot.b] + (ot.b.subs or []):
                        if ob.last_w is not None:
                            weak.add(ob.last_w)
                        weak.update(ob.rd_comp.values())
                        weak.update(ob.rd_dma)
                b.pre = None
        for d in weak:
            deps.add(d)
        if dma:
            slot = self.dma_rr[eng]
            self.dma_rr[eng] = (slot + 1) % DMA_SLOTS[eng]
            prev = self.dma_last.get((eng, slot))
            if prev is not None:
                deps.add(prev)
            self.dma_last[(eng, slot)] = op
            tot = self.dma_tot.get((eng, slot), 0) + 16
            self.dma_tot[(eng, slot)] = tot
            op.slot = slot
            op.sem_val = tot
        op.deps = [d for d in deps if not (d is op) and not (eng == "pe" and d.eng == "pe" and not d.is_dma and not dma)]
        for d in op.deps:
            if not d.is_dma:
                d.signal = True
        for b in reads:
            if dma:
                b.rd_dma.append(op)
            else:
                b.rd_comp[eng] = op
        for b in writes:
            b.last_w = op
            b.rd_comp = {}
            b.rd_dma = []
        self.by_eng[eng].append(op)
        return op

    def emit(self):
        nc = self.nc
        from contextlib import ExitStack
        with ExitStack() as es:
            csem = {e: es.enter_context(nc.semaphore("c_" + e)) for e in ENGS}
            dsem = {}
            for e, n in DMA_SLOTS.items():
                for s in range(n):
                    dsem[(e, s)] = es.enter_context(nc.semaphore("d_%s_%d" % (e, s)))
            for e in ENGS:
                c = 0
                for op in self.by_eng[e]:
                    if op.signal and not op.is_dma:
                        c += 1
                        op.cnt = c
            engobj = {"pe": "tensor", "act": "scalar", "dve": "vector", "pool": "gpsimd", "sp": "sync"}
            block = es.enter_context(nc.Block())

            def run(e, eng):
                waited = {}
                for op in self.by_eng[e]:
                    need = {}
                    for d in op.deps:
                        if d.is_dma:
                            key = ("d", d.eng, d.slot)
                            val = d.sem_val
                        else:
                            key = ("c", d.eng)
                            val = d.cnt
                        if need.get(key, 0) < val:
                            need[key] = val
                    for key, val in need.items():
                        if waited.get(key, 0) >= val:
                            continue
                        waited[key] = val
                        sem = csem[key[1]] if key[0] == "c" else dsem[(key[1], key[2])]
                        eng.wait_ge(sem, val)
                    ins = op.fn(eng)
                    if op.is_dma:
                        ins.then_inc(dsem[(e, op.slot)], 16)
                    elif op.signal:
                        ins.then_inc(csem[e], 1)
                if e == "sp":
                    for (qe, s), tot in self.dma_tot.items():
                        eng.wait_ge(dsem[(qe, s)], tot)

            for e in ENGS:
                if not self.by_eng[e] and e != "sp":
                    continue
                getattr(block, engobj[e])(lambda eng, e=e: run(e, eng))


class Tile:
    __slots__ = ("t", "b", "lo", "hi", "pre")

    def __getitem__(self, k):
        return self.t[k]

    def sub(self, n):
        out = []
        for i in range(n):
            b = Buf("%s.%d" % (self.b.name, i))
            b.pre = list(self.pre)
            out.append(b)
        self.b.subs = (self.b.subs or []) + out
        return out


class Arena:
    def __init__(self, nc, limit=229344):
        self.nc = nc
        self.off = 16512
        self.limit = limit
        self.n = 0
        self.hist = []

    def alloc(self, shape, dtype, name="t"):
        esz = {F32: 4, BF16: 2, U32: 4}[dtype]
        free = 1
        for s in shape[1:]:
            free *= s
        nbytes = (free * esz + 63) // 64 * 64
        assert self.off + nbytes <= self.limit, ("SBUF overflow", name, self.off, nbytes)
        self.n += 1
        T = Tile()
        T.t = self.nc.alloc_sbuf_tensor_at("%s_%d" % (name, self.n), list(shape), dtype, offset=self.off)
        T.lo = self.off
        T.hi = self.off + nbytes
        T.b = Buf(name)
        T.pre = [o for o in self.hist if o[0] < T.hi and T.lo < o[1]]
        T.b.pre = list(T.pre)
        self.hist.append((T.lo, T.hi, T))
        self.off += nbytes
        return T

    def mark(self):
        return self.off

    def release(self, m):
        self.off = m


class KB:
    def __init__(self, debug=False):
        self.nc = nc = bass.Bass("TRN2", target_bir_lowering=False)
        self.P = Prog(nc)
        self.A = Arena(nc)
        self.debug = debug
        self.ps = [nc.alloc_psum_tensor("ps%d" % i, [128, 512], F32).ap() for i in range(8)]
        self.psb = [p.bitcast(BF16) for p in self.ps]
        self.bps = [[Buf("ps%d" % i)] for i in range(8)]

    def din(self, name, shape, dt=F32):
        return self.nc.dram_tensor(name, list(shape), dt, kind="ExternalInput").ap()

    def dout(self, name, shape, dt=F32):
        return self.nc.dram_tensor(name, list(shape), dt, kind="ExternalOutput").ap()

    def dscr(self, name, shape, dt):
        return self.nc.dram_tensor(name, list(shape), dt, kind="Internal").ap()

    def dma(self, out, in_, reads=(), writes=(), q="sp", **kw):
        return self.P.add(q, lambda e: e.dma_start(out=out, in_=in_, **kw), reads, writes, dma=True)

    def act(self, out, in_, func, reads, writes, **kw):
        return self.P.add("act", lambda e: e.activation(out=out, in_=in_, func=func, **kw), reads, writes)

    def tt(self, eng, out, in0, in1, op, reads, writes):
        return self.P.add(eng, lambda e: e.tensor_tensor(out=out, in0=in0, in1=in1, op=op), reads, writes)

    def ts(self, eng, out, in0, s1, s2, op0, op1, reads, writes):
        if op1 is None:
            return self.P.add(eng, lambda e: e.tensor_scalar(out=out, in0=in0, scalar1=s1, scalar2=None, op0=op0), reads, writes)
        return self.P.add(eng, lambda e: e.tensor_scalar(out=out, in0=in0, scalar1=s1, scalar2=s2, op0=op0, op1=op1), reads, writes)

    def stt(self, out, in0, scalar, in1, op0, op1, reads, writes):
        return self.P.add("dve", lambda e: e.scalar_tensor_tensor(out=out, in0=in0, scalar=scalar, in1=in1, op0=op0, op1=op1), reads, writes)

    def cp(self, eng, out, in_, reads, writes):
        if eng == "act":
            return self.P.add("act", lambda e: e.copy(out=out, in_=in_), reads, writes)
        return self.P.add(eng, lambda e: e.tensor_copy(out=out, in_=in_), reads, writes)

    def mm(self, out, lhsT, rhs, start, stop, reads, writes, skip=False):
        return self.P.add("pe", lambda e: e.matmul(out, lhsT=lhsT, rhs=rhs, start=start, stop=stop, skip_group_check=skip), reads, writes)

    def tr(self, out, in_, ident, reads, writes):
        return self.P.add("pe", lambda e: e.transpose(out=out, in_=in_, identity=ident), reads, writes)

    def recip(self, out, in_, reads, writes):
        return self.P.add("dve", lambda e: e.reciprocal(out=out, in_=in_), reads, writes)

    def memset(self, eng, ap, val, writes):
        return self.P.add(eng, lambda e: e.memset(ap, val), (), writes)


def load_norm_T(K, src, xt, xn, xnT, ss, junk, g_col, idb, bidb, bconst, tr_banks, ntt, rd=()):
    nt = ntt * 128
    K.dma(xt[:, 0:ntt, :], src.rearrange("(tt p) d -> p tt d", p=128), reads=list(rd), writes=[xt.b])
    for tt in range(ntt):
        if junk is None:
            K.act(xn[:, tt, :], xt[:, tt, :], AF.Square, [xt.b], [xn.b, ss.b], accum_out=ss[:, tt:tt + 1])
        else:
            K.act(junk[:], xt[:, tt, :], AF.Square, [xt.b], [junk.b, ss.b], accum_out=ss[:, tt:tt + 1])
    K.ts("pool", ss[:, 8:8 + ntt], ss[:, 0:ntt], 1.0 / D, EPS, ALU.mult, ALU.add, [ss.b], [ss.b])
    K.tt("pool", ss[:, 16:16 + ntt], ss[:, 8:8 + ntt], K.mhalf[:, 0:ntt], ALU.pow, [ss.b, K.mhalf.b], [ss.b])
    for tt in range(ntt):
        K.ts("dve", xn[:, tt, :], xt[:, tt, :], ss[:, 16 + tt:17 + tt], None, ALU.mult, None, [xt.b, ss.b], [xn.b])
    for k in range(8):
        bk = tr_banks[k % len(tr_banks)]
        for tt in range(ntt):
            K.tr(K.psb[bk][:, tt * 128:(tt + 1) * 128], xn[:, tt, k * 128:(k + 1) * 128], idb, [xn.b, bidb], K.bps[bk])
        if k % 2 == 0:
            K.act(xnT[:, k, 0:nt], K.psb[bk][:, 0:nt], AF.Copy, [K.bps[bk][0], bconst], [xnT.b], scale=g_col[:, k:k + 1])
        else:
            K.ts("dve", xnT[:, k, 0:nt], K.psb[bk][:, 0:nt], g_col[:, k:k + 1], None, ALU.mult, None, [K.bps[bk][0], bconst], [xnT.b])


def build(debug=False, phases="TCAP"):
    K = KB(debug)
    nc, P, A = K.nc, K.P, K.A
    x_d = K.din("x", [S, D])
    p_d = K.din("p", [S, 256])
    w_in_d = K.din("w_in", [D, 2560])
    w_out_d = K.din("w_out", [D, D])
    wq_d = K.din("wq", [D, 2048])
    keys_d = K.din("keys", [2, 128, 128])
    wg_d = K.din("wg", [D, D])
    wp_d = K.din("wp", [256, D])
    tab_d = K.din("tab", [128, 128, 2048])
    pk_d = K.din("pk", [128, 160])
    rows_d = K.din("rows", [128, 1408])
    cst_d = K.din("cst", [128, 3, 128])
    rope_d = K.din("rope", [128, 2, S])
    out_d = K.dout("out", [S, D])
    tabb_d = K.dscr("tabb", [128, 128, 2048], BF16)
    h1_d = K.dscr("h1s", [S, D], F32)
    cvT_d = K.dscr("cvTs", [128, 4, S], BF16)
    btab = [Buf("tabb%d" % j) for j in range(128)]
    bh1 = [Buf("h1s%d" % i) for i in range(NT)]
    bcv = [Buf("cvT%d" % i) for i in range(NCH)]
    dbg = {}

    pk = A.alloc([128, 160], F32, "pk")
    rows = A.alloc([128, 384], F32, "rows")
    cst = A.alloc([128, 3, 128], F32, "cst")
    cstb = A.alloc([128, 2, 128], BF16, "cstb")
    onesf = A.alloc([128, 128], F32, "onesf")
    K.dma(pk[:], pk_d, writes=[pk.b])
    K.dma(rows[:], rows_d[:, 1024:1408], writes=[rows.b])
    K.dma(cst[:], cst_d, writes=[cst.b])
    K.cp("dve", cstb[:], cst[:, 0:2, :], [cst.b], [cstb.b])
    K.memset("dve", onesf[:], 1.0 / 512, [onesf.b])
    mhalf = A.alloc([128, 8], F32, "mhalf")
    K.memset("pool", mhalf[:], -0.5, [mhalf.b])
    K.mhalf = mhalf
    idb = cstb[:, 0, :]
    trib = cstb[:, 1, :]
    idf = cst[:, 0, :]
    iota = cst[:, 2, :]
    g_attn = pk[:, 0:8]
    g_ffn = pk[:, 8:16]
    g_ple = pk[:, 16:24]
    conv_w = pk[:, 24:148].rearrange("p (c k) -> p c k", c=4)
    conv_b = pk[:, 148:152]
    ln_g = pk[:, 152:156]
    ln_b = pk[:, 156:160]
    g_fin = None
    subg = rows[:, 0:128]
    lamv = rows[:, 128:384]
    base_mark = A.mark()

    if "T" in phases:
        TG = 8
        for j0 in range(0, 128, TG):
            K.dma(tabb_d[j0:j0 + TG], tab_d[j0:j0 + TG], writes=btab[j0:j0 + TG], q="pool")

    if "C" in phases:
        m0 = A.mark()
        wC = A.alloc([128, 8, 1024], BF16, "wC")
        diag = A.alloc([128, 4, 31, 128], BF16, "diag")
        m1 = A.mark()
        stg = [A.alloc([128, 1024], F32, "stg") for _ in range(2)]
        for k in range(8):
            s_ = stg[k % 2]
            K.dma(s_[:], w_in_d[k * 128:(k + 1) * 128, 1536:2560], writes=[s_.b])
            K.cp("act" if k % 2 == 0 else "dve", wC[:, k, :], s_[:], [s_.b], [wC.b])
        for c in range(4):
            for k in range(31):
                K.ts("pool" if (k % 2) else "dve", diag[:, c, k, :], idf, conv_w[:, c, k:k + 1], None, ALU.mult, None, [cst.b, pk.b], [diag.b])
        A.release(m1)
        xt = A.alloc([128, 4, 1024], F32, "xt")
        xn = A.alloc([128, 4, 1024], BF16, "xn")
        xnT = A.alloc([128, 8, 512], BF16, "xnT")
        ss = A.alloc([128, 24], F32, "ss")
        junk = A.alloc([128, 1024], BF16, "junk")
        u = [A.alloc([128, 4, 542], BF16, "u") for _ in range(3)]
        sig = A.alloc([128, 512], F32, "sig")
        ysb = A.alloc([128, 4, 512], F32, "ysb")
        ysq = A.alloc([128, 4, 512], F32, "ysq")
        mean_sb = A.alloc([128, 512], F32, "mean")
        m2 = A.alloc([128, 512], F32, "m2")
        var = A.alloc([128, 512], F32, "var")
        rstdb = A.alloc([128, 512], F32, "rstdb")
        zt = A.alloc([128, 512], F32, "zt")
        z2 = A.alloc([128, 512], F32, "z2")
        cvo = A.alloc([128, 4, 512], BF16, "cvo")
        K.memset("pool", u[0][:, :, 0:30], 0.0, [u[0].b])
        import os
        NCHC = int(os.environ.get('DBG_NCHC', NCH))

        def c_front(ci):
            uc, un = u[ci % 3], u[(ci + 1) % 3]
            load_norm_T(K, x_d[ci * TC:(ci + 1) * TC, :], xt, xn, xnT, ss, junk, g_attn, idb, cstb.b, pk.b, [0, 1], 4)
            yield
            for c in range(4):
                for k in range(8):
                    K.mm(K.ps[2][:, :], wC[:, k, c * 128:(c + 1) * 128], xnT[:, k, :], k == 0, k == 7, [wC.b, xnT.b], K.bps[2])
                for k in range(8):
                    K.mm(K.ps[3][:, :], wC[:, k, 512 + c * 128:512 + (c + 1) * 128], xnT[:, k, :], k == 0, k == 7, [wC.b, xnT.b], K.bps[3])
                K.act(sig[:], K.ps[3][:, :], AF.Sigmoid, K.bps[3], [sig.b])
                K.tt("dve", uc[:, c, 30:542], K.ps[2][:, :], sig[:], ALU.mult, K.bps[2] + [sig.b], [uc.b])
                yield
            if ci + 1 < NCH:
                K.cp("pool", un[:, :, 0:30], uc[:, :, 512:542], [uc.b], [un.b])
            yield

        def c_back(ci):
            uc = u[ci % 3]
            for c in range(4):
                yb = 4 + c % 2
                for k in range(31):
                    K.mm(K.ps[yb][:, :], diag[:, c, k, :], uc[:, c, k:k + 512], k == 0, k == 30, [diag.b, uc.b], K.bps[yb])
                K.act(ysb[:, c, :], K.ps[yb][:, :], AF.Identity, K.bps[yb] + [pk.b], [ysb.b], bias=conv_b[:, c:c + 1])
                K.act(ysq[:, c, :], K.ps[yb][:, :], AF.Square, K.bps[yb] + [pk.b], [ysq.b], bias=conv_b[:, c:c + 1])
                yield
            for c in range(4):
                K.mm(K.ps[6][:, :], onesf[:], ysb[:, c, :], c == 0, c == 3, [onesf.b, ysb.b], K.bps[6])
            for c in range(4):
                K.mm(K.ps[7][:, :], onesf[:], ysq[:, c, :], c == 0, c == 3, [onesf.b, ysq.b], K.bps[7])
            K.cp("act", mean_sb[:], K.ps[6][:, :], K.bps[6], [mean_sb.b])
            K.tt("dve", m2[:], mean_sb[:], mean_sb[:], ALU.mult, [mean_sb.b], [m2.b])
            K.tt("dve", var[:], K.ps[7][:, :], m2[:], ALU.subtract, K.bps[7] + [m2.b], [var.b])
            K.act(var[:], var[:], AF.Sqrt, [var.b], [var.b], bias=EPS)
            K.recip(rstdb[:], var[:], [var.b], [rstdb.b])
            yield
            for c in range(4):
                K.tt("dve", zt[:], ysb[:, c, :], mean_sb[:], ALU.subtract, [ysb.b, mean_sb.b], [zt.b])
                K.tt("dve", z2[:], zt[:], rstdb[:], ALU.mult, [zt.b, rstdb.b], [z2.b])
                K.act(cvo[:, c, :], z2[:], AF.Silu, [z2.b, pk.b], [cvo.b], scale=ln_g[:, c:c + 1], bias=ln_b[:, c:c + 1])
                yield
            K.dma(cvT_d[:, :, ci * TC:(ci + 1) * TC], cvo[:], reads=[cvo.b], writes=[bcv[ci]])

        def rr(gens):
            live = [g for g in gens if g is not None]
            while live:
                for g_ in list(live):
                    try:
                        next(g_)
                    except StopIteration:
                        live.remove(g_)

        rr([c_front(0)])
        for ci in range(NCHC):
            rr([c_back(ci), c_front(ci + 1) if ci + 1 < NCHC else None])
        A.release(m0)

    K.extra = dict(x_d=x_d, p_d=p_d, out_d=out_d, h1_d=h1_d, cvT_d=cvT_d, tabb_d=tabb_d, btab=btab, bh1=bh1, bcv=bcv,
                   rows_d=rows_d, w_in_d=w_in_d, w_out_d=w_out_d, wq_d=wq_d, keys_d=keys_d, wg_d=wg_d, wp_d=wp_d, rope_d=rope_d,
                   pk=pk, rows=rows, cst=cst, cstb=cstb, idb=idb, trib=trib, idf=idf, iota=iota, g_attn=g_attn,
                   g_ffn=g_ffn, g_ple=g_ple, g_fin=g_fin, subg=subg, lamv=lamv, onesf=onesf)
    if "A" in phases:
        phase_A(K)
    if "P" in phases:
        phase_P(K)
    if debug:
        if "C" in phases and "A" not in phases:
            o = K.dout("dbg_cvT", [128, 4, S], BF16)
            K.dma(o, cvT_d, reads=bcv)
        if "A" in phases and "P" not in phases:
            o = K.dout("dbg_h1", [S, D], F32)
            K.dma(o, h1_d, reads=bh1)
    P.emit()
    return nc


def phase_A(K):
    nc, P, A = K.nc, K.P, K.A
    X = K.extra
    x_d, w_in_d, w_out_d, rope_d, cvT_d, h1_d = X["x_d"], X["w_in_d"], X["w_out_d"], X["rope_d"], X["cvT_d"], X["h1_d"]
    pk, rows, cstb = X["pk"], X["rows"], X["cstb"]
    idb, trib, g_attn, subg, lamv = X["idb"], X["trib"], X["g_attn"], X["subg"], X["lamv"]
    bcv, bh1 = X["bcv"], X["bh1"]
    m0 = A.mark()
    wA = A.alloc([128, 8, 2560], BF16, "wA")
    wo = A.alloc([128, 8, 1024], BF16, "wo")
    kT = A.alloc([128, 4, S], BF16, "kT")
    bkT = kT.sub(NCH)
    Va = A.alloc([128, NT, 4, 130], BF16, "Va")
    bVa = Va.sub(NCH)
    lam = A.alloc([128, 8], F32, "lam")
    sg = A.alloc([128, 128], F32, "sg")
    ltmp = A.alloc([128, 128], F32, "ltmp")
    m1 = A.mark()
    stg = [A.alloc([128, 1536], F32, "stgA") for _ in range(2)]
    for k in range(8):
        s_ = stg[k % 2]
        K.dma(s_[:], w_in_d[k * 128:(k + 1) * 128, 0:1536], writes=[s_.b])
        K.cp("act", wA[:, k, 0:1536], s_[:], [s_.b], [wA.b])
        sv = s_[:, 0:1024].rearrange("p (j d) -> p j d", d=64)
        wr = wA[:, k, 1536:2560].rearrange("p (j d) -> p j d", d=64)
        K.ts("dve", wr[:, :, 0:32], sv[:, :, 32:64], -1.0, None, ALU.mult, None, [s_.b], [wA.b])
        K.cp("pool", wr[:, :, 32:64], sv[:, :, 0:32], [s_.b], [wA.b])
    stg2 = [A.alloc([128, 1024], F32, "stgO") for _ in range(2)]
    for k in range(8):
        s_ = stg2[k % 2]
        K.dma(s_[:], w_out_d[k * 128:(k + 1) * 128, :], writes=[s_.b])
        K.cp("act" if k % 2 else "dve", wo[:, k, :], s_[:], [s_.b], [wo.b])
    A.release(m1)
    K.tt("dve", ltmp[:, 0:64], lamv[:, 0:64], lamv[:, 64:128], ALU.mult, [rows.b], [ltmp.b])
    K.tt("dve", ltmp[:, 64:128], lamv[:, 128:192], lamv[:, 192:256], ALU.mult, [rows.b], [ltmp.b])
    K.P.add("dve", lambda e: e.tensor_reduce(out=lam[:, 0:2], in_=ltmp[:].rearrange("p (a b) -> p a b", a=2), axis=AX.X, op=ALU.add), [ltmp.b], [lam.b])
    K.act(lam[:, 2:4], lam[:, 0:2], AF.Exp, [lam.b], [lam.b])
    K.tt("dve", lam[:, 4:5], lam[:, 2:3], lam[:, 3:4], ALU.subtract, [lam.b], [lam.b])
    K.ts("dve", lam[:, 5:6], lam[:, 4:5], LAMBDA_INIT, -1.0, ALU.add, ALU.mult, [lam.b], [lam.b])
    K.ts("dve", sg[:], subg, 1.0 - LAMBDA_INIT, None, ALU.mult, None, [rows.b], [sg.b])
    neglam = lam[:, 5:6]
    K.memset("pool", Va[:], 1.0, bVa)

    import os
    LVL = int(os.environ.get('DBG_STOP', 9))
    xt = A.alloc([128, 4, 1024], F32, "xt")
    xn = A.alloc([128, 4, 1024], BF16, "xn")
    xnT = A.alloc([128, 8, 512], BF16, "xnT")
    ss = A.alloc([128, 24], F32, "ss")
    rp = [A.alloc([128, 2, 512], F32, "rp") for _ in range(1)]
    t1 = [A.alloc([128, 512], F32, "t1") for _ in range(1)]
    t2 = [A.alloc([128, 512], F32, "t2") for _ in range(1)]
    qTz = [A.alloc([128, 4, 512], BF16, "qTz") for _ in range(2)]
    bqT = [qTz[0].sub(4), qTz[1].sub(4)]
    K.memset("pool", qTz[0][64:128, :, :], 0.0, bqT[0])
    K.memset("pool", qTz[1][0:64, :, :], 0.0, bqT[1])
    PT = [A.alloc([128, 512], BF16, "PT") for _ in range(4)]
    att = A.alloc([128, 128], F32, "att")
    a1 = A.alloc([128, 128], F32, "a1")
    sm = A.alloc([128, 4, 8], F32, "sm")
    Osb = A.alloc([128, 8, 129], F32, "Osb")
    attn_sb = A.alloc([128, 4, 512], BF16, "attn_sb")
    catT = A.alloc([128, 8, 512], BF16, "catT")
    h1o = [A.alloc([128, 1024], F32, "h1o") for _ in range(2)]
    cnt = 0
    tcnt = 0
    import os
    for ci in range(int(os.environ.get('DBG_NCH', NCH))):
        if LVL < 2:
            break
        load_norm_T(K, x_d[ci * TC:(ci + 1) * TC, :], xt, xn, xnT, ss, None, g_attn, idb, cstb.b, pk.b, [0, 1], 4)
        rpc = rp[0]
        K.dma(rpc[:], rope_d[:, :, ci * TC:(ci + 1) * TC], writes=[rpc.b])
        K.dma(catT[:, 4:8, :], cvT_d[:, :, ci * TC:(ci + 1) * TC], reads=[bcv[ci]], writes=[catT.b])
        cos, sin = rpc[:, 0, :], rpc[:, 1, :]
        for h in range(4):
            for which in range(2):
                c0 = which * 512 + h * 128
                for k in range(8):
                    K.mm(K.ps[0][:, :], wA[:, k, c0:c0 + 128], xnT[:, k, :], k == 0, k == 7, [wA.b, xnT.b], K.bps[0])
                for k in range(8):
                    K.mm(K.ps[1][:, :], wA[:, k, 1536 + c0:1536 + c0 + 128], xnT[:, k, :], k == 0, k == 7, [wA.b, xnT.b], K.bps[1])
                ta, tb = t1[0], t2[0]
                tcnt += 1
                K.tt("dve", ta[:], K.ps[0][:, :], cos, ALU.mult, K.bps[0] + [rpc.b], [ta.b])
                K.tt("dve", tb[:], K.ps[1][:, :], sin, ALU.mult, K.bps[1] + [rpc.b], [tb.b])
                if which == 0:
                    K.tt("pool", qTz[0][0:64, h, :], ta[0:64, :], tb[0:64, :], ALU.add, [ta.b, tb.b], [bqT[0][h]])
                    K.tt("pool", qTz[1][64:128, h, :], ta[64:128, :], tb[64:128, :], ALU.add, [ta.b, tb.b], [bqT[1][h]])
                else:
                    K.tt("pool", kT[:, h, ci * TC:(ci + 1) * TC], ta[:], tb[:], ALU.add, [ta.b, tb.b], [bkT[ci]])
        if LVL < 3:
            continue
        for tt_ in range(4):
            bk = tt_ % 2
            for k in range(8):
                K.mm(K.ps[bk][:, :], xnT[:, k, tt_ * 128:(tt_ + 1) * 128], wA[:, k, 1024:1536], k == 0, k == 7, [wA.b, xnT.b], K.bps[bk])
            K.P.add("act", lambda e, bk=bk, tt_=tt_, ci=ci: e.copy(out=Va[:, ci * 4 + tt_, :, 0:128], in_=K.ps[bk][:, :].rearrange("p (h d) -> p h d", d=128)),
                    K.bps[bk], [bVa[ci]])
        if LVL < 4:
            continue
        items = [(h, c, kt) for h in range(4) for c in range(2) for kt in range(4 * ci + 4)]
        DP = 3

        def emit_qk(h, c, kt, idx):
            r = kt - 4 * ci
            c0 = max(r, 0) * 128
            sbk = idx % 4
            pt = PT[idx % 4]
            K.mm(K.ps[sbk][:, c0:512], kT[:, h, kt * 128:(kt + 1) * 128], qTz[c][:, h, c0:512],
                 True, True, [bkT[kt // 4], bqT[c][h]], K.bps[sbk])
            K.act(pt[:, c0:512], K.ps[sbk][:, c0:512], AF.Exp, K.bps[sbk], [pt.b], scale=0.125)
            if r >= 0:
                K.tt("dve", pt[:, c0:c0 + 128], pt[:, c0:c0 + 128], trib, ALU.mult, [pt.b, cstb.b], [pt.b])

        def emit_pv(h, c, kt, idx):
            r = kt - 4 * ci
            pt = PT[idx % 4]
            for tqi in range(max(r, 0), 4):
                a = c * 4 + tqi
                bank, col = 4 + a // 2, (a % 2) * 256
                K.mm(K.ps[bank][:, col:col + 129], pt[:, tqi * 128:(tqi + 1) * 128], Va[:, kt, h, 0:129],
                     kt == 0 and a % 2 == 0, kt == 4 * ci + tqi, [pt.b, bVa[kt // 4]], K.bps[bank], skip=True)
            if c == 1 and kt == 4 * ci + 3:
                post(h)

        def post(h):
            if LVL >= 5:
                for bi in range(4):
                    src = K.ps[4 + bi][:, :].rearrange("p (a b) -> p a b", b=256)[:, :, 0:129]
                    K.cp("act" if bi % 2 == 0 else "dve", Osb[:, 2 * bi:2 * bi + 2, :], src, K.bps[4 + bi], [Osb.b])
            for tqi in range(4):
                if LVL < 5:
                    continue
                O1 = Osb[:, tqi, :]
                O2 = Osb[:, 4 + tqi, :]
                b1 = [Osb.b]
                b2 = [Osb.b]
                SUB = int(os.environ.get("DBG_SUB", 9))
                K.recip(sm[:, tqi, 0:1], O1[:, 128:129], b1, [sm.b])
                K.recip(sm[:, tqi, 1:2], O2[:, 128:129], b2, [sm.b])
                if SUB < 2:
                    continue
                K.tt("dve", sm[:, tqi, 2:3], sm[:, tqi, 1:2], neglam, ALU.mult, [sm.b, lam.b], [sm.b])
                K.act(a1[:], O1[:, 0:128], AF.Copy, b1 + [sm.b], [a1.b], scale=sm[:, tqi, 0:1])
                if SUB < 3:
                    continue
                K.stt(att[:], O2[:, 0:128], sm[:, tqi, 2:3], a1[:], ALU.mult, ALU.add, b2 + [sm.b, a1.b], [att.b])
                if SUB < 4:
                    continue
                K.act(a1[:], att[:], AF.Square, [att.b], [a1.b, sm.b], accum_out=sm[:, tqi, 3:4])
                if SUB < 5:
                    continue
                K.ts("pool", sm[:, tqi, 4:5], sm[:, tqi, 3:4], 1.0 / 128, EPS, ALU.mult, ALU.add, [sm.b], [sm.b])
                K.tt("pool", sm[:, tqi, 5:6], sm[:, tqi, 4:5], K.mhalf[:, 0:1], ALU.pow, [sm.b, K.mhalf.b], [sm.b])
                if SUB < 6:
                    continue
                K.stt(attn_sb[:, tqi, h * 128:(h + 1) * 128], att[:], sm[:, tqi, 5:6], sg[:], ALU.mult, ALU.mult, [att.b, sm.b, sg.b], [attn_sb.b])
        for idx in range(len(items) + DP):
            if idx < len(items):
                emit_qk(*items[idx], cnt + idx)
            if idx >= DP:
                emit_pv(*items[idx - DP], cnt + idx - DP)
        cnt += len(items)
        if LVL < 6:
            continue
        for tqi in range(4):
            bk = tqi % 2
            for h in range(4):
                K.tr(K.psb[bk][:, h * 128:(h + 1) * 128], attn_sb[:, tqi, h * 128:(h + 1) * 128], idb, [attn_sb.b, cstb.b], K.bps[bk])
            if os.environ.get("DBG_V", "0") == "1":
                continue
            if os.environ.get("DBG_V", "0") == "2":
                for h in range(4):
                    K.cp("dve", catT[:, h, tqi * 128:(tqi + 1) * 128], K.psb[bk][:, h * 128:(h + 1) * 128], K.bps[bk], [catT.b])
                continue
            K.cp("dve", catT[:, 0:4, tqi * 128:(tqi + 1) * 128], K.psb[bk][:, 0:512].rearrange("p (h d) -> p h d", d=128), K.bps[bk], [catT.b])
        for tqi in range(4):
            if LVL < 7:
                continue
            ho = h1o[tqi % 2]
            for half in range(2):
                for k in range(8):
                    K.mm(K.ps[half][:, :], catT[:, k, tqi * 128:(tqi + 1) * 128], wo[:, k, half * 512:(half + 1) * 512], k == 0, k == 7, [catT.b, wo.b], K.bps[half])
                K.tt("dve", ho[:, half * 512:(half + 1) * 512], K.ps[half][:, :], xt[:, tqi, half * 512:(half + 1) * 512], ALU.add, K.bps[half] + [xt.b], [ho.b])
            ti = ci * 4 + tqi
            if LVL < 8:
                continue
            K.dma(h1_d[ti * 128:(ti + 1) * 128, :], ho[:], reads=[ho.b], writes=[bh1[ti]])
    A.release(m0)


def phase_P(K):
    import os
    nc, P, A = K.nc, K.P, K.A
    X = K.extra
    h1_d, out_d, p_d, wq_d, keys_d, wg_d, wp_d, tabb_d = X["h1_d"], X["out_d"], X["p_d"], X["wq_d"], X["keys_d"], X["wg_d"], X["wp_d"], X["tabb_d"]
    pk, rows, cst, cstb = X["pk"], X["rows"], X["cst"], X["cstb"]
    idb, idf, iota, g_ffn, g_ple, g_fin = X["idb"], X["idf"], X["iota"], X["g_ffn"], X["g_ple"], X["g_fin"]
    bh1, btab = X["bh1"], X["btab"]
    mT_d = K.dscr("mTs", [128, 8, S], BF16)
    pk_s = K.dscr("picks", [S, 3, 128], F32)
    bmT = [Buf("mTs%d" % i) for i in range(NPC)]
    bpk = [Buf("pks%d" % i) for i in range(NPC)]
    NPCR = int(os.environ.get("DBG_NPC", NPC))
    SUBP = os.environ.get("DBG_SUBP", "abc")

    sc_d = K.dscr("scs", [S, 2048], F32)
    bscd = [Buf("scs%d" % i) for i in range(NT)]
    bpkt = [Buf("pkt%d" % i) for i in range(NT)]
    NTR = NPCR * 2
    K.tk_done = 0
    B4 = [128, 8, 16, 16]
    iota16 = iota[:, 0:16].unsqueeze(1).unsqueeze(1).to_broadcast(B4)

    def alloc_topk_ws():
        w = dict(sc=A.alloc([128, 16, 128], F32, "sc"), sc2=A.alloc([128, 16, 128], F32, "sc2"),
                 sv=A.alloc([128, 16, 16], F32, "sv"), si=A.alloc([128, 16, 16], U32, "si"), sif=A.alloc([128, 16, 16], F32, "sif"),
                 cs=A.alloc([128, 8, 16], F32, "cs"), ci=A.alloc([128, 8, 16], U32, "ci"),
                 abu=A.alloc([128, 2, 8, 16], U32, "abu"), abf=A.alloc([128, 2, 8, 16], F32, "abf"),
                 pkt=A.alloc([128, 3, 128], F32, "pkt"),
                 ex=A.alloc([128, 8, 16], F32, "ex"), rs=A.alloc([128, 16], F32, "rs"))
        w["bsc"] = w["sc"].sub(4)
        w["bsv"] = w["sv"].sub(16)
        w["bsi"] = w["si"].sub(16)
        w["bsc2"] = w["sc2"].sub(16)
        w["bcs"] = w["cs"].sub(8)
        w["bci"] = w["ci"].sub(8)
        return w

    def topk_steps(ti, w):
        sc, sc2, sv, si, sif, cs, ci = w["sc"], w["sc2"], w["sv"], w["si"], w["sif"], w["cs"], w["ci"]
        abu, abf, pkt, ex, rs = w["abu"], w["abf"], w["pkt"], w["ex"], w["rs"]
        bsc, bsv, bsi, bsc2, bcs, bci = w["bsc"], w["bsv"], w["bsi"], w["bsc2"], w["bcs"], w["bci"]
        cand = sc[:].rearrange("p (h c) n -> p h (c n)", c=2)
        cand2 = sc2[:].rearrange("p (h c) n -> p h (c n)", c=2)
        eq = sc[:].rearrange("p (h c) (a b) -> p h (c a) b", c=2, b=16)
        svv = sv[:].rearrange("p (h c) k -> p h c k", c=2)
        sifv = sif[:].rearrange("p (h c) k -> p h c k", c=2)
        K.dma(sc[:].rearrange("p g n -> p (g n)"), sc_d[ti * 128:(ti + 1) * 128, :], reads=[bscd[ti]], writes=bsc)
        yield
        for g in range(16):
            K.P.add("dve", lambda e, g=g: e.max(out=sv[:, g, 0:8], in_=sc[:, g, :]), [bsc[g // 4]], [bsv[g]])
            if g % 3 == 2:
                yield
        yield
        for g in range(16):
            K.P.add("dve", lambda e, g=g: e.max_index(out=si[:, g, 0:8], in_max=sv[:, g, 0:8], in_values=sc[:, g, :]), [bsc[g // 4], bsv[g]], [bsi[g]])
            if g % 3 == 2:
                yield
        yield
        for g in range(16):
            K.P.add("dve", lambda e, g=g: e.match_replace(out=sc2[:, g, :], in_to_replace=sv[:, g, 0:8], in_values=sc[:, g, :], imm_value=-1e30), [bsc[g // 4], bsv[g]], [bsc2[g]])
            if g % 3 == 2:
                yield
        yield
        for g in range(16):
            K.P.add("dve", lambda e, g=g: e.max(out=sv[:, g, 8:16], in_=sc2[:, g, :]), [bsc2[g]], [bsv[g]])
            if g % 3 == 2:
                yield
        yield
        for g in range(16):
            K.P.add("dve", lambda e, g=g: e.max_index(out=si[:, g, 8:16], in_max=sv[:, g, 8:16], in_values=sc2[:, g, :]), [bsc2[g], bsv[g]], [bsi[g]])
            if g % 3 == 2:
                yield
        yield
        K.cp("dve", sif[:], si[:], bsi, [sif.b])
        K.tt("dve", cand.rearrange("p h (a b) -> p h a b", b=16), svv[:, :, 0, :].unsqueeze(3).to_broadcast(B4),
             svv[:, :, 1, :].unsqueeze(2).to_broadcast(B4), ALU.add, bsv, bsc)
        yield
        for h in range(8):
            K.P.add("dve", lambda e, h=h: e.max(out=cs[:, h, 0:8], in_=cand[:, h, :]), [bsc[h // 2]], [bcs[h]])
            if h % 3 == 2:
                yield
        yield
        for h in range(8):
            K.P.add("dve", lambda e, h=h: e.max_index(out=ci[:, h, 0:8], in_max=cs[:, h, 0:8], in_values=cand[:, h, :]), [bsc[h // 2], bcs[h]], [bci[h]])
            if h % 3 == 2:
                yield
        for h in range(8):
            K.P.add("dve", lambda e, h=h: e.match_replace(out=cand2[:, h, :], in_to_replace=cs[:, h, 0:8], in_values=cand[:, h, :], imm_value=-1e30),
                    [bsc[h // 2], bcs[h]], [bsc2[2 * h], bsc2[2 * h + 1]])
            if h % 3 == 2:
                yield
        yield
        for h in range(8):
            K.P.add("dve", lambda e, h=h: e.max(out=cs[:, h, 8:16], in_=cand2[:, h, :]), [bsc2[2 * h], bsc2[2 * h + 1]], [bcs[h]])
            if h % 3 == 2:
                yield
        yield
        for h in range(8):
            K.P.add("dve", lambda e, h=h: e.max_index(out=ci[:, h, 8:16], in_max=cs[:, h, 8:16], in_values=cand2[:, h, :]), [bsc2[2 * h], bsc2[2 * h + 1], bcs[h]], [bci[h]])
            if h % 3 == 2:
                yield
        yield
        K.P.add("dve", lambda e: e.tensor_single_scalar(out=abu[:, 0, :, :], in_=ci[:], scalar=4, op=ALU.logical_shift_right), bci, [abu.b])
        K.P.add("dve", lambda e: e.tensor_single_scalar(out=abu[:, 1, :, :], in_=ci[:], scalar=15, op=ALU.bitwise_and), bci, [abu.b])
        K.cp("dve", abf[:], abu[:], [abu.b], [abf.b])
        K.tt("dve", ex[:], cs[:], cs[:, :, 0:1].to_broadcast([128, 8, 16]), ALU.subtract, bcs, [ex.b])
        yield
        for a in range(2):
            K.tt("dve", eq, iota16, abf[:, a, :, :].unsqueeze(3).to_broadcast(B4), ALU.is_equal, [cst.b, abf.b], bsc)
            yield
            K.tt("dve", eq, eq, sifv[:, :, a, :].unsqueeze(2).to_broadcast(B4), ALU.mult, bsc + [sif.b], bsc)
            yield
            if a == 0:
                K.act(ex[:], ex[:], AF.Exp, [ex.b], [ex.b])
            K.P.add("dve", lambda e, a=a: e.tensor_reduce(out=pkt[:, a, :].rearrange("p (h k) -> p h k", k=16), in_=eq, axis=AX.X, op=ALU.add), bsc, [pkt.b])
            yield
        K.P.add("dve", lambda e: e.tensor_reduce(out=rs[:, 0:8], in_=ex[:], axis=AX.X, op=ALU.add), [ex.b], [rs.b])
        K.recip(rs[:, 8:16], rs[:, 0:8], [rs.b], [rs.b])
        K.tt("dve", pkt[:, 2, :].rearrange("p (h k) -> p h k", k=16), ex[:], rs[:, 8:16].unsqueeze(2).to_broadcast([128, 8, 16]), ALU.mult,
             [ex.b, rs.b], [pkt.b])
        K.dma(pk_s[ti * 128:(ti + 1) * 128], pkt[:], reads=[pkt.b], writes=[bpkt[ti]])
        yield

    def drain(g_):
        if g_ is not None:
            for _ in g_:
                pass

    def chain(*gs):
        for g_ in gs:
            yield from g_

    if "a" in SUBP:
        m0 = A.mark()
        wqb = A.alloc([128, 8, 2048], BF16, "wqb")
        keysT = A.alloc([128, 2, 128], BF16, "keysT")
        m1 = A.mark()
        stg = [A.alloc([128, 2048], F32, "stgq") for _ in range(2)]
        for k in range(8):
            s_ = stg[k % 2]
            K.dma(s_[:], wq_d[k * 128:(k + 1) * 128, :], writes=[s_.b])
            K.cp("act" if k % 2 else "dve", wqb[:, k, :], s_[:], [s_.b], [wqb.b])
        kst = A.alloc([128, 2, 128], F32, "kst")
        K.dma(kst[:], keys_d.rearrange("c n d -> n c d"), writes=[kst.b])
        for c in range(2):
            K.tr(K.ps[0][:, c * 128:(c + 1) * 128], kst[:, c, :], idf, [kst.b, cst.b], K.bps[0])
        K.cp("act", keysT[:], K.ps[0][:, 0:256].rearrange("p (c n) -> p c n", c=2), K.bps[0], [keysT.b])
        A.release(m1)
        ss = A.alloc([128, 24], F32, "ss")
        junk = A.alloc([128, 1024], BF16, "junk")
        CH = []
        for _ in range(2):
            CH.append(dict(ht=A.alloc([128, 2, 1024], F32, "ht"), hn=A.alloc([128, 2, 1024], BF16, "hn"),
                           mT=A.alloc([128, 8, PC], BF16, "mT"), qpT=A.alloc([128, 16, PC], BF16, "qpT")))
        scb = [A.alloc([128, 16, 128], F32, "scb") for _ in range(2)]

        def chunk_front(pc):
            c_ = CH[pc % 2]
            ht, hn, mT, qpT = c_["ht"], c_["hn"], c_["mT"], c_["qpT"]
            load_norm_T(K, h1_d[pc * PC:(pc + 1) * PC, :], ht, hn, mT, ss, junk, g_ffn, idb, cstb.b, pk.b, [0, 1], 2,
                        rd=bh1[pc * 2:pc * 2 + 2])
            K.dma(mT_d[:, :, pc * PC:(pc + 1) * PC], mT[:], reads=[mT.b], writes=[bmT[pc]])
            yield
            for g in range(16):
                bk = 2 + g % 2
                for k in range(8):
                    K.mm(K.ps[bk][:, 0:PC], wqb[:, k, g * 128:(g + 1) * 128], mT[:, k, :], k == 0, k == 7, [wqb.b, mT.b], K.bps[bk])
                K.cp("act" if g % 2 else "dve", qpT[:, g, :], K.ps[bk][:, 0:PC], K.bps[bk], [qpT.b])
                if g % 4 == 3:
                    yield

        def chunk_scores(pc):
            qpT = CH[pc % 2]["qpT"]
            for tt_ in range(2):
                sct = scb[tt_]
                for g in range(16):
                    bk = 4 + g // 4
                    K.mm(K.ps[bk][:, (g % 4) * 128:(g % 4 + 1) * 128], qpT[:, g, tt_ * 128:(tt_ + 1) * 128], keysT[:, g % 2, :], True, True,
                         [qpT.b, keysT.b], K.bps[bk], skip=True)
                yield
                for q4 in range(4):
                    K.cp("act" if q4 % 2 else "dve", sct[:, q4 * 4:q4 * 4 + 4, :], K.ps[4 + q4][:, :].rearrange("p (g n) -> p g n", n=128), K.bps[4 + q4], [sct.b])
                ti = pc * 2 + tt_
                K.dma(sc_d[ti * 128:(ti + 1) * 128, :], sct[:].rearrange("p g n -> p (g n)"), reads=[sct.b], writes=[bscd[ti]])
                yield

        def rr3(gens):
            live = [g for g in gens if g is not None]
            while live:
                for g_ in list(live):
                    try:
                        next(g_)
                    except StopIteration:
                        live.remove(g_)

        NEARLY = min(4, NTR)
        WSa = alloc_topk_ws()

        def early_topk():
            inner = chain(*[topk_steps(ti, WSa) for ti in range(NEARLY)])
            while True:
                for _ in range(3):
                    try:
                        next(inner)
                    except StopIteration:
                        return
                yield

        etk = None
        rr3([chunk_front(0)])
        for pc in range(NPCR):
            if pc == 2:
                etk = early_topk()
            rr3([chunk_scores(pc), chunk_front(pc + 1) if pc + 1 < NPCR else None])
            if etk is not None:
                for _ in range(6):
                    next(etk, None)
        if NPCR <= 2:
            etk = early_topk()
        drain(etk)
        K.tk_done = NEARLY
        A.release(m0)

    if "b" in SUBP:
        m0 = A.mark()
        NB = 6
        G = [A.alloc([128, PC, 128], BF16, "G") for _ in range(2)]
        wt = [A.alloc([128, 2048], BF16, "wt") for _ in range(NB)]
        mTb = [A.alloc([128, 8, PC], BF16, "mTb") for _ in range(1)]
        pks = [A.alloc([128, 3, PC], F32, "pks") for _ in range(2)]
        npk = [A.alloc([128, 2, PC], F32, "npk") for _ in range(2)]
        ht = A.alloc([128, 512], F32, "htb")
        Ast = [A.alloc([128, 4, 128], BF16, "Ast") for _ in range(2)]
        Bst = [A.alloc([128, 4, 128], BF16, "Bst") for _ in range(2)]
        tmpa = [A.alloc([128, 128], F32, "tmpa") for _ in range(1)]
        ge = [A.alloc([128, PC], BF16, "ge") for _ in range(3)]
        Hs = [A.alloc([128, PC], BF16, "Hs") for _ in range(4)]
        WS = alloc_topk_ws()
        pkl = A.alloc([128, 2, 3, 128], F32, "pkl")
        iotaB = iota.unsqueeze(1).to_broadcast([128, 4, 128])

        def gbuild(pc):
            Gc, pk_, nk = G[pc % 2], pks[pc % 2], npk[pc % 2]
            K.dma(pkl[:], pk_s[pc * PC:(pc + 1) * PC].rearrange("(tt p) a k -> p tt a k", p=128), reads=bpkt[2 * pc:2 * pc + 2], writes=[pkl.b])
            yield
            for tt_ in range(2):
                for a in range(3):
                    K.tr(K.ps[7][:, a * 128:(a + 1) * 128], pkl[:, tt_, a, :], idf, [pkl.b, cst.b], K.bps[7])
                yield
                K.cp("act", pk_[:, :, tt_ * 128:(tt_ + 1) * 128], K.ps[7][:, 0:384].rearrange("p (a t) -> p a t", a=3), K.bps[7], [pk_.b])
                yield
            K.ts("dve", nk[:, 0, :], pk_[:, 0, :], -1.0, None, ALU.mult, None, [pk_.b], [nk.b])
            K.ts("dve", nk[:, 1, :], pk_[:, 2, :], -1.0, None, ALU.mult, None, [pk_.b], [nk.b])
            yield
            for grp in range(PC // 4 + 1):
                if grp < PC // 4:
                    As, Bs = Ast[grp % 2], Bst[grp % 2]
                    for tl in range(4):
                        t = grp * 4 + tl
                        if tl % 2 == 0:
                            K.ts("dve", As[:, tl, :], iota, pk_[:, 0, t:t + 1], pk_[:, 2, t:t + 1], ALU.is_equal, ALU.mult, [cst.b, pk_.b], [As.b])
                        else:
                            ta = tmpa[0]
                            K.act(ta[:], iota, AF.Abs, [cst.b, nk.b], [ta.b], bias=nk[:, 0, t:t + 1])
                            K.act(As[:, tl, :], ta[:], AF.Relu, [ta.b, nk.b, pk_.b], [As.b], scale=nk[:, 1, t:t + 1], bias=pk_[:, 2, t:t + 1])
                    K.tt("dve", Bs[:], iotaB, pk_[:, 1, grp * 4:grp * 4 + 4].unsqueeze(2).to_broadcast([128, 4, 128]), ALU.is_equal, [cst.b, pk_.b], [Bs.b])
                if grp >= 1:
                    g1 = grp - 1
                    As, Bs = Ast[g1 % 2], Bst[g1 % 2]
                    for tl in range(4):
                        K.mm(K.ps[7][:, tl * 128:(tl + 1) * 128], As[:, tl, :], Bs[:, tl, :], True, True, [As.b, Bs.b], K.bps[7], skip=True)
                    K.cp("act", Gc[:, g1 * 4:g1 * 4 + 4, :], K.ps[7][:, :].rearrange("p (t j) -> p t j", j=128), K.bps[7], [Gc.b])
                yield

        LEAD = 4

        def wdma(g):
            jw = g % 128
            K.dma(wt[g % NB][:], tabb_d[jw], reads=[btab[jw]], writes=[wt[g % NB].b])

        def dense(pc, nxt, nxt2):
            Gc, mT = G[pc % 2], mTb[0]
            K.dma(mT[:], mT_d[:, :, pc * PC:(pc + 1) * PC], reads=[bmT[pc]], writes=[mT.b])
            for j in range(130):
                if j < 128:
                    g0 = pc * 128 + j
                    w_ = wt[g0 % NB]
                    ab = 4 + j % 3
                    for k in range(8):
                        K.mm(K.ps[ab][:, 0:PC], w_[:, k * 128:(k + 1) * 128], mT[:, k, :], k == 0, k == 7, [w_.b, mT.b], K.bps[ab])
                    g_, h_ = ge[j % 3], Hs[j % 4]
                    K.act(g_[:], K.ps[ab][:, 0:PC], AF.Gelu, K.bps[ab], [g_.b])
                    K.tt("pool", h_[:], g_[:], Gc[:, :, j], ALU.mult, [g_.b, Gc.b], [h_.b])
                if j >= 2:
                    jj = j - 2
                    w_, h_ = wt[(pc * 128 + jj) % NB], Hs[jj % 4]
                    for tt_ in range(2):
                        for half in range(2):
                            yb = tt_ * 2 + half
                            K.mm(K.ps[yb][:, :], h_[:, tt_ * 128:(tt_ + 1) * 128], w_[:, 1024 + half * 512:1024 + (half + 1) * 512], jj == 0, jj == 127,
                                 [h_.b, w_.b], K.bps[yb])
                if j < 128 and pc * 128 + j + LEAD < NPCR * 128:
                    wdma(pc * 128 + j + LEAD)
                if nxt is not None and j % 2 == 1:
                    next(nxt, None)
                if nxt2 is not None:
                    next(nxt2, None)
            for tt_ in range(2):
                ti = pc * 2 + tt_
                for half in range(2):
                    yb = tt_ * 2 + half
                    hsl = h1_d[ti * 128:(ti + 1) * 128, half * 512:(half + 1) * 512]
                    K.dma(ht[:], hsl, reads=[bh1[ti]], writes=[ht.b])
                    K.tt("dve", ht[:], K.ps[yb][:, :], ht[:], ALU.add, K.bps[yb] + [ht.b], [ht.b])
                    K.dma(hsl, ht[:], reads=[ht.b], writes=[bh1[ti]])

        def topk_chunk(pc):
            if pc >= NPCR:
                return None
            tiles = [ti for ti in (2 * pc, 2 * pc + 1) if ti >= K.tk_done]
            if not tiles:
                return None
            return chain(*[topk_steps(ti, WS) for ti in tiles])

        drain(topk_chunk(0))
        drain(topk_chunk(1))
        drain(gbuild(0))
        for g_ in range(LEAD):
            wdma(g_)
        for pc in range(NPCR):
            nxt = gbuild(pc + 1) if pc + 1 < NPCR else None
            nxt2 = topk_chunk(pc + 2)
            dense(pc, nxt, nxt2)
            drain(nxt2)
            drain(nxt)
        A.release(m0)

    if "c" in SUBP:
        m0 = A.mark()
        wgb = A.alloc([128, 8, 1024], BF16, "wgb")
        wpb = A.alloc([128, 2, 1024], BF16, "wpb")
        m1 = A.mark()
        stg = [A.alloc([128, 1024], F32, "stgg") for _ in range(2)]
        for k in range(8):
            s_ = stg[k % 2]
            K.dma(s_[:], wg_d[k * 128:(k + 1) * 128, :], writes=[s_.b])
            K.cp("act" if k % 2 else "dve", wgb[:, k, :], s_[:], [s_.b], [wgb.b])
        for k in range(2):
            s_ = stg[k % 2]
            K.dma(s_[:], wp_d[k * 128:(k + 1) * 128, :], writes=[s_.b])
            K.cp("act" if k % 2 else "dve", wpb[:, k, :], s_[:], [s_.b], [wpb.b])
        A.release(m1)
        ss = A.alloc([128, 24], F32, "ssc")
        junk = A.alloc([128, 1024], BF16, "junkc")
        CHc = []
        for _ in range(2):
            CHc.append(dict(ht=A.alloc([128, 2, 1024], F32, "htc"), hn=A.alloc([128, 2, 1024], BF16, "hnc"), mT=A.alloc([128, 8, PC], BF16, "mTc"),
                            pt=A.alloc([128, 2, 256], F32, "pt"), pb=A.alloc([128, 2, 256], BF16, "pb"), pT=A.alloc([128, 2, PC], BF16, "pT")))
        gate = [A.alloc([128, 512], F32, "gate") for _ in range(4)]
        tmp = [A.alloc([128, 512], F32, "tmp") for _ in range(4)]
        ot = [A.alloc([128, 1024], F32, "ot") for _ in range(2)]
        s3 = A.alloc([128, 8], F32, "s3")
        gfin = A.alloc([128, 1024], F32, "gfin")
        K.dma(gfin[:], X["rows_d"][:, 0:1024], writes=[gfin.b])

        def pc_front(pc):
            c_ = CHc[pc % 2]
            ht, hn, mT, pt_, pb_, pT = c_["ht"], c_["hn"], c_["mT"], c_["pt"], c_["pb"], c_["pT"]
            load_norm_T(K, h1_d[pc * PC:(pc + 1) * PC, :], ht, hn, mT, ss, junk, g_ple, idb, cstb.b, pk.b, [0, 1], 2, rd=bh1[pc * 2:pc * 2 + 2])
            yield
            K.dma(pt_[:], p_d[pc * PC:(pc + 1) * PC, :].rearrange("(tt p) d -> p tt d", p=128), writes=[pt_.b])
            K.cp("pool", pb_[:], pt_[:], [pt_.b], [pb_.b])
            for kk in range(2):
                for tt_ in range(2):
                    K.tr(K.psb[2][:, kk * PC + tt_ * 128:kk * PC + (tt_ + 1) * 128], pb_[:, tt_, kk * 128:(kk + 1) * 128], idb, [pb_.b, cstb.b], K.bps[2])
            K.cp("act", pT[:], K.psb[2][:, 0:2 * PC].rearrange("p (k t) -> p k t", k=2), K.bps[2], [pT.b])
            yield

        def pc_back(pc):
            c_ = CHc[pc % 2]
            ht, mT, pT = c_["ht"], c_["mT"], c_["pT"]
            for tt_ in range(2):
                for half in range(2):
                    gbk, ebk = 4 + half, 6 + half
                    gt_, tm_ = gate[tt_ * 2 + half], tmp[tt_ * 2 + half]
                    for k in range(8):
                        K.mm(K.ps[gbk][:, :], mT[:, k, tt_ * 128:(tt_ + 1) * 128], wgb[:, k, half * 512:(half + 1) * 512], k == 0, k == 7, [mT.b, wgb.b], K.bps[gbk])
                    K.act(gt_[:], K.ps[gbk][:, :], AF.Sigmoid, K.bps[gbk], [gt_.b])
                    for kk in range(2):
                        K.mm(K.ps[ebk][:, :], pT[:, kk, tt_ * 128:(tt_ + 1) * 128], wpb[:, kk, half * 512:(half + 1) * 512], kk == 0, kk == 1, [pT.b, wpb.b], K.bps[ebk])
                    K.tt("dve", tm_[:], K.ps[ebk][:, :], gt_[:], ALU.mult, K.bps[ebk] + [gt_.b], [tm_.b])
                    K.tt("dve", ht[:, tt_, half * 512:(half + 1) * 512], tm_[:], ht[:, tt_, half * 512:(half + 1) * 512], ALU.add, [tm_.b, ht.b], [ht.b])
                    yield
                o_ = ot[tt_]
                K.act(junk[:], ht[:, tt_, :], AF.Square, [ht.b], [junk.b, s3.b], accum_out=s3[:, 0:1])
                K.ts("pool", s3[:, 1:2], s3[:, 0:1], 1.0 / D, EPS, ALU.mult, ALU.add, [s3.b], [s3.b])
                K.tt("pool", s3[:, 2:3], s3[:, 1:2], K.mhalf[:, 0:1], ALU.pow, [s3.b, K.mhalf.b], [s3.b])
                K.stt(o_[:], ht[:, tt_, :], s3[:, 2:3], gfin[:], ALU.mult, ALU.mult, [ht.b, s3.b, gfin.b], [o_.b])
                r0 = pc * PC + tt_ * 128
                K.dma(out_d[r0:r0 + 128, :], o_[:], reads=[o_.b])
                yield

        def rr2(gens):
            live = [g for g in gens if g is not None]
            while live:
                for g_ in list(live):
                    try:
                        next(g_)
                    except StopIteration:
                        live.remove(g_)

        rr2([pc_front(0)])
        for pc in range(NPCR):
            rr2([pc_back(pc), pc_front(pc + 1) if pc + 1 < NPCR else None])
        A.release(m0)


def _consts():
    ident = np.eye(128, dtype=np.float32)
    tri = (np.arange(128)[:, None] <= np.arange(128)[None, :]).astype(np.float32)
    iota = np.broadcast_to(np.arange(128, dtype=np.float32)[None, :], (128, 128))
    cst = np.ascontiguousarray(np.stack([ident, tri, iota], axis=1))
    half = 32
    inv_freq = (1.0 / (10000.0 ** (np.arange(half, dtype=np.float32) * 2.0 / 64))).astype(np.float32)
    ang = np.arange(S, dtype=np.float32)[:, None] * inv_freq[None, :]
    cos, sin = np.cos(ang).astype(np.float32), np.sin(ang).astype(np.float32)
    f = (np.arange(128) % 64) % 32
    rope = np.ascontiguousarray(np.stack([cos[:, f].T, sin[:, f].T], axis=1))
    return cst, rope


def prep_inputs(inp):
    l = 0
    f32 = lambda a: np.ascontiguousarray(np.asarray(a, dtype=np.float32))
    pkv = np.zeros((128, 160), np.float32)
    pkv[:, 0:8] = f32(inp["attn_norm_g"])[l].reshape(8, 128).T
    pkv[:, 8:16] = f32(inp["ffn_norm_g"])[l].reshape(8, 128).T
    pkv[:, 16:24] = f32(inp["ple_norm_g"])[l].reshape(8, 128).T
    cw = f32(inp["conv_w"])[l]
    pkv[:, 24:148] = cw.reshape(31, 4, 128).transpose(2, 1, 0).reshape(128, 124)
    pkv[:, 148:152] = f32(inp["conv_b"])[l].reshape(4, 128).T
    pkv[:, 152:156] = f32(inp["conv_ln_g"])[l].reshape(4, 128).T
    pkv[:, 156:160] = f32(inp["conv_ln_b"])[l].reshape(4, 128).T
    row = np.concatenate([f32(inp["final_norm_g"]), f32(inp["subln_g"])[l], f32(inp["lambda_q1"])[l],
                          f32(inp["lambda_k1"])[l], f32(inp["lambda_q2"])[l], f32(inp["lambda_k2"])[l]])
    rows = np.ascontiguousarray(np.broadcast_to(row[None, :], (128, 1408)))
    U = f32(inp["peer_u"])[l]
    V = f32(inp["peer_v"])[l]
    tab = np.empty((128, 128, 2048), np.float32)
    tab[:, :, 0:1024] = U.reshape(128, 128, 8, 128).transpose(1, 3, 2, 0).reshape(128, 128, 1024)
    tab[:, :, 1024:2048] = V.reshape(128, 128, 1024).transpose(1, 0, 2)
    cst, rope = _consts()
    shared = dict(w_in=f32(inp["w_in"])[l], w_out=f32(inp["w_out"])[l], wq=f32(inp["peer_wq"])[l],
                  keys=f32(inp["peer_keys"])[l], wg=f32(inp["ple_w_gate"])[l], wp=f32(inp["ple_w_proj"])[l],
                  tab=tab, pk=pkv, rows=rows, cst=cst, rope=rope)
    x = f32(inp["x"])
    p = f32(inp["p"])[l]
    maps = []
    for b in range(NCORES):
        m = dict(shared)
        m["x"] = np.ascontiguousarray(x[b])
        m["p"] = np.ascontiguousarray(p[b])
        maps.append(m)
    return maps


_NC_CACHE = {}


def kernel(**inputs):
    maps = prep_inputs(inputs)
    if "nc" not in _NC_CACHE:
        _NC_CACHE["nc"] = build()
    res = run_bass_kernel_spmd(_NC_CACHE["nc"], maps, core_ids=list(range(NCORES)))
    return np.stack([np.asarray(r["out"], dtype=np.float32) for r in res.results], axis=0)
```
